# Optimizing a Trainium2 kernel written in Bass

```python
import jax
import jax.numpy as jnp
from jax import lax
import numpy as np

D_MODEL = 1024
BATCH = 8
SEQ = 4096
DEPTH = 4

N_MIXERS = 3
N_HEADS = 16
HEAD_DIM = D_MODEL // N_HEADS
CONV_WIDTH = 3
Q_BLOCK = 128
N_EXPERTS = 32
TOP_K = 4
D_EXPERT = D_MODEL
SWIGLU_LIMIT = 7.0
SWIGLU_ALPHA = 1.702
PLE_DIM = 256
EXPERT_ROW_BLOCK = 512
LN_EPS = 1e-5
DEEPNORM_ALPHA = (2 * DEPTH) ** 0.25
DEEPNORM_BETA = (8 * DEPTH) ** -0.25
FOX_GATE_BIAS = 3.0

kernel_name = 'hybrid_conv_stickbreak_fox_moe'


def layer_norm(x, g, b):
    xf = x.astype(jnp.float32)
    mu = jnp.mean(xf, axis=-1, keepdims=True)
    var = jnp.mean(jnp.square(xf - mu), axis=-1, keepdims=True)
    y = (xf - mu) * lax.rsqrt(var + LN_EPS)
    return (y * g.astype(jnp.float32) + b.astype(jnp.float32)).astype(x.dtype)


def split_heads(t):
    bsz, s, _ = t.shape
    return t.reshape(bsz, s, N_HEADS, HEAD_DIM)


def short_conv_mixer(x, w_in, w_conv, w_out):
    s = x.shape[1]
    gate_b, gate_c, h = jnp.split(x @ w_in, 3, axis=-1)
    u = jnp.pad(gate_c * h, ((0, 0), (CONV_WIDTH - 1, 0), (0, 0)))
    conv = sum(w_conv[k] * u[:, k:k + s] for k in range(CONV_WIDTH))
    return (gate_b * conv) @ w_out


def stick_breaking_mixer(x, w_in, w_out):
    bsz, s, d = x.shape
    q, k, v = (split_heads(t) for t in jnp.split(x @ w_in, 3, axis=-1))
    scale = HEAD_DIM ** -0.5
    outs = []
    for q0 in range(0, s, Q_BLOCK):
        q1 = q0 + Q_BLOCK
        z = jnp.einsum('bqhd,bkhd->bhqk', q[:, q0:q1], k[:, :q1],
                       preferred_element_type=jnp.float32) * scale
        t_pos = jnp.arange(q0, q1)[:, None]
        s_pos = jnp.arange(q1)[None, :]
        strict = s_pos < t_pos
        log_not = jnp.where(strict, -jax.nn.softplus(z), 0.0)
        between = lax.cumsum(log_not, axis=3, reverse=True) - log_not
        w = jnp.where(strict, jnp.exp(jax.nn.log_sigmoid(z) + between), 0.0)
        outs.append(jnp.einsum('bhqk,bkhd->bqhd', w.astype(v.dtype), v[:, :q1]))
    o = jnp.concatenate(outs, axis=1).reshape(bsz, s, d)
    return o @ w_out


def forgetting_attention_mixer(x, w_in, b_f, w_out):
    bsz, s, d = x.shape
    proj = x @ w_in
    q, k, v = (split_heads(t) for t in jnp.split(proj[..., :3 * d], 3, axis=-1))
    log_f = jax.nn.log_sigmoid(proj[..., 3 * d:].astype(jnp.float32) + b_f.astype(jnp.float32))
    cum = jnp.swapaxes(jnp.cumsum(log_f, axis=1), 1, 2)
    scale = HEAD_DIM ** -0.5
    outs = []
    for q0 in range(0, s, Q_BLOCK):
        q1 = q0 + Q_BLOCK
        z = jnp.einsum('bqhd,bkhd->bhqk', q[:, q0:q1], k[:, :q1],
                       preferred_element_type=jnp.float32) * scale
        z = z + cum[:, :, q0:q1, None] - cum[:, :, None, :q1]
        causal = jnp.arange(q1)[None, :] <= jnp.arange(q0, q1)[:, None]
        probs = jax.nn.softmax(jnp.where(causal, z, -jnp.inf), axis=-1)
        outs.append(jnp.einsum('bhqk,bkhd->bqhd', probs.astype(v.dtype), v[:, :q1]))
    o = jnp.concatenate(outs, axis=1).reshape(bsz, s, d)
    return o @ w_out


def clamped_swiglu(h_gate, h_up):
    h_gate = jnp.minimum(h_gate, SWIGLU_LIMIT)
    h_up = jnp.clip(h_up, -SWIGLU_LIMIT, SWIGLU_LIMIT)
    return h_gate * jax.nn.sigmoid(SWIGLU_ALPHA * h_gate) * (h_up + 1.0)


def moe_ffn(x2d, w_router, b_router, w_gate, b_gate, w_up, b_up, w_down, b_down):
    n_tok, d = x2d.shape
    logits = (x2d @ w_router).astype(jnp.float32) + b_router.astype(jnp.float32)
    top_logit, top_e = lax.top_k(logits, TOP_K)
    gate = jax.nn.softmax(top_logit, axis=-1)
    n_slots = n_tok * TOP_K
    flat_e = top_e.reshape(-1)
    flat_tok = jnp.repeat(jnp.arange(n_tok, dtype=jnp.int32), TOP_K)
    order = jnp.argsort(flat_e)
    se, stok, sgate = flat_e[order], flat_tok[order], gate.reshape(-1)[order]
    counts = jnp.bincount(flat_e, length=N_EXPERTS)
    padded = (counts + EXPERT_ROW_BLOCK - 1) // EXPERT_ROW_BLOCK * EXPERT_ROW_BLOCK
    start = jnp.cumsum(counts) - counts
    pend = jnp.cumsum(padded)
    pstart = pend - padded
    dest = pstart[se] + jnp.arange(n_slots) - start[se]
    n_blocks = -(-n_slots // EXPERT_ROW_BLOCK) + N_EXPERTS
    n_rows = n_blocks * EXPERT_ROW_BLOCK
    row_tok = jnp.zeros((n_rows,), jnp.int32).at[dest].set(stok)
    row_gate = jnp.zeros((n_rows,), jnp.float32).at[dest].set(sgate)
    block_start = jnp.arange(n_blocks) * EXPERT_ROW_BLOCK
    block_e = jnp.minimum(jnp.sum(pend[None, :] <= block_start[:, None], axis=1), N_EXPERTS - 1)

    def expert_block(args):
        tok, g, e = args
        xb = x2d[tok]
        a = clamped_swiglu(xb @ w_gate[e] + b_gate[e], xb @ w_up[e] + b_up[e])
        return (a @ w_down[e] + b_down[e]) * g[:, None].astype(x2d.dtype)

    out = lax.map(expert_block, (row_tok.reshape(n_blocks, EXPERT_ROW_BLOCK),
                                 row_gate.reshape(n_blocks, EXPERT_ROW_BLOCK), block_e))
    return jax.ops.segment_sum(out.reshape(n_rows, d), row_tok, num_segments=n_tok)


def setup_inputs(seed: int = 0) -> dict:
    key = jax.random.key(seed)
    ks = jax.random.split(key, 25)
    d, f, h, e = D_MODEL, D_EXPERT, N_HEADS, N_EXPERTS
    n_a = len(range(0, DEPTH, N_MIXERS))
    n_b = len(range(1, DEPTH, N_MIXERS))
    n_c = len(range(2, DEPTH, N_MIXERS))

    def nrm(k, shape, s):
        return jax.random.normal(k, shape, jnp.float32) * s

    return {
        'x': nrm(ks[0], (BATCH, SEQ, d), 1.0),
        'p': nrm(ks[1], (DEPTH, BATCH, SEQ, PLE_DIM), 1.0),
        'conv_w_in': nrm(ks[2], (n_a, d, 3 * d), d ** -0.5),
        'conv_w': nrm(ks[3], (n_a, CONV_WIDTH, d), CONV_WIDTH ** -0.5),
        'conv_w_out': nrm(ks[4], (n_a, d, d), d ** -0.5 * DEEPNORM_BETA),
        'sb_w_in': nrm(ks[5], (n_b, d, 3 * d), d ** -0.5),
        'sb_w_out': nrm(ks[6], (n_b, d, d), d ** -0.5 * DEEPNORM_BETA),
        'fox_w_in': nrm(ks[7], (n_c, d, 3 * d + h), d ** -0.5),
        'fox_b_f': FOX_GATE_BIAS + nrm(ks[8], (n_c, h), 0.5),
        'fox_w_out': nrm(ks[9], (n_c, d, d), d ** -0.5 * DEEPNORM_BETA),
        'ln1_g': 1.0 + nrm(ks[10], (DEPTH, d), 0.02),
        'ln1_b': nrm(ks[11], (DEPTH, d), 0.02),
        'ln2_g': 1.0 + nrm(ks[12], (DEPTH, d), 0.02),
        'ln2_b': nrm(ks[13], (DEPTH, d), 0.02),
        'router_w': nrm(ks[14], (DEPTH, d, e), d ** -0.5),
        'router_b': nrm(ks[15], (DEPTH, e), 0.01),
        'exp_w_gate': nrm(ks[16], (DEPTH, e, d, f), d ** -0.5),
        'exp_b_gate': nrm(ks[17], (DEPTH, e, f), 0.01),
        'exp_w_up': nrm(ks[18], (DEPTH, e, d, f), d ** -0.5),
        'exp_b_up': nrm(ks[19], (DEPTH, e, f), 0.01),
        'exp_w_down': nrm(ks[20], (DEPTH, e, f, d), f ** -0.5 * DEEPNORM_BETA),
        'exp_b_down': nrm(ks[21], (DEPTH, e, d), 0.01),
        'ple_w_proj': nrm(ks[22], (DEPTH, PLE_DIM, d), PLE_DIM ** -0.5 * DEEPNORM_BETA),
        'ple_w_gate': nrm(ks[23], (DEPTH, d, d), d ** -0.5),
        'ple_b_gate': nrm(ks[24], (DEPTH, d), 0.01),
    }


def reference(x, p, conv_w_in, conv_w, conv_w_out, sb_w_in, sb_w_out, fox_w_in, fox_b_f, fox_w_out,
              ln1_g, ln1_b, ln2_g, ln2_b, router_w, router_b, exp_w_gate, exp_b_gate, exp_w_up,
              exp_b_up, exp_w_down, exp_b_down, ple_w_proj, ple_w_gate, ple_b_gate):
    bsz, s, d = x.shape
    for i in range(DEPTH):
        kind, j = i % N_MIXERS, i // N_MIXERS
        if kind == 0:
            mix = short_conv_mixer(x, conv_w_in[j], conv_w[j], conv_w_out[j])
        elif kind == 1:
            mix = stick_breaking_mixer(x, sb_w_in[j], sb_w_out[j])
        else:
            mix = forgetting_attention_mixer(x, fox_w_in[j], fox_b_f[j], fox_w_out[j])
        x = layer_norm(DEEPNORM_ALPHA * x + mix, ln1_g[i], ln1_b[i])
        ffn = moe_ffn(x.reshape(bsz * s, d), router_w[i], router_b[i], exp_w_gate[i], exp_b_gate[i],
                      exp_w_up[i], exp_b_up[i], exp_w_down[i], exp_b_down[i]).reshape(bsz, s, d)
        x = layer_norm(DEEPNORM_ALPHA * x + ffn, ln2_g[i], ln2_b[i])
        x = x + (p[i] @ ple_w_proj[i]) * jax.nn.sigmoid(x @ ple_w_gate[i] + ple_b_gate[i])
    return x
```

```python
import contextlib
import numpy as np
import concourse.bass as bass
import concourse.mybir as mybir
from concourse.bass_utils import run_bass_kernel_spmd

F32 = mybir.dt.float32
BF16 = mybir.dt.bfloat16
I32 = mybir.dt.int32
AF = mybir.ActivationFunctionType
ALU = mybir.AluOpType
AX = mybir.AxisListType

D = 1024
NE = 32
PLE = 256
ALPHA = 8 ** 0.25
LN_EPS = 1e-5
KINDS = (0, 1, 2, 0)
JIDX = (0, 0, 0, 1)


class Tok:
    __slots__ = ("name", "w", "r", "dkey")

    def __init__(self, name):
        self.name = name
        self.w = {}
        self.r = {}
        self.dkey = None


class KB:
    def __init__(self, nc, es):
        self.nc = nc
        self.es = es
        self.eng = dict(pe=nc.tensor, dve=nc.vector, act=nc.scalar, pool=nc.gpsimd, sp=nc.sync)
        self.sems, self.cnt, self.isdma = {}, {}, {}
        self.waited = {k: {} for k in self.eng}
        for k in self.eng:
            self._newsem(k, False)
        self.ntok = 0
        self.free_slots = []
        for i in range(64):
            self._newsem(f"d{i}", True)
            self.free_slots.append(f"d{i}")
        self.live = []
        self.mark = {}

    def _newsem(self, key, isdma):
        self.sems[key] = self.es.enter_context(self.nc.semaphore("s_" + key))
        self.cnt[key] = 0
        self.isdma[key] = isdma

    def tok(self, name="t"):
        self.ntok += 1
        return Tok(f"{name}{self.ntok}")

    def _wait(self, e, evs):
        need = {}
        for ev in evs:
            for k, v in ev.items():
                if self.isdma[k]:
                    if v <= self.mark.get(k, 0):
                        continue
                    v = self.cnt[k]
                if v > need.get(k, 0):
                    need[k] = v
        for k, v in need.items():
            if self.waited[e].get(k, 0) >= v:
                continue
            self.eng[e].wait_ge(self.sems[k], v)
            self.waited[e][k] = v

    def op(self, e, emit, reads=(), writes=()):
        evs = [t.w for t in reads] + [t.w for t in writes] + [t.r for t in writes]
        self._wait(e, evs)
        inst = emit(self.eng[e])
        self.cnt[e] += 1
        inst.then_inc(self.sems[e], 1)
        v = self.cnt[e]
        for t in reads:
            t.r[e] = v
        for t in writes:
            t.w = {e: v}
            t.r = {}
        return inst

    def dma(self, q, emit, reads=(), writes=(), key=None, join=False):
        evs = [t.w for t in reads] + [t.r for t in writes]
        if not join:
            evs += [t.w for t in writes]
        self._wait(q, evs)
        if key.dkey is None:
            key.dkey = self.free_slots.pop(0)
            self.live.append(key)
        k = key.dkey
        inst = emit(self.eng[q])
        self.cnt[k] += 16
        inst.then_inc(self.sems[k], 16)
        v = self.cnt[k]
        for t in reads:
            t.r[k] = v
        for t in writes:
            if join:
                t.w[k] = v
            else:
                t.w = {k: v}
                t.r = {}
        return inst

    def barrier(self):
        allev = [dict(self.cnt)]
        for e in self.eng:
            self._wait(e, allev)
        self.mark = {k: v for k, v in self.cnt.items() if self.isdma[k]}
        for t in self.live:
            self.free_slots.append(t.dkey)
            t.dkey = None
        self.live = []


def _cfg_caps(T):
    return {4096: 640, 512: 128, 1024: 256}[T]


def build_program(T=4096, layers=(0, 1, 2, 3), dbg=False):
    C = _cfg_caps(T)
    NT = T // 128
    NQ = T // 512
    CR = C // 128
    NSLOT = NE * C
    nc = bass.Bass("TRN2", target_bir_lowering=False)

    def din(name, shape, dt=F32):
        return nc.dram_tensor(name, list(shape), dt, kind="ExternalInput").ap()

    def dscr(name, shape, dt=F32):
        return nc.dram_tensor(name, list(shape), dt).ap()

    x_in = din("x", [T, D])
    p_in = din("p", [4, T, PLE])
    conv_w_in = din("conv_w_in", [2, D, 3 * D])
    conv_w = din("conv_w", [2, 3, D])
    conv_w_out = din("conv_w_out", [2, D, D])
    sb_w_in = din("sb_w_in", [1, D, 3 * D])
    sb_w_out = din("sb_w_out", [1, D, D])
    fox_w_in = din("fox_w_in", [1, D, 3 * D + 16])
    fox_b_f = din("fox_b_f", [1, 16])
    fox_w_out = din("fox_w_out", [1, D, D])
    ln1_g = din("ln1_g", [4, D]); ln1_b = din("ln1_b", [4, D])
    ln2_g = din("ln2_g", [4, D]); ln2_b = din("ln2_b", [4, D])
    router_w = din("router_w", [4, D, NE]); router_b = din("router_b", [4, NE])
    exp_w_gate = din("exp_w_gate", [4, NE, D, D]); exp_b_gate = din("exp_b_gate", [4, NE, D])
    exp_w_up = din("exp_w_up", [4, NE, D, D]); exp_b_up = din("exp_b_up", [4, NE, D])
    exp_w_down = din("exp_w_down", [4, NE, D, D]); exp_b_down = din("exp_b_down", [4, NE, D])
    ple_w_proj = din("ple_w_proj", [4, PLE, D]); ple_w_gate = din("ple_w_gate", [4, D, D])
    ple_b_gate = din("ple_b_gate", [4, D])
    out_d = nc.dram_tensor("out", [T, D], F32, kind="ExternalOutput").ap()

    xres = [dscr("xres0", [T, D]), dscr("xres1", [T, D])]
    x1res = dscr("x1res", [T, D])
    x1bf = dscr("x1bf", [T, D], BF16)
    yslots = dscr("yslots", [NSLOT + 128, D])
    toklist = dscr("toklist", [NSLOT + 128, 1], I32)
    qT_d = dscr("qT_d", [8, 128, T], BF16)
    kT_d = dscr("kT_d", [8, 128, T], BF16)
    v_d = dscr("v_d", [T, D], BF16)
    dbg_d = None
    if dbg:
        dbg_d = nc.dram_tensor("dbg", [T, D], F32, kind="ExternalOutput").ap()

    es = contextlib.ExitStack()
    with es:
        kb = KB(nc, es)
        op, dma = kb.op, kb.dma

        uid = [0]

        def sb(name, shape, dt=F32, scope=es):
            uid[0] += 1
            return scope.enter_context(nc.sbuf_tensor(f"{name}_{uid[0]}", list(shape), dt))

        def ps(name, shape, dt=F32, scope=es):
            uid[0] += 1
            return scope.enter_context(nc.psum_tensor(f"{name}_{uid[0]}", list(shape), dt))

        t_const = kb.tok("const")
        ident32 = sb("ident32", [128, 128])
        identb = sb("identb", [128, 128], BF16)
        ones_b = sb("ones_b", [128, 128], BF16)
        tri_lt = sb("tri_lt", [128, 128], BF16)
        ntri_ge = sb("ntri_ge", [128, 128], BF16)
        nones_b = sb("nones_b", [128, 128], BF16)
        mask_lt = sb("mask_lt", [128, 128], BF16)
        mask_le = sb("mask_le", [128, 128], BF16)
        c_one = sb("c_one", [128, 1])
        c_eps = sb("c_eps", [128, 1])
        c_nhalf = sb("c_nhalf", [128, 1])
        tokid = sb("tokid", [128, NT], I32)
        ecol = sb("ecol", [128, NE])
        zrow = sb("zrow", [128, D])
        zi = sb("zi", [128, (NSLOT + 128) // 128], I32)
        tmp32 = sb("tmp32", [128, 128])

        def mk_tri(dst, cmp, mult_p, mult_f, fillv, inv):
            def e1(g):
                return g.memset(tmp32[:], inv)
            op("pool", e1, writes=[t_const])

            def e2(g):
                return g.affine_select(out=tmp32[:], in_=tmp32[:], pattern=[[mult_f, 128]], compare_op=cmp,
                                       fill=fillv, base=0, channel_multiplier=mult_p)
            op("pool", e2, writes=[t_const])
            op("dve", lambda v: v.tensor_copy(out=dst[:], in_=tmp32[:]), reads=[t_const], writes=[t_const])

        mk_tri(ident32, ALU.not_equal, 1, -1, 1.0, 0.0)
        mk_tri(identb, ALU.not_equal, 1, -1, 1.0, 0.0)
        mk_tri(tri_lt, ALU.is_gt, -1, 1, 0.0, 1.0)
        mk_tri(mask_lt, ALU.is_gt, -1, 1, 0.0, 1.0)
        mk_tri(mask_le, ALU.is_ge, -1, 1, 0.0, 1.0)
        mk_tri(ntri_ge, ALU.is_ge, 1, -1, 0.0, -1.0)
        zeros_b = sb("zeros_b", [128, 128], BF16)
        sel0 = sb("sel0", [128, 128], BF16)
        mk_tri(sel0, ALU.is_equal, 1, 0, 0.0, 1.0)
        negmask_gt = sb("negmask_gt", [128, 128], BF16)
        mk_tri(negmask_gt, ALU.is_gt, 1, -1, 0.0, -30000.0)
        selh = sb("selh", [16, 16, 128], BF16)
        selt = sb("selt", [16, 16, 128])
        op("pool", lambda g: g.memset(selt[:], 1.0), writes=[t_const])
        op("pool", lambda g: g.affine_select(out=selt[:], in_=selt[:], pattern=[[-1, 16], [0, 128]], compare_op=ALU.is_equal,
                                             fill=0.0, base=0, channel_multiplier=1), writes=[t_const])
        op("dve", lambda v: v.tensor_copy(out=selh[:], in_=selt[:]), reads=[t_const], writes=[t_const])
        op("pool", lambda g: g.memset(zeros_b[:], 0.0), writes=[t_const])
        op("pool", lambda g: g.memset(ones_b[:], 1.0), writes=[t_const])
        op("pool", lambda g: g.memset(nones_b[:], -1.0), writes=[t_const])
        op("pool", lambda g: g.memset(c_one[:], 1.0), writes=[t_const])
        op("pool", lambda g: g.memset(c_eps[:], LN_EPS), writes=[t_const])
        op("pool", lambda g: g.memset(c_nhalf[:], -0.5), writes=[t_const])
        op("pool", lambda g: g.memset(zrow[:], 0.0), writes=[t_const])
        op("pool", lambda g: g.memset(zi[:], 0), writes=[t_const])
        op("pool", lambda g: g.iota(tokid[:], pattern=[[128, NT]], base=0, channel_multiplier=1), writes=[t_const])
        op("pool", lambda g: g.iota(ecol[:], pattern=[[C, NE]], base=0, channel_multiplier=0,
                                    allow_small_or_imprecise_dtypes=True), writes=[t_const])
        t_ys = kb.tok("yslots")
        t_tl = kb.tok("toklist")
        dma("sp", lambda q: q.dma_start(out=yslots[NSLOT:NSLOT + 128, :], in_=zrow[:]), reads=[t_const],
            writes=[t_ys], key=t_ys, join=True)

        t_xres = [kb.tok("xres0"), kb.tok("xres1")]
        t_x1res = kb.tok("x1res")
        t_x1bf = kb.tok("x1bf")
        t_qkv = kb.tok("qkv")
        t_out = kb.tok("out")

        def layer_norm_tile(h, g_bc, b_bc, xo, stat, t_h, t_xo, t_stat, t_gb, eng2="pool"):
            def s1(v):
                v.bn_stats(out=stat[:, 0:6], in_=h[:, 0:512])
                return v.bn_stats(out=stat[:, 6:12], in_=h[:, 512:1024])
            op("dve", s1, reads=[t_h], writes=[t_stat])
            op("dve", lambda v: v.bn_aggr(out=stat[:, 12:14], in_=stat[:, 0:12]), reads=[t_stat], writes=[t_stat])
            op("pool", lambda g: g.tensor_tensor(out=stat[:, 14:15], in0=stat[:, 13:14], in1=c_eps[:], op=ALU.add),
               reads=[t_stat, t_const], writes=[t_stat])
            op("pool", lambda g: g.tensor_tensor(out=stat[:, 15:16], in0=stat[:, 14:15], in1=c_nhalf[:], op=ALU.pow),
               reads=[t_stat, t_const], writes=[t_stat])
            op("dve", lambda v: v.tensor_scalar(out=xo[:], in0=h[:], scalar1=stat[:, 12:13], scalar2=stat[:, 15:16],
                                                op0=ALU.subtract, op1=ALU.mult),
               reads=[t_h, t_stat], writes=[t_xo])
            op(eng2, lambda g: g.tensor_tensor(out=xo[:], in0=xo[:], in1=g_bc[:], op=ALU.mult),
               reads=[t_gb], writes=[t_xo])
            op(eng2, lambda g: g.tensor_tensor(out=xo[:], in0=xo[:], in1=b_bc[:], op=ALU.add),
               reads=[t_gb], writes=[t_xo])

        def bcast_load(dst, src_row, t_dst, q="sp"):
            n = dst.shape[-1] if hasattr(dst, "shape") else None
            dma(q, lambda e: e.dma_start(out=dst[:], in_=src_row.partition_broadcast(128)), writes=[t_dst], key=t_dst)

        def phase_xT(xin, t_xin, xT, t_xT):
            with contextlib.ExitStack() as sc:
                xs = [sb(f"xs{i}", [128, D], scope=sc) for i in range(2)]
                xb = [sb(f"xb{i}", [128, D], BF16, scope=sc) for i in range(2)]
                pT = [ps(f"pT{i}", [128, 8, 128], BF16, scope=sc) for i in range(2)]
                t_xs = [kb.tok("xs") for _ in range(2)]
                t_xb = [kb.tok("xb") for _ in range(2)]
                t_pT = [kb.tok("pT") for _ in range(2)]
                for i in range(NT):
                    b = i % 2
                    dma("sp", lambda q: q.dma_start(out=xs[b][:], in_=xin[i * 128:(i + 1) * 128, :]),
                        reads=[t_xin], writes=[t_xs[b]], key=t_xs[b])
                    op("act", lambda a: a.copy(out=xb[b][:], in_=xs[b][:]), reads=[t_xs[b]], writes=[t_xb[b]])

                    def tr(pe):
                        for c in range(8):
                            r = pe.transpose(out=pT[b][:, c, :], in_=xb[b][:, c * 128:(c + 1) * 128], identity=identb[:])
                        return r
                    op("pe", tr, reads=[t_xb[b], t_const], writes=[t_pT[b]])
                    op("dve", lambda v: v.tensor_copy(out=xT[:, :, i * 128:(i + 1) * 128], in_=pT[b][:]),
                       reads=[t_pT[b]], writes=[t_xT])
                kb.barrier()

        def phase_conv(j, xT, t_xT, mT, t_mT):
            with contextlib.ExitStack() as sc:
                wc = [sb(f"wc{i}", [128, 8, 384], BF16, scope=sc) for i in range(2)]
                t_wc = [kb.tok("wc") for _ in range(2)]
                cw = sb("cw", [128, 8, 3], scope=sc)
                cwr = sb("cwr", [3, D], scope=sc)
                cwb = sb("cwb", [3, D], BF16, scope=sc)
                t_cw = kb.tok("cw")
                pcw = ps("pcw", [128, 8, 4], BF16, scope=sc)
                t_pcw = kb.tok("pcw")
                pB = [ps(f"pB{i}", [128, 512], scope=sc) for i in range(2)]
                pC = [ps(f"pC{i}", [128, 512], scope=sc) for i in range(2)]
                pH = [ps(f"pH{i}", [128, 512], scope=sc) for i in range(2)]
                t_pB = [kb.tok("pB") for _ in range(2)]
                t_pC = [kb.tok("pC") for _ in range(2)]
                t_pH = [kb.tok("pH") for _ in range(2)]
                ctmp = [sb(f"ctmp{i}", [128, 512], scope=sc) for i in range(2)]
                t_ct = [kb.tok("ct") for _ in range(2)]
                u = [sb(f"u{i}", [128, 514], scope=sc) for i in range(2)]
                t_u = [kb.tok("u") for _ in range(2)]
                cv = [sb(f"cv{i}", [128, 512], scope=sc) for i in range(2)]
                t_cv = [kb.tok("cv") for _ in range(2)]
                cwlo = sb("cwlo", [3, D], BF16, scope=sc)
                cwt = sb("cwt", [3, D], scope=sc)
                dma("sp", lambda q: q.dma_start(out=cwr[:], in_=conv_w[j]), writes=[t_cw], key=t_cw)
                op("dve", lambda v: v.tensor_copy(out=cwb[:], in_=cwr[:]), reads=[t_cw], writes=[t_cw])
                op("dve", lambda v: v.tensor_copy(out=cwt[:], in_=cwb[:]), reads=[t_cw], writes=[t_cw])
                op("dve", lambda v: v.tensor_tensor(out=cwt[:], in0=cwr[:], in1=cwt[:], op=ALU.subtract), writes=[t_cw])
                op("dve", lambda v: v.tensor_copy(out=cwlo[:], in_=cwt[:]), writes=[t_cw])
                for part, src in ((0, cwb), (1, cwlo)):
                    def tr(pe):
                        for c in range(8):
                            r = pe.transpose(out=pcw[:, c, 0:3], in_=src[:, c * 128:(c + 1) * 128], identity=identb[0:3, 0:3])
                        return r
                    op("pe", tr, reads=[t_cw, t_const], writes=[t_pcw])
                    if part == 0:
                        op("dve", lambda v: v.tensor_copy(out=cw[:], in_=pcw[:, :, 0:3]), reads=[t_pcw], writes=[t_cw])
                    else:
                        op("dve", lambda v: v.tensor_tensor(out=cw[:], in0=cw[:], in1=pcw[:, :, 0:3], op=ALU.add),
                           reads=[t_pcw], writes=[t_cw])
                wsrc = conv_w_in[j].rearrange("(kc p) n -> p kc n", p=128)

                def load_w(c):
                    b = c % 2
                    for g in range(3):
                        dma("pool", lambda q: q.dma_start(out=wc[b][:, :, g * 128:(g + 1) * 128],
                                                          in_=wsrc[:, :, g * 1024 + c * 128:g * 1024 + (c + 1) * 128]),
                            writes=[t_wc[b]], key=t_wc[b], join=(g > 0))
                load_w(0)
                it = 0
                for c in range(8):
                    if c + 1 < 8:
                        load_w(c + 1)
                    wb_ = wc[c % 2]
                    for tq in range(NQ):
                        b = it % 2
                        it += 1
                        for (pp, tp, g) in ((pB[b], t_pB[b], 0), (pC[b], t_pC[b], 1), (pH[b], t_pH[b], 2)):
                            def mm(pe):
                                for kc in range(8):
                                    r = pe.matmul(pp[:], lhsT=wb_[:, kc, g * 128:(g + 1) * 128],
                                                  rhs=xT[:, kc, tq * 512:(tq + 1) * 512], start=(kc == 0), stop=(kc == 7))
                                return r
                            op("pe", mm, reads=[t_wc[c % 2], t_xT], writes=[tp])
                        op("act", lambda a: a.copy(out=ctmp[b][:], in_=pC[b][:]), reads=[t_pC[b]], writes=[t_ct[b]])
                        if tq == 0:
                            op("pool", lambda g_: g_.memset(u[b][:, 0:2], 0.0), writes=[t_u[b]])
                        else:
                            op("pool", lambda g_: g_.tensor_copy(out=u[b][:, 0:2], in_=u[1 - b][:, 512:514]),
                               reads=[t_u[1 - b]], writes=[t_u[b]])
                        op("dve", lambda v: v.tensor_tensor(out=u[b][:, 2:514], in0=ctmp[b][:], in1=pH[b][:], op=ALU.mult),
                           reads=[t_ct[b], t_pH[b]], writes=[t_u[b]])
                        op("dve", lambda v: v.tensor_scalar(out=cv[b][:], in0=u[b][:, 2:514], scalar1=cw[:, c, 2:3],
                                                            scalar2=None, op0=ALU.mult),
                           reads=[t_u[b], t_cw], writes=[t_cv[b]])
                        op("dve", lambda v: v.scalar_tensor_tensor(out=cv[b][:], in0=u[b][:, 1:513], scalar=cw[:, c, 1:2],
                                                                   in1=cv[b][:], op0=ALU.mult, op1=ALU.add),
                           reads=[t_u[b], t_cw], writes=[t_cv[b]])
                        op("dve", lambda v: v.scalar_tensor_tensor(out=cv[b][:], in0=u[b][:, 0:512], scalar=cw[:, c, 0:1],
                                                                   in1=cv[b][:], op0=ALU.mult, op1=ALU.add),
                           reads=[t_u[b], t_cw], writes=[t_cv[b]])
                        op("dve", lambda v: v.tensor_tensor(out=mT[:, c, tq * 512:(tq + 1) * 512], in0=cv[b][:],
                                                            in1=pB[b][:], op=ALU.mult),
                           reads=[t_cv[b], t_pB[b]], writes=[t_mT])
                kb.barrier()

        def phase_qkv(kind, w_in_d, xT, t_xT, ncum, refB, t_cum):
            with contextlib.ExitStack() as sc:
                wsrc = w_in_d.rearrange("(kc p) n -> p kc n", p=128)
                wq = [sb(f"wq{i}", [128, 8, 512], BF16, scope=sc) for i in range(2)]; t_wq = [kb.tok("wq") for _ in range(2)]
                wv = sb("wv", [128, 8, D], BF16, scope=sc); t_wv = kb.tok("wv")
                stg = [sb(f"stg{i}", [128, T], BF16, scope=sc) for i in range(2)]; t_stg = [kb.tok("stg") for _ in range(2)]
                vst = [sb(f"vst{i}", [128, D], BF16, scope=sc) for i in range(2)]; t_vst = [kb.tok("vst") for _ in range(2)]
                pq = [ps(f"pq{i}", [128, 512], scope=sc) for i in range(2)]; t_pq = [kb.tok("pq") for _ in range(2)]
                pv = [ps(f"pv{i}", [128, 512], scope=sc) for i in range(2)]; t_pv = [kb.tok("pv") for _ in range(2)]
                dma("pool", lambda q: q.dma_start(out=wv[:], in_=wsrc[:, :, 2048:3072]), writes=[t_wv], key=t_wv)

                def load_g(g):
                    dma("pool", lambda q: q.dma_start(out=wq[g % 2][:], in_=wsrc[:, :, g * 512:(g + 1) * 512]),
                        writes=[t_wq[g % 2]], key=t_wq[g % 2])
                load_g(0)
                it = 0
                for g in range(4):
                    if g + 1 < 4:
                        load_g(g + 1)
                    for cc in range(4):
                        c16 = g * 4 + cc
                        sbuf_ = stg[c16 % 2]; ts_ = t_stg[c16 % 2]
                        for tq in range(NQ):
                            b = it % 2
                            it += 1

                            def mm(pe):
                                for kc in range(8):
                                    r = pe.matmul(pq[b][:], lhsT=wq[g % 2][:, kc, cc * 128:(cc + 1) * 128],
                                                  rhs=xT[:, kc, tq * 512:(tq + 1) * 512], start=(kc == 0), stop=(kc == 7))
                                return r
                            op("pe", mm, reads=[t_wq[g % 2], t_xT], writes=[t_pq[b]])
                            scl = 0.125 if c16 < 8 else 1.0
                            if it % 2 == 0:
                                op("act", lambda a: a.mul(out=sbuf_[:, tq * 512:(tq + 1) * 512], in_=pq[b][:], mul=scl),
                                   reads=[t_pq[b]], writes=[ts_])
                            else:
                                op("dve", lambda v: v.tensor_scalar(out=sbuf_[:, tq * 512:(tq + 1) * 512], in0=pq[b][:], scalar1=scl,
                                                                    scalar2=None, op0=ALU.mult), reads=[t_pq[b]], writes=[ts_])
                        dst = qT_d[c16] if c16 < 8 else kT_d[c16 - 8]
                        dma("sp", lambda q: q.dma_start(out=dst, in_=sbuf_[:]), reads=[ts_], writes=[t_qkv], key=ts_, join=True)
                for i in range(NT):
                    b = i % 2
                    for hh in range(2):
                        def mmv(pe):
                            for kc in range(8):
                                r = pe.matmul(pv[hh][:], lhsT=xT[:, kc, i * 128:(i + 1) * 128],
                                              rhs=wv[:, kc, hh * 512:(hh + 1) * 512], start=(kc == 0), stop=(kc == 7))
                            return r
                        op("pe", mmv, reads=[t_xT, t_wv], writes=[t_pv[hh]])
                        if hh == 0:
                            op("act", lambda a: a.copy(out=vst[b][:, 0:512], in_=pv[hh][:]), reads=[t_pv[hh]], writes=[t_vst[b]])
                        else:
                            op("dve", lambda v: v.tensor_copy(out=vst[b][:, 512:1024], in_=pv[hh][:]), reads=[t_pv[hh]], writes=[t_vst[b]])
                    dma("sp", lambda q: q.dma_start(out=v_d[i * 128:(i + 1) * 128, :], in_=vst[b][:]), reads=[t_vst[b]],
                        writes=[t_qkv], key=t_vst[b], join=True)
                if kind == 2:
                    wf = sb("wf", [128, 8, 16], BF16, scope=sc); t_wf = kb.tok("wf")
                    bf_bc = sb("bf_bc", [128, 16], scope=sc)
                    basef = sb("basef", [128, 16], scope=sc); t_bf = kb.tok("basef")
                    fl = [sb(f"fl{i}", [128, 64], scope=sc) for i in range(2)]; t_fl = [kb.tok("fl") for _ in range(2)]
                    flb = [sb(f"flb{i}", [128, 32], BF16, scope=sc) for i in range(2)]; t_flb = [kb.tok("flb") for _ in range(2)]
                    pf = ps("pf", [128, 16], scope=sc); t_pf = kb.tok("pf")
                    pc = ps("pc", [128, 2, 16], scope=sc); t_pc = kb.tok("pc")
                    ptr = ps("ptr", [128, 3, 128], BF16, scope=sc); t_ptr = kb.tok("ptr")
                    nsp = [sb(f"nsp{i}", [128, 3, 16], BF16, scope=sc) for i in range(2)]; t_nsp = [kb.tok("nsp") for _ in range(2)]
                    nr = [sb(f"nr{i}", [128, 48], scope=sc) for i in range(2)]
                    dma("pool", lambda q: q.dma_start(out=wf[:], in_=wsrc[:, :, 3072:3088]), writes=[t_wf], key=t_wf)
                    dma("sp", lambda q: q.dma_start(out=bf_bc[:], in_=fox_b_f[0].partition_broadcast(128)), writes=[t_wf], key=t_wf, join=True)
                    op("pool", lambda g_: g_.memset(basef[:], 0.0), writes=[t_bf])
                    for i in range(NT):
                        b = i % 2
                        F = fl[b]

                        def mmf(pe):
                            for kc in range(8):
                                r = pe.matmul(pf[:], lhsT=xT[:, kc, i * 128:(i + 1) * 128], rhs=wf[:, kc, :], start=(kc == 0), stop=(kc == 7))
                            return r
                        op("pe", mmf, reads=[t_xT, t_wf], writes=[t_pf])
                        op("dve", lambda v: v.tensor_tensor(out=F[:, 0:16], in0=pf[:], in1=bf_bc[:], op=ALU.add), reads=[t_pf, t_wf], writes=[t_fl[b]])
                        op("act", lambda a: a.activation(out=F[:, 16:32], in_=F[:, 0:16], func=AF.Exp, scale=-1.0), writes=[t_fl[b]])
                        op("act", lambda a: a.activation(out=F[:, 32:48], in_=F[:, 16:32], func=AF.Ln, bias=c_one[:], scale=1.0),
                           reads=[t_const], writes=[t_fl[b]])
                        op("dve", lambda v: v.tensor_copy(out=flb[b][:, 0:16], in_=F[:, 32:48]), reads=[t_fl[b]], writes=[t_flb[b]])
                        op("dve", lambda v: v.tensor_copy(out=F[:, 48:64], in_=flb[b][:, 0:16]), writes=[t_fl[b]])
                        op("dve", lambda v: v.tensor_tensor(out=F[:, 48:64], in0=F[:, 32:48], in1=F[:, 48:64], op=ALU.subtract), writes=[t_fl[b]])
                        op("dve", lambda v: v.tensor_copy(out=flb[b][:, 16:32], in_=F[:, 48:64]), reads=[t_fl[b]], writes=[t_flb[b]])

                        def mmc(pe):
                            pe.matmul(pc[:, 0, :], lhsT=mask_le[:], rhs=flb[b][:, 0:16], start=True, stop=False)
                            pe.matmul(pc[:, 0, :], lhsT=mask_le[:], rhs=flb[b][:, 16:32], start=False, stop=True)
                            pe.matmul(pc[:, 1, :], lhsT=ones_b[:], rhs=flb[b][:, 0:16], start=True, stop=False)
                            return pe.matmul(pc[:, 1, :], lhsT=ones_b[:], rhs=flb[b][:, 16:32], start=False, stop=True)
                        op("pe", mmc, reads=[t_flb[b], t_const], writes=[t_pc])
                        op("dve", lambda v: v.tensor_tensor(out=ncum[:, i, :], in0=pc[:, 0, :], in1=basef[:], op=ALU.add),
                           reads=[t_pc, t_bf], writes=[t_cum])
                        op("dve", lambda v: v.tensor_tensor(out=basef[:], in0=basef[:], in1=pc[:, 1, :], op=ALU.add),
                           reads=[t_pc], writes=[t_bf])
                        N3 = nsp[b]
                        R_ = nr[b]
                        op("dve", lambda v: v.tensor_copy(out=N3[:, 0, :], in_=ncum[:, i, :]), reads=[t_cum], writes=[t_nsp[b]])
                        op("dve", lambda v: v.tensor_copy(out=R_[:, 0:16], in_=N3[:, 0, :]), writes=[t_nsp[b]])
                        op("dve", lambda v: v.tensor_tensor(out=R_[:, 16:32], in0=ncum[:, i, :], in1=R_[:, 0:16], op=ALU.subtract), writes=[t_nsp[b]])
                        op("dve", lambda v: v.tensor_copy(out=N3[:, 1, :], in_=R_[:, 16:32]), writes=[t_nsp[b]])
                        op("dve", lambda v: v.tensor_copy(out=R_[:, 0:16], in_=N3[:, 1, :]), writes=[t_nsp[b]])
                        op("dve", lambda v: v.tensor_tensor(out=R_[:, 32:48], in0=R_[:, 16:32], in1=R_[:, 0:16], op=ALU.subtract), writes=[t_nsp[b]])
                        op("dve", lambda v: v.tensor_copy(out=N3[:, 2, :], in_=R_[:, 32:48]), writes=[t_nsp[b]])

                        def trn(pe):
                            for g_ in range(3):
                                r = pe.transpose(out=ptr[0:16, g_, :], in_=N3[:, g_, :], identity=identb[:])
                            return r
                        op("pe", trn, reads=[t_nsp[b], t_const], writes=[t_ptr])
                        op("dve", lambda v: v.tensor_scalar(out=refB[0:16, :, i * 128:(i + 1) * 128], in0=ptr[0:16, :, :], scalar1=-1.0,
                                                            scalar2=None, op0=ALU.mult), reads=[t_ptr], writes=[t_cum])
                kb.barrier()

        def phase_attn(kind, mT, t_mT, ncum, refB, t_cum):
            with contextlib.ExitStack() as sc:
                qs = [sb(f"qs{i}", [128, T], BF16, scope=sc) for i in range(2)]
                ks = [sb(f"ks{i}", [128, T], BF16, scope=sc) for i in range(2)]
                vs = [sb(f"vs{i}", [128, NT, 128], BF16, scope=sc) for i in range(2)]
                t_q = [kb.tok("q") for _ in range(2)]; t_k = [kb.tok("k") for _ in range(2)]; t_v = [kb.tok("v") for _ in range(2)]
                ex = [sb(f"ex{i}", [128, 512], scope=sc) for i in range(2)]; t_ex = [kb.tok("ex") for _ in range(2)]
                spb = [sb(f"spb{i}", [128, 512], BF16, scope=sc) for i in range(2)]; t_sp = [kb.tok("sp") for _ in range(2)]
                wt = [sb(f"wt{i}", [128, 512], BF16, scope=sc) for i in range(2)]; t_wt = [kb.tok("wt") for _ in range(2)]
                acc = sb("acc", [128, 512], BF16, scope=sc); t_acc = kb.tok("acc")
                rden = sb("rden", [128, 512], scope=sc); t_rden = kb.tok("rden")
                biasT = [sb(f"biasT{i}", [128, NT], scope=sc) for i in range(2)]; t_bias = [kb.tok("bias") for _ in range(2)]
                pst = [ps(f"pst{i}", [128, 512], scope=sc) for i in range(2)]; t_pst = [kb.tok("pst") for _ in range(2)]
                parg = [ps(f"parg{i}", [128, 512], scope=sc) for i in range(2)]; t_parg = [kb.tok("parg") for _ in range(2)]
                po = ps("po", [128, 512], scope=sc); t_po = kb.tok("po")
                pden = ps("pden", [128, 512], scope=sc); t_pden = kb.tok("pden")
                vsrc = v_d.rearrange("(j p) d -> p j d", p=128)

                def load_c(c):
                    b = c % 2
                    dma("sp", lambda q: q.dma_start(out=qs[b][:], in_=qT_d[c]), reads=[t_qkv], writes=[t_q[b]], key=t_q[b])
                    dma("sp", lambda q: q.dma_start(out=ks[b][:], in_=kT_d[c]), reads=[t_qkv], writes=[t_k[b]], key=t_k[b])
                    dma("sp", lambda q: q.dma_start(out=vs[b][:], in_=vsrc[:, :, c * 128:(c + 1) * 128]), reads=[t_qkv],
                        writes=[t_v[b]], key=t_v[b])
                load_c(0)
                it = 0
                ib = 0
                for c in range(8):
                    cb = c % 2
                    if c + 1 < 8:
                        load_c(c + 1)
                    for hh in range(2):
                        hd = 2 * c + hh
                        P0 = hh * 64
                        for tq in range(NQ):
                            jmax = 4 * tq + 3
                            if kind != 2:
                                op("pool", lambda g: g.memset(acc[:], 0.0), writes=[t_acc])
                            op("pe", lambda pe: pe.matmul(po[:], lhsT=zeros_b[:], rhs=qs[cb][:, 0:512], start=True, stop=False),
                               reads=[t_const, t_q[cb]], writes=[t_po])
                            if kind == 2:
                                op("pe", lambda pe: pe.matmul(pden[:], lhsT=zeros_b[:], rhs=qs[cb][:, 0:512], start=True, stop=False),
                                   reads=[t_const, t_q[cb]], writes=[t_pden])
                            order = range(jmax + 1) if kind == 2 else range(jmax, -1, -1)
                            for nj, j in enumerate(order):
                                b = it % 2
                                it += 1
                                last = (nj == jmax)
                                col0 = max(0, j - 4 * tq) * 128
                                diag = j >= 4 * tq
                                q0 = tq * 512 + col0

                                def mms(pe):
                                    r = pe.matmul(pst[b][:, col0:512], lhsT=ks[cb][P0:P0 + 64, j * 128:(j + 1) * 128],
                                                  rhs=qs[cb][P0:P0 + 64, q0:(tq + 1) * 512], start=True, stop=(kind != 2))
                                    if kind == 2:
                                        for g_ in range(3):
                                            r = pe.matmul(pst[b][:, col0:512], lhsT=selh[0:16, hd, :], rhs=refB[0:16, g_, q0:(tq + 1) * 512],
                                                          start=False, stop=(g_ == 2 and not diag))
                                        if diag:
                                            r = pe.matmul(pst[b][:, col0:col0 + 128], lhsT=identb[:], rhs=negmask_gt[:], start=False, stop=True)
                                    return r
                                op("pe", mms, reads=[t_q[cb], t_k[cb], t_cum, t_const], writes=[t_pst[b]])
                                if kind == 2:
                                    op("act", lambda a: a.activation(out=wt[b][:, col0:512], in_=pst[b][:, col0:512], func=AF.Exp,
                                                                     bias=ncum[:, j, hd:hd + 1], scale=1.0),
                                       reads=[t_pst[b], t_cum], writes=[t_wt[b]])

                                    def mmo(pe):
                                        pe.matmul(po[:, col0:512], lhsT=vs[cb][:, j, :], rhs=wt[b][:, col0:512], start=False, stop=last)
                                        return pe.matmul(pden[:, col0:512], lhsT=ones_b[:], rhs=wt[b][:, col0:512], start=False, stop=last)
                                    op("pe", mmo, reads=[t_wt[b], t_v[cb], t_const], writes=[t_po, t_pden])
                                else:
                                    op("act", lambda a: a.activation(out=ex[b][:, col0:512], in_=pst[b][:, col0:512], func=AF.Exp),
                                       reads=[t_pst[b]], writes=[t_ex[b]])
                                    op("act", lambda a: a.activation(out=spb[b][:, col0:512], in_=ex[b][:, col0:512], func=AF.Ln,
                                                                     bias=c_one[:], scale=1.0), reads=[t_ex[b], t_const], writes=[t_sp[b]])
                                    if diag:
                                        op("pool", lambda g: g.tensor_tensor(out=spb[b][:, col0:col0 + 128], in0=spb[b][:, col0:col0 + 128],
                                                                             in1=mask_lt[:], op=ALU.mult), reads=[t_const], writes=[t_sp[b]])

                                    def mma(pe):
                                        pe.matmul(parg[b][:, col0:512], lhsT=ks[cb][P0:P0 + 64, j * 128:(j + 1) * 128],
                                                  rhs=qs[cb][P0:P0 + 64, q0:(tq + 1) * 512], start=True, stop=False)
                                        pe.matmul(parg[b][:, col0:512], lhsT=ntri_ge[:], rhs=spb[b][:, col0:512], start=False, stop=False)
                                        return pe.matmul(parg[b][:, col0:512], lhsT=nones_b[:], rhs=acc[:, col0:512], start=False, stop=True)
                                    op("pe", mma, reads=[t_q[cb], t_k[cb], t_sp[b], t_acc, t_const], writes=[t_parg[b]])
                                    op("act", lambda a: a.activation(out=wt[b][:, col0:512], in_=parg[b][:, col0:512], func=AF.Exp),
                                       reads=[t_parg[b]], writes=[t_wt[b]])
                                    if diag:
                                        op("pool", lambda g: g.tensor_tensor(out=wt[b][:, col0:col0 + 128], in0=wt[b][:, col0:col0 + 128],
                                                                             in1=mask_lt[:], op=ALU.mult), reads=[t_const], writes=[t_wt[b]])
                                    op("pool", lambda g: g.tensor_tensor(out=acc[:, col0:512], in0=acc[:, col0:512], in1=spb[b][:, col0:512],
                                                                         op=ALU.add), reads=[t_sp[b]], writes=[t_acc])
                                    op("pe", lambda pe: pe.matmul(po[:, col0:512], lhsT=vs[cb][:, j, :], rhs=wt[b][:, col0:512],
                                                                  start=False, stop=last), reads=[t_wt[b], t_v[cb]], writes=[t_po])
                            osl = mT[P0:P0 + 64, c, tq * 512:(tq + 1) * 512]
                            if kind == 2:
                                op("dve", lambda v: v.reciprocal(out=rden[P0:P0 + 64, :], in_=pden[P0:P0 + 64, :]), reads=[t_pden], writes=[t_rden])
                                op("dve", lambda v: v.tensor_tensor(out=osl, in0=po[P0:P0 + 64, :], in1=rden[P0:P0 + 64, :], op=ALU.mult),
                                   reads=[t_po, t_rden], writes=[t_mT])
                            else:
                                op("dve", lambda v: v.tensor_copy(out=osl, in_=po[P0:P0 + 64, :]), reads=[t_po], writes=[t_mT])
                kb.barrier()

        def phase_outproj_ln1_route(li, w_out_d, xin, t_xin, mT, t_mT, gates, dests, t_route):
            with contextlib.ExitStack() as sc:
                wo = sb("wo", [128, 8, D], BF16, scope=sc); t_wo = kb.tok("wo")
                g_bc = sb("g_bc", [128, D], scope=sc); b_bc = sb("b_bc", [128, D], scope=sc); t_gb = kb.tok("gb")
                wr32 = sb("wr32", [128, 8, NE], scope=sc)
                wrh = sb("wrh", [128, 8, NE], BF16, scope=sc)
                wrl = sb("wrl", [128, 8, NE], BF16, scope=sc)
                wrt = sb("wrt", [128, 8, NE], scope=sc)
                rb_bc = sb("rb_bc", [128, NE], scope=sc)
                t_wr = kb.tok("wr")
                base = sb("base", [128, NE], scope=sc); t_base = kb.tok("base")
                xs = [sb(f"xs{i}", [128, D], scope=sc) for i in range(2)]; t_xs = [kb.tok("xs") for _ in range(2)]
                h = [sb(f"h{i}", [128, D], scope=sc) for i in range(2)]; t_h = [kb.tok("h") for _ in range(2)]
                x1 = [sb(f"x1_{i}", [128, D], scope=sc) for i in range(2)]; t_x1 = [kb.tok("x1") for _ in range(2)]
                x1h = [sb(f"x1h{i}", [128, D], BF16, scope=sc) for i in range(2)]; t_x1h = [kb.tok("x1h") for _ in range(2)]
                x1l = [sb(f"x1l{i}", [128, D], BF16, scope=sc) for i in range(2)]; t_x1l = [kb.tok("x1l") for _ in range(2)]
                x1t = [sb(f"x1t{i}", [128, D], scope=sc) for i in range(2)]
                xTh = [sb(f"xTh{i}", [128, 8, 128], BF16, scope=sc) for i in range(2)]; t_xTh = [kb.tok("xTh") for _ in range(2)]
                xTl = [sb(f"xTl{i}", [128, 8, 128], BF16, scope=sc) for i in range(2)]; t_xTl = [kb.tok("xTl") for _ in range(2)]
                stat = [sb(f"stat{i}", [128, 16], scope=sc) for i in range(2)]; t_stat = [kb.tok("stat") for _ in range(2)]
                rt = [sb(f"rt{i}", [128, 8 * NE], scope=sc) for i in range(2)]; t_rt = [kb.tok("rt") for _ in range(2)]
                mkb = [sb(f"mkb{i}", [128, NE], BF16, scope=sc) for i in range(2)]; t_mkb = [kb.tok("mkb") for _ in range(2)]
                sm = [sb(f"sm{i}", [128, 32], scope=sc) for i in range(2)]
                desti = sb("desti", [128, NT, 4], I32, scope=sc)
                py = [ps(f"py{i}", [128, D], scope=sc) for i in range(2)]; t_py = [kb.tok("py") for _ in range(2)]
                pTh = ps("pTh", [128, 8, 128], BF16, scope=sc); t_pTh = kb.tok("pTh")
                pTl = ps("pTl", [128, 8, 128], BF16, scope=sc); t_pTl = kb.tok("pTl")
                plog = ps("plog", [128, NE], scope=sc); t_plog = kb.tok("plog")
                pcnt = ps("pcnt", [128, 2, NE], scope=sc); t_pcnt = kb.tok("pcnt")

                dma("pool", lambda q: q.dma_start(out=wo[:], in_=w_out_d.rearrange("(kc p) n -> p kc n", p=128)),
                    writes=[t_wo], key=t_wo)
                dma("sp", lambda q: q.dma_start(out=g_bc[:], in_=ln1_g[li].partition_broadcast(128)), writes=[t_gb], key=t_gb)
                dma("sp", lambda q: q.dma_start(out=b_bc[:], in_=ln1_b[li].partition_broadcast(128)), writes=[t_gb], key=t_gb, join=True)
                dma("sp", lambda q: q.dma_start(out=wr32[:], in_=router_w[li].rearrange("(kc p) n -> p kc n", p=128)),
                    writes=[t_wr], key=t_wr)
                dma("sp", lambda q: q.dma_start(out=rb_bc[:], in_=router_b[li].partition_broadcast(128)), writes=[t_wr], key=t_wr, join=True)
                op("dve", lambda v: v.tensor_copy(out=wrh[:], in_=wr32[:]), reads=[t_wr], writes=[t_wr])
                op("dve", lambda v: v.tensor_copy(out=wrt[:], in_=wrh[:]), writes=[t_wr])
                op("dve", lambda v: v.tensor_tensor(out=wrt[:], in0=wr32[:], in1=wrt[:], op=ALU.subtract), writes=[t_wr])
                op("dve", lambda v: v.tensor_copy(out=wrl[:], in_=wrt[:]), writes=[t_wr])
                op("pool", lambda g: g.memset(base[:], 0.0), writes=[t_base])
                dma("sp", lambda q: q.dma_start(out=toklist.rearrange("(p r) o -> p (r o)", p=128), in_=zi[:]),
                    reads=[t_const], writes=[t_tl], key=t_tl)

                for i in range(NT):
                    b = i % 2
                    dma("sp", lambda q: q.dma_start(out=xs[b][:], in_=xin[i * 128:(i + 1) * 128, :]),
                        reads=[t_xin], writes=[t_xs[b]], key=t_xs[b])

                    def mm(pe):
                        for hh in range(2):
                            for c in range(8):
                                r = pe.matmul(py[b][:, hh * 512:(hh + 1) * 512], lhsT=mT[:, c, i * 128:(i + 1) * 128],
                                              rhs=wo[:, c, hh * 512:(hh + 1) * 512], start=(c == 0), stop=(c == 7))
                        return r
                    op("pe", mm, reads=[t_mT, t_wo], writes=[t_py[b]])
                    op("dve", lambda v: v.scalar_tensor_tensor(out=h[b][:], in0=xs[b][:], scalar=ALPHA, in1=py[b][:],
                                                               op0=ALU.mult, op1=ALU.add),
                       reads=[t_xs[b], t_py[b]], writes=[t_h[b]])
                    layer_norm_tile(h[b], g_bc, b_bc, x1[b], stat[b], t_h[b], t_x1[b], t_stat[b], t_gb)
                    dma("sp", lambda q: q.dma_start(out=x1res[i * 128:(i + 1) * 128, :], in_=x1[b][:]),
                        reads=[t_x1[b]], writes=[t_x1res], key=t_x1[b], join=True)
                    op("act", lambda a: a.copy(out=x1h[b][:], in_=x1[b][:]), reads=[t_x1[b]], writes=[t_x1h[b]])
                    dma("sp", lambda q: q.dma_start(out=x1bf[i * 128:(i + 1) * 128, :], in_=x1h[b][:]),
                        reads=[t_x1h[b]], writes=[t_x1bf], key=t_x1h[b], join=True)
                    op("pool", lambda g: g.tensor_copy(out=x1t[b][:], in_=x1h[b][:]), reads=[t_x1h[b]], writes=[t_x1l[b]])
                    op("pool", lambda g: g.tensor_tensor(out=x1t[b][:], in0=x1[b][:], in1=x1t[b][:], op=ALU.subtract),
                       reads=[t_x1[b]], writes=[t_x1l[b]])
                    op("pool", lambda g: g.tensor_copy(out=x1l[b][:], in_=x1t[b][:]), writes=[t_x1l[b]])
                    for (src, tsrc, pp, tpp, dst, tdst) in ((x1h[b], t_x1h[b], pTh, t_pTh, xTh[b], t_xTh[b]),
                                                            (x1l[b], t_x1l[b], pTl, t_pTl, xTl[b], t_xTl[b])):
                        def tr(pe):
                            for c in range(8):
                                r = pe.transpose(out=pp[:, c, :], in_=src[:, c * 128:(c + 1) * 128], identity=identb[:])
                            return r
                        op("pe", tr, reads=[tsrc, t_const], writes=[tpp])
                        op("act", lambda a: a.copy(out=dst[:], in_=pp[:]), reads=[tpp], writes=[tdst])

                    def mmr(pe):
                        n = 0
                        for (xa, wa) in ((xTh[b], wrh), (xTh[b], wrl), (xTl[b], wrh)):
                            for c in range(8):
                                r = pe.matmul(plog[:], lhsT=xa[:, c, :], rhs=wa[:, c, :], start=(n == 0), stop=(n == 23))
                                n += 1
                        return r
                    op("pe", mmr, reads=[t_xTh[b], t_xTl[b], t_wr], writes=[t_plog])
                    R = rt[b]
                    L = R[:, 0:32]; MK = R[:, 32:64]; EX = R[:, 64:96]; G = R[:, 96:128]
                    POS = R[:, 128:160]; TMP = R[:, 160:192]; OH = R[:, 192:224]; TMP2 = R[:, 224:256]
                    S = sm[b]
                    top8 = S[:, 0:8]; nmax = S[:, 8:9]; den = S[:, 9:10]; rden = S[:, 10:11]
                    dstf = S[:, 12:16]
                    tr_ = t_rt[b]
                    op("dve", lambda v: v.tensor_tensor(out=L, in0=plog[:], in1=rb_bc[:], op=ALU.add),
                       reads=[t_plog, t_wr], writes=[tr_])
                    op("dve", lambda v: v.max(out=top8, in_=L), writes=[tr_])
                    op("dve", lambda v: v.tensor_scalar(out=MK, in0=L, scalar1=S[:, 3:4], scalar2=None, op0=ALU.is_ge), writes=[tr_])
                    op("dve", lambda v: v.tensor_scalar(out=nmax, in0=S[:, 0:1], scalar1=-1.0, scalar2=None, op0=ALU.mult), writes=[tr_])
                    op("act", lambda a: a.activation(out=EX, in_=L, func=AF.Exp, bias=nmax, scale=1.0), writes=[tr_])
                    op("dve", lambda v: v.tensor_tensor(out=EX, in0=EX, in1=MK, op=ALU.mult), writes=[tr_])
                    op("dve", lambda v: v.reduce_sum(out=den, in_=EX, axis=AX.X), writes=[tr_])
                    op("dve", lambda v: v.reciprocal(out=rden, in_=den), writes=[tr_])
                    op("dve", lambda v: v.tensor_scalar(out=G, in0=EX, scalar1=rden, scalar2=None, op0=ALU.mult), writes=[tr_])
                    op("dve", lambda v: v.tensor_copy(out=mkb[b][:], in_=MK), reads=[tr_], writes=[t_mkb[b]])

                    def mmc(pe):
                        pe.matmul(pcnt[:, 0, :], lhsT=tri_lt[:], rhs=mkb[b][:], start=True, stop=True)
                        return pe.matmul(pcnt[:, 1, :], lhsT=ones_b[:], rhs=mkb[b][:], start=True, stop=True)
                    op("pe", mmc, reads=[t_mkb[b], t_const], writes=[t_pcnt])
                    op("dve", lambda v: v.tensor_tensor(out=POS, in0=pcnt[:, 0, :], in1=base[:], op=ALU.add),
                       reads=[t_pcnt, t_base], writes=[tr_])
                    op("dve", lambda v: v.tensor_tensor(out=base[:], in0=base[:], in1=pcnt[:, 1, :], op=ALU.add),
                       reads=[t_pcnt], writes=[t_base])
                    op("dve", lambda v: v.tensor_scalar(out=TMP, in0=POS, scalar1=float(C), scalar2=None, op0=ALU.is_lt), writes=[tr_])
                    op("dve", lambda v: v.tensor_tensor(out=POS, in0=POS, in1=ecol[:], op=ALU.add), reads=[t_const], writes=[tr_])
                    op("dve", lambda v: v.tensor_scalar(out=POS, in0=POS, scalar1=float(-NSLOT), scalar2=None, op0=ALU.add), writes=[tr_])
                    op("dve", lambda v: v.tensor_tensor(out=POS, in0=POS, in1=TMP, op=ALU.mult), writes=[tr_])
                    op("dve", lambda v: v.tensor_scalar(out=POS, in0=POS, scalar1=float(NSLOT), scalar2=None, op0=ALU.add), writes=[tr_])
                    for k in range(4):
                        op("dve", lambda v: v.tensor_scalar(out=OH, in0=L, scalar1=S[:, k:k + 1], scalar2=None, op0=ALU.is_equal), writes=[tr_])
                        op("dve", lambda v: v.tensor_tensor(out=TMP2, in0=OH, in1=POS, op=ALU.mult), writes=[tr_])
                        op("dve", lambda v: v.reduce_sum(out=dstf[:, k:k + 1], in_=TMP2, axis=AX.X), writes=[tr_])
                        op("dve", lambda v: v.tensor_tensor(out=TMP2, in0=OH, in1=G, op=ALU.mult), writes=[tr_])
                        op("dve", lambda v: v.reduce_sum(out=gates[:, i, k:k + 1], in_=TMP2, axis=AX.X), writes=[tr_, t_route])
                    op("dve", lambda v: v.tensor_copy(out=dests[:, i, :], in_=dstf), reads=[tr_], writes=[t_route])
                    for k in range(4):
                        dma("pool", lambda q: q.indirect_dma_start(
                            out=toklist[:, :], out_offset=bass.IndirectOffsetOnAxis(ap=dests[:, i, k:k + 1], axis=0),
                            in_=tokid[:, i:i + 1], in_offset=None), reads=[t_route, t_const], writes=[t_tl], key=t_tl, join=True)
                kb.barrier()

        def phase_experts(li):
            with contextlib.ExitStack() as sc:
                wg = [sb(f"wg{i}", [128, 8, D], BF16, scope=sc) for i in range(2)]
                wu = [sb(f"wu{i}", [128, 8, D], BF16, scope=sc) for i in range(2)]
                wd = [sb(f"wd{i}", [128, 8, D], BF16, scope=sc) for i in range(2)]
                t_wg = [kb.tok("wg") for _ in range(2)]; t_wu = [kb.tok("wu") for _ in range(2)]; t_wd = [kb.tok("wd") for _ in range(2)]
                bd = [sb(f"bd{i}", [128, D], scope=sc) for i in range(2)]; t_bd = [kb.tok("bd") for _ in range(2)]
                braw = sb("braw", [NE, 2, D], scope=sc)
                brb = sb("brb", [NE, 2, D], BF16, scope=sc)
                bT = sb("bT", [128, 2, 8, NE], scope=sc)
                t_b = kb.tok("bias")
                idx = [sb(f"idx{i}", [128, CR], I32, scope=sc) for i in range(2)]; t_idx = [kb.tok("idx") for _ in range(2)]
                xg = [sb(f"xg{i}", [128, CR, D], BF16, scope=sc) for i in range(2)]; t_xg = [kb.tok("xg") for _ in range(2)]
                xeT = sb("xeT", [128, 8, C], BF16, scope=sc); t_xeT = kb.tok("xeT")
                aT = sb("aT", [128, 8, C], BF16, scope=sc); t_aT = kb.tok("aT")
                gt = [sb(f"gt{i}", [128, C], scope=sc) for i in range(2)]; t_gt = [kb.tok("gt") for _ in range(2)]
                sg = [sb(f"sg{i}", [128, C], scope=sc) for i in range(2)]; t_sg = [kb.tok("sg") for _ in range(2)]
                ut = [sb(f"ut{i}", [128, C], scope=sc) for i in range(2)]; t_ut = [kb.tok("ut") for _ in range(2)]
                yo = [sb(f"yo{i}", [128, D], scope=sc) for i in range(2)]; t_yo = [kb.tok("yo") for _ in range(2)]
                pT = ps("pT", [128, 8, 128], BF16, scope=sc); t_pT = kb.tok("pT")
                pg = ps("pg", [128, 1024], scope=sc); t_pg = kb.tok("pg")
                pu = ps("pu", [128, 1024], scope=sc); t_pu = kb.tok("pu")
                py = ps("py", [128, 1024], scope=sc); t_py = kb.tok("py")
                pbt = ps("pbt", [128, 8, NE], BF16, scope=sc); t_pbt = kb.tok("pbt")

                dma("sp", lambda q: q.dma_start(out=braw[:, 0, :], in_=exp_b_gate[li]), writes=[t_b], key=t_b)
                dma("sp", lambda q: q.dma_start(out=braw[:, 1, :], in_=exp_b_up[li]), writes=[t_b], key=t_b, join=True)
                op("dve", lambda v: v.tensor_copy(out=brb[:], in_=braw[:]), reads=[t_b], writes=[t_b])
                for g in range(2):
                    def tr(pe):
                        for c in range(8):
                            r = pe.transpose(out=pbt[:, c, :], in_=brb[:, g, c * 128:(c + 1) * 128], identity=identb[0:NE, 0:NE])
                        return r
                    op("pe", tr, reads=[t_b, t_const], writes=[t_pbt])
                    op("dve", lambda v: v.tensor_copy(out=bT[:, g, :, :], in_=pbt[:]), reads=[t_pbt], writes=[t_b])

                def load_w(e):
                    b = e % 2
                    for (dst, td, src) in ((wg[b], t_wg[b], exp_w_gate), (wu[b], t_wu[b], exp_w_up), (wd[b], t_wd[b], exp_w_down)):
                        dma("pool", lambda q: q.dma_start(out=dst[:], in_=src[li, e].rearrange("(kc p) n -> p kc n", p=128)),
                            writes=[td], key=td)
                    dma("sp", lambda q: q.dma_start(out=bd[b][:], in_=exp_b_down[li, e].partition_broadcast(128)),
                        writes=[t_bd[b]], key=t_bd[b])

                def load_x(e):
                    b = e % 2
                    dma("sp", lambda q: q.dma_start(out=idx[b][:], in_=toklist[e * C:(e + 1) * C, :].rearrange("(p r) o -> p (r o)", p=128)),
                        reads=[t_tl], writes=[t_idx[b]], key=t_idx[b])
                    for r in range(CR):
                        dma("pool", lambda q: q.indirect_dma_start(
                            out=xg[b][:, r, :], out_offset=None, in_=x1bf[:, :],
                            in_offset=bass.IndirectOffsetOnAxis(ap=idx[b][:, r:r + 1], axis=0)),
                            reads=[t_idx[b], t_x1bf], writes=[t_xg[b]], key=t_xg[b], join=(r > 0))

                load_x(0)
                load_w(0)
                it = 0
                for e in range(NE):
                    b = e % 2
                    if e + 1 < NE:
                        load_x(e + 1)
                        load_w(e + 1)
                    for r in range(CR):
                        def tr(pe):
                            for c in range(8):
                                rr = pe.transpose(out=pT[:, c, :], in_=xg[b][:, r, c * 128:(c + 1) * 128], identity=identb[:])
                            return rr
                        op("pe", tr, reads=[t_xg[b], t_const], writes=[t_pT])
                        op("act", lambda a: a.copy(out=xeT[:, :, r * 128:(r + 1) * 128], in_=pT[:]), reads=[t_pT], writes=[t_xeT])
                    segs = [(s0, min(512, C - s0)) for s0 in range(0, C, 512)]
                    for fc in range(8):
                        bb = it % 2
                        it += 1
                        for (pp, tp, w_, tw) in ((pg, t_pg, wg[b], t_wg[b]), (pu, t_pu, wu[b], t_wu[b])):
                            def mm(pe):
                                for (s0, sn) in segs:
                                    for kc in range(8):
                                        rr = pe.matmul(pp[:, s0:s0 + sn], lhsT=w_[:, kc, fc * 128:(fc + 1) * 128],
                                                       rhs=xeT[:, kc, s0:s0 + sn], start=(kc == 0), stop=(kc == 7))
                                return rr
                            op("pe", mm, reads=[tw, t_xeT], writes=[tp])
                        op("dve", lambda v: v.tensor_scalar(out=gt[bb][:], in0=pg[:, 0:C], scalar1=bT[:, 0, fc, e:e + 1], scalar2=7.0,
                                                            op0=ALU.add, op1=ALU.min), reads=[t_pg, t_b], writes=[t_gt[bb]])
                        op("act", lambda a: a.activation(out=sg[bb][:], in_=gt[bb][:], func=AF.Sigmoid, scale=1.702),
                           reads=[t_gt[bb]], writes=[t_sg[bb]])
                        op("dve", lambda v: v.tensor_scalar(out=ut[bb][:], in0=pu[:, 0:C], scalar1=bT[:, 1, fc, e:e + 1], scalar2=7.0,
                                                            op0=ALU.add, op1=ALU.min), reads=[t_pu, t_b], writes=[t_ut[bb]])
                        op("pool", lambda g: g.tensor_scalar(out=ut[bb][:], in0=ut[bb][:], scalar1=-7.0, scalar2=1.0,
                                                             op0=ALU.max, op1=ALU.add), writes=[t_ut[bb]])
                        op("pool", lambda g: g.tensor_tensor(out=gt[bb][:], in0=gt[bb][:], in1=sg[bb][:], op=ALU.mult),
                           reads=[t_sg[bb]], writes=[t_gt[bb]])
                        op("pool", lambda g: g.tensor_tensor(out=aT[:, fc, :], in0=gt[bb][:], in1=ut[bb][:], op=ALU.mult),
                           reads=[t_gt[bb], t_ut[bb]], writes=[t_aT])
                    for r in range(CR):
                        yb = (e * CR + r) % 2

                        def mmd(pe):
                            for hh in range(2):
                                for fc in range(8):
                                    rr = pe.matmul(py[:, hh * 512:(hh + 1) * 512], lhsT=aT[:, fc, r * 128:(r + 1) * 128],
                                                   rhs=wd[b][:, fc, hh * 512:(hh + 1) * 512], start=(fc == 0), stop=(fc == 7))
                            return rr
                        op("pe", mmd, reads=[t_aT, t_wd[b]], writes=[t_py])
                        op("dve", lambda v: v.tensor_tensor(out=yo[yb][:], in0=py[:], in1=bd[b][:], op=ALU.add),
                           reads=[t_py, t_bd[b]], writes=[t_yo[yb]])
                        ydst = yslots[e * C:(e + 1) * C, :].rearrange("(p r) d -> p r d", p=128)
                        dma("sp", lambda q: q.dma_start(out=ydst[:, r, :], in_=yo[yb][:]),
                            reads=[t_yo[yb]], writes=[t_ys], key=t_yo[yb], join=True)
                kb.barrier()

        def phase_combine_ln2_ple(li, gates, dests, t_route, xout, t_xout):
            with contextlib.ExitStack() as sc:
                wpg = sb("wpg", [128, 8, D], BF16, scope=sc); wpp = sb("wpp", [128, 2, D], BF16, scope=sc); t_w = kb.tok("wple")
                g_bc = sb("g_bc", [128, D], scope=sc); b_bc = sb("b_bc", [128, D], scope=sc)
                bpg = sb("bpg", [128, D], scope=sc); t_gb = kb.tok("gb")
                yk = [[sb(f"yk{i}_{k}", [128, D], scope=sc) for k in range(4)] for i in range(2)]
                t_yk = [[kb.tok("yk") for k in range(4)] for i in range(2)]
                xs = [sb(f"xs{i}", [128, D], scope=sc) for i in range(2)]; t_xs = [kb.tok("xs") for _ in range(2)]
                h = [sb(f"h{i}", [128, D], scope=sc) for i in range(2)]; t_h = [kb.tok("h") for _ in range(2)]
                x2 = [sb(f"x2_{i}", [128, D], scope=sc) for i in range(2)]; t_x2 = [kb.tok("x2") for _ in range(2)]
                x2b = [sb(f"x2b{i}", [128, D], BF16, scope=sc) for i in range(2)]; t_x2b = [kb.tok("x2b") for _ in range(2)]
                x2T = [sb(f"x2T{i}", [128, 8, 128], BF16, scope=sc) for i in range(2)]; t_x2T = [kb.tok("x2T") for _ in range(2)]
                pp32 = [sb(f"pp32_{i}", [128, PLE], scope=sc) for i in range(2)]; t_pp = [kb.tok("pp") for _ in range(2)]
                ppb = [sb(f"ppb{i}", [128, PLE], BF16, scope=sc) for i in range(2)]; t_ppb = [kb.tok("ppb") for _ in range(2)]
                ppT = [sb(f"ppT{i}", [128, 2, 128], BF16, scope=sc) for i in range(2)]; t_ppT = [kb.tok("ppT") for _ in range(2)]
                stat = [sb(f"stat{i}", [128, 16], scope=sc) for i in range(2)]; t_stat = [kb.tok("stat") for _ in range(2)]
                gs = [sb(f"gs{i}", [128, D], scope=sc) for i in range(2)]; t_gs = [kb.tok("gs") for _ in range(2)]
                xo = [sb(f"xo{i}", [128, D], scope=sc) for i in range(2)]; t_xo = [kb.tok("xo") for _ in range(2)]
                pT = ps("pT", [128, 8, 128], BF16, scope=sc); t_pT = kb.tok("pT")
                pT2 = ps("pT2", [128, 2, 128], BF16, scope=sc); t_pT2 = kb.tok("pT2")
                pgt = ps("pgt", [128, D], scope=sc); t_pgt = kb.tok("pgt")
                ppj = ps("ppj", [128, D], scope=sc); t_ppj = kb.tok("ppj")
                dma("pool", lambda q: q.dma_start(out=wpg[:], in_=ple_w_gate[li].rearrange("(kc p) n -> p kc n", p=128)), writes=[t_w], key=t_w)
                dma("pool", lambda q: q.dma_start(out=wpp[:], in_=ple_w_proj[li].rearrange("(kc p) n -> p kc n", p=128)), writes=[t_w], key=t_w, join=True)
                dma("sp", lambda q: q.dma_start(out=g_bc[:], in_=ln2_g[li].partition_broadcast(128)), writes=[t_gb], key=t_gb)
                dma("sp", lambda q: q.dma_start(out=b_bc[:], in_=ln2_b[li].partition_broadcast(128)), writes=[t_gb], key=t_gb, join=True)
                dma("sp", lambda q: q.dma_start(out=bpg[:], in_=ple_b_gate[li].partition_broadcast(128)), writes=[t_gb], key=t_gb, join=True)
                for i in range(NT):
                    b = i % 2
                    dma("sp", lambda q: q.dma_start(out=xs[b][:], in_=x1res[i * 128:(i + 1) * 128, :]),
                        reads=[t_x1res], writes=[t_xs[b]], key=t_xs[b])
                    dma("sp", lambda q: q.dma_start(out=pp32[b][:], in_=p_in[li, i * 128:(i + 1) * 128, :]),
                        writes=[t_pp[b]], key=t_pp[b])
                    for k in range(4):
                        dma("pool", lambda q: q.indirect_dma_start(
                            out=yk[b][k][:], out_offset=None, in_=yslots[:, :],
                            in_offset=bass.IndirectOffsetOnAxis(ap=dests[:, i, k:k + 1], axis=0)),
                            reads=[t_route, t_ys], writes=[t_yk[b][k]], key=t_yk[b][k])
                    op("act", lambda a: a.mul(out=h[b][:], in_=xs[b][:], mul=ALPHA), reads=[t_xs[b]], writes=[t_h[b]])
                    for k in range(4):
                        op("dve", lambda v: v.scalar_tensor_tensor(out=h[b][:], in0=yk[b][k][:], scalar=gates[:, i, k:k + 1],
                                                                   in1=h[b][:], op0=ALU.mult, op1=ALU.add),
                           reads=[t_yk[b][k], t_route], writes=[t_h[b]])
                    layer_norm_tile(h[b], g_bc, b_bc, x2[b], stat[b], t_h[b], t_x2[b], t_stat[b], t_gb)
                    op("act", lambda a: a.copy(out=x2b[b][:], in_=x2[b][:]), reads=[t_x2[b]], writes=[t_x2b[b]])
                    op("act", lambda a: a.copy(out=ppb[b][:], in_=pp32[b][:]), reads=[t_pp[b]], writes=[t_ppb[b]])

                    def tr(pe):
                        for c in range(8):
                            r = pe.transpose(out=pT[:, c, :], in_=x2b[b][:, c * 128:(c + 1) * 128], identity=identb[:])
                        return r
                    op("pe", tr, reads=[t_x2b[b], t_const], writes=[t_pT])
                    op("act", lambda a: a.copy(out=x2T[b][:], in_=pT[:]), reads=[t_pT], writes=[t_x2T[b]])

                    def tr2(pe):
                        for c in range(2):
                            r = pe.transpose(out=pT2[:, c, :], in_=ppb[b][:, c * 128:(c + 1) * 128], identity=identb[:])
                        return r
                    op("pe", tr2, reads=[t_ppb[b], t_const], writes=[t_pT2])
                    op("act", lambda a: a.copy(out=ppT[b][:], in_=pT2[:]), reads=[t_pT2], writes=[t_ppT[b]])

                    def mmg(pe):
                        for hh in range(2):
                            for c in range(8):
                                r = pe.matmul(pgt[:, hh * 512:(hh + 1) * 512], lhsT=x2T[b][:, c, :], rhs=wpg[:, c, hh * 512:(hh + 1) * 512],
                                              start=(c == 0), stop=(c == 7))
                        return r
                    op("pe", mmg, reads=[t_x2T[b], t_w], writes=[t_pgt])

                    def mmp(pe):
                        for hh in range(2):
                            for c in range(2):
                                r = pe.matmul(ppj[:, hh * 512:(hh + 1) * 512], lhsT=ppT[b][:, c, :], rhs=wpp[:, c, hh * 512:(hh + 1) * 512],
                                              start=(c == 0), stop=(c == 1))
                        return r
                    op("pe", mmp, reads=[t_ppT[b], t_w], writes=[t_ppj])
                    op("dve", lambda v: v.tensor_tensor(out=gs[b][:], in0=pgt[:], in1=bpg[:], op=ALU.add),
                       reads=[t_pgt, t_gb], writes=[t_gs[b]])
                    op("act", lambda a: a.activation(out=gs[b][:], in_=gs[b][:], func=AF.Sigmoid), writes=[t_gs[b]])
                    op("dve", lambda v: v.tensor_tensor(out=xo[b][:], in0=ppj[:], in1=gs[b][:], op=ALU.mult),
                       reads=[t_ppj, t_gs[b]], writes=[t_xo[b]])
                    op("pool", lambda g: g.tensor_tensor(out=xo[b][:], in0=xo[b][:], in1=x2[b][:], op=ALU.add),
                       reads=[t_x2[b]], writes=[t_xo[b]])
                    dma("sp", lambda q: q.dma_start(out=xout[i * 128:(i + 1) * 128, :], in_=xo[b][:]),
                        reads=[t_xo[b]], writes=[t_xout], key=t_xo[b], join=True)
                kb.barrier()

        gates = sb("gates", [128, NT, 4])
        dests = sb("dests", [128, NT, 4], I32)
        t_route = kb.tok("route")
        xin, t_xin = x_in, kb.tok("xin")
        nl = len(layers)
        for n, li in enumerate(layers):
            kind, j = KINDS[li], JIDX[li]
            last = (n == nl - 1)
            xout, t_xout = (out_d, t_out) if last else (xres[n % 2], t_xres[n % 2])
            with contextlib.ExitStack() as lsc:
                t_mT = kb.tok("mT")
                if kind == 0:
                    mT = sb("mT", [128, 8, T], BF16, scope=lsc)
                    with contextlib.ExitStack() as asc:
                        xT = sb("xT", [128, 8, T], BF16, scope=asc); t_xT = kb.tok("xT")
                        phase_xT(xin, t_xin, xT, t_xT)
                        phase_conv(j, xT, t_xT, mT, t_mT)
                    w_out_d = conv_w_out[j]
                else:
                    ncum = sb("ncum", [128, NT, 16], scope=lsc)
                    refB = sb("refB", [16, 3, T], BF16, scope=lsc)
                    t_cum = kb.tok("cum")
                    w_in_d = sb_w_in[0] if kind == 1 else fox_w_in[0]
                    with contextlib.ExitStack() as asc:
                        xT = sb("xT", [128, 8, T], BF16, scope=asc); t_xT = kb.tok("xT")
                        phase_xT(xin, t_xin, xT, t_xT)
                        phase_qkv(kind, w_in_d, xT, t_xT, ncum, refB, t_cum)
                    mT = sb("mT", [128, 8, T], BF16, scope=lsc)
                    phase_attn(kind, mT, t_mT, ncum, refB, t_cum)
                    w_out_d = sb_w_out[0] if kind == 1 else fox_w_out[0]
                phase_outproj_ln1_route(li, w_out_d, xin, t_xin, mT, t_mT, gates, dests, t_route)
            phase_experts(li)
            phase_combine_ln2_ple(li, gates, dests, t_route, xout, t_xout)
            xin, t_xin = xout, t_xout
        kb.barrier()
    return nc


_NAMES = ["x", "p", "conv_w_in", "conv_w", "conv_w_out", "sb_w_in", "sb_w_out", "fox_w_in", "fox_b_f", "fox_w_out",
          "ln1_g", "ln1_b", "ln2_g", "ln2_b", "router_w", "router_b", "exp_w_gate", "exp_b_gate", "exp_w_up",
          "exp_b_up", "exp_w_down", "exp_b_down", "ple_w_proj", "ple_w_gate", "ple_b_gate"]


def kernel(**inputs):
    B = inputs["x"].shape[0]
    T = inputs["x"].shape[1]
    nc = build_program(T=T)
    in_maps = []
    shared = {k: np.ascontiguousarray(inputs[k], dtype=np.float32) for k in _NAMES if k not in ("x", "p")}
    for b in range(B):
        m = dict(shared)
        m["x"] = np.ascontiguousarray(inputs["x"][b], dtype=np.float32)
        m["p"] = np.ascontiguousarray(inputs["p"][:, b], dtype=np.float32)
        in_maps.append(m)
    res = run_bass_kernel_spmd(nc, in_maps, core_ids=list(range(B)))
    return np.stack([np.asarray(r["out"]) for r in res.results], axis=0).astype(np.float32)
```

```python
import contextlib
import numpy as np
import concourse.bass as bass
import concourse.mybir as mybir
from concourse.bass_utils import run_bass_kernel_spmd

F32 = mybir.dt.float32
BF16 = mybir.dt.bfloat16
I32 = mybir.dt.int32
AF = mybir.ActivationFunctionType
ALU = mybir.AluOpType
AX = mybir.AxisListType

D = 1024
NE = 32
PLE = 256
ALPHA = 8 ** 0.25
LN_EPS = 1e-5
KINDS = (0, 1, 2, 0)
JIDX = (0, 0, 0, 1)


class Tok:
    __slots__ = ("name", "w", "r", "dkey")

    def __init__(self, name):
        self.name = name
        self.w = {}
        self.r = {}
        self.dkey = None


class KB:
    def __init__(self, nc, es):
        self.nc = nc
        self.es = es
        self.eng = dict(pe=nc.tensor, dve=nc.vector, act=nc.scalar, pool=nc.gpsimd, sp=nc.sync)
        self.sems, self.cnt, self.isdma = {}, {}, {}
        self.waited = {k: {} for k in self.eng}
        for k in self.eng:
            self._newsem(k, False)
        self.ntok = 0
        self.free_slots = []
        for i in range(64):
            self._newsem(f"d{i}", True)
            self.free_slots.append(f"d{i}")
        self.live = []
        self.mark = {}

    def _newsem(self, key, isdma):
        self.sems[key] = self.es.enter_context(self.nc.semaphore("s_" + key))
        self.cnt[key] = 0
        self.isdma[key] = isdma

    def tok(self, name="t"):
        self.ntok += 1
        return Tok(f"{name}{self.ntok}")

    def _wait(self, e, evs):
        need = {}
        for ev in evs:
            for k, v in ev.items():
                if self.isdma[k]:
                    if v <= self.mark.get(k, 0):
                        continue
                    v = self.cnt[k]
                if v > need.get(k, 0):
                    need[k] = v
        for k, v in need.items():
            if self.waited[e].get(k, 0) >= v:
                continue
            self.eng[e].wait_ge(self.sems[k], v)
            self.waited[e][k] = v

    def op(self, e, emit, reads=(), writes=()):
        evs = [t.w for t in reads] + [t.w for t in writes] + [t.r for t in writes]
        self._wait(e, evs)
        inst = emit(self.eng[e])
        self.cnt[e] += 1
        inst.then_inc(self.sems[e], 1)
        v = self.cnt[e]
        for t in reads:
            t.r[e] = v
        for t in writes:
            t.w = {e: v}
            t.r = {}
        return inst

    def dma(self, q, emit, reads=(), writes=(), key=None, join=False):
        evs = [t.w for t in reads] + [t.r for t in writes]
        if not join:
            evs += [t.w for t in writes]
        self._wait(q, evs)
        if key.dkey is None:
            key.dkey = self.free_slots.pop(0)
            self.live.append(key)
        k = key.dkey
        inst = emit(self.eng[q])
        self.cnt[k] += 16
        inst.then_inc(self.sems[k], 16)
        v = self.cnt[k]
        for t in reads:
            t.r[k] = v
        for t in writes:
            if join:
                t.w[k] = v
            else:
                t.w = {k: v}
                t.r = {}
        return inst

    def barrier(self):
        allev = [dict(self.cnt)]
        for e in self.eng:
            self._wait(e, allev)
        self.mark = {k: v for k, v in self.cnt.items() if self.isdma[k]}
        for t in self.live:
            self.free_slots.append(t.dkey)
            t.dkey = None
        self.live = []


def _cfg_caps(T):
    return {4096: 640, 512: 128, 1024: 256}[T]


def build_program(T=4096, layers=(0, 1, 2, 3), dbg=False):
    C = _cfg_caps(T)
    NT = T // 128
    NQ = T // 512
    CR = C // 128
    NSLOT = NE * C
    nc = bass.Bass("TRN2", target_bir_lowering=False)

    def din(name, shape, dt=F32):
        return nc.dram_tensor(name, list(shape), dt, kind="ExternalInput").ap()

    def dscr(name, shape, dt=F32):
        return nc.dram_tensor(name, list(shape), dt).ap()

    x_in = din("x", [T, D])
    p_in = din("p", [4, T, PLE])
    conv_w_in = din("conv_w_in", [2, D, 3 * D])
    conv_w = din("conv_w", [2, 3, D])
    conv_w_out = din("conv_w_out", [2, D, D])
    sb_w_in = din("sb_w_in", [1, D, 3 * D])
    sb_w_out = din("sb_w_out", [1, D, D])
    fox_w_in = din("fox_w_in", [1, D, 3 * D + 16])
    fox_b_f = din("fox_b_f", [1, 16])
    fox_w_out = din("fox_w_out", [1, D, D])
    ln1_g = din("ln1_g", [4, D]); ln1_b = din("ln1_b", [4, D])
    ln2_g = din("ln2_g", [4, D]); ln2_b = din("ln2_b", [4, D])
    router_w = din("router_w", [4, D, NE]); router_b = din("router_b", [4, NE])
    exp_w_gate = din("exp_w_gate", [4, NE, D, D]); exp_b_gate = din("exp_b_gate", [4, NE, D])
    exp_w_up = din("exp_w_up", [4, NE, D, D]); exp_b_up = din("exp_b_up", [4, NE, D])
    exp_w_down = din("exp_w_down", [4, NE, D, D]); exp_b_down = din("exp_b_down", [4, NE, D])
    ple_w_proj = din("ple_w_proj", [4, PLE, D]); ple_w_gate = din("ple_w_gate", [4, D, D])
    ple_b_gate = din("ple_b_gate", [4, D])
    out_d = nc.dram_tensor("out", [T, D], F32, kind="ExternalOutput").ap()

    xres = [dscr("xres0", [T, D]), dscr("xres1", [T, D])]
    x1res = dscr("x1res", [T, D])
    x1bf = dscr("x1bf", [T, D], BF16)
    yslots = dscr("yslots", [NSLOT + 128, D])
    toklist = dscr("toklist", [NSLOT + 128, 1], I32)
    qT_d = dscr("qT_d", [8, 128, T], BF16)
    kT_d = dscr("kT_d", [8, 128, T], BF16)
    v_d = dscr("v_d", [T, D], BF16)
    nrow_d = dscr("nrow_d", [16, 3, T], BF16)
    dbg_d = None
    if dbg:
        dbg_d = nc.dram_tensor("dbg", [T, D], F32, kind="ExternalOutput").ap()

    es = contextlib.ExitStack()
    with es:
        kb = KB(nc, es)
        op, dma = kb.op, kb.dma

        uid = [0]

        def sb(name, shape, dt=F32, scope=es):
            uid[0] += 1
            return scope.enter_context(nc.sbuf_tensor(f"{name}_{uid[0]}", list(shape), dt))

        def ps(name, shape, dt=F32, scope=es):
            uid[0] += 1
            return scope.enter_context(nc.psum_tensor(f"{name}_{uid[0]}", list(shape), dt))

        t_const = kb.tok("const")
        ident32 = sb("ident32", [128, 128])
        identb = sb("identb", [128, 128], BF16)
        ones_b = sb("ones_b", [128, 128], BF16)
        tri_lt = sb("tri_lt", [128, 128], BF16)
        ntri_ge = sb("ntri_ge", [128, 128], BF16)
        nones_b = sb("nones_b", [128, 128], BF16)
        mask_lt = sb("mask_lt", [128, 128], BF16)
        mask_le = sb("mask_le", [128, 128], BF16)
        c_one = sb("c_one", [128, 1])
        c_eps = sb("c_eps", [128, 1])
        c_nhalf = sb("c_nhalf", [128, 1])
        tokid = sb("tokid", [128, NT], I32)
        ecol = sb("ecol", [128, NE])
        zrow = sb("zrow", [128, D])
        zi = sb("zi", [128, (NSLOT + 128) // 128], I32)
        tmp32 = sb("tmp32", [128, 128])

        def mk_tri(dst, cmp, mult_p, mult_f, fillv, inv):
            def e1(g):
                return g.memset(tmp32[:], inv)
            op("pool", e1, writes=[t_const])

            def e2(g):
                return g.affine_select(out=tmp32[:], in_=tmp32[:], pattern=[[mult_f, 128]], compare_op=cmp,
                                       fill=fillv, base=0, channel_multiplier=mult_p)
            op("pool", e2, writes=[t_const])
            op("dve", lambda v: v.tensor_copy(out=dst[:], in_=tmp32[:]), reads=[t_const], writes=[t_const])

        mk_tri(ident32, ALU.not_equal, 1, -1, 1.0, 0.0)
        mk_tri(identb, ALU.not_equal, 1, -1, 1.0, 0.0)
        mk_tri(tri_lt, ALU.is_gt, -1, 1, 0.0, 1.0)
        mk_tri(mask_lt, ALU.is_gt, -1, 1, 0.0, 1.0)
        mk_tri(mask_le, ALU.is_ge, -1, 1, 0.0, 1.0)
        mk_tri(ntri_ge, ALU.is_ge, 1, -1, 0.0, -1.0)
        zeros_b = sb("zeros_b", [128, 128], BF16)
        sel0 = sb("sel0", [128, 128], BF16)
        mk_tri(sel0, ALU.is_equal, 1, 0, 0.0, 1.0)
        negmask_gt = sb("negmask_gt", [128, 128], BF16)
        mk_tri(negmask_gt, ALU.is_gt, 1, -1, 0.0, -30000.0)
        selh = sb("selh", [16, 16, 128], BF16)
        selt = sb("selt", [16, 16, 128])
        op("pool", lambda g: g.memset(selt[:], 1.0), writes=[t_const])
        op("pool", lambda g: g.affine_select(out=selt[:], in_=selt[:], pattern=[[-1, 16], [0, 128]], compare_op=ALU.is_equal,
                                             fill=0.0, base=0, channel_multiplier=1), writes=[t_const])
        op("dve", lambda v: v.tensor_copy(out=selh[:], in_=selt[:]), reads=[t_const], writes=[t_const])
        zsrc = sb("zsrc", [128, 512], BF16)
        op("pool", lambda g: g.memset(zsrc[:], 0.0), writes=[t_const])
        op("pool", lambda g: g.memset(zeros_b[:], 0.0), writes=[t_const])
        op("pool", lambda g: g.memset(ones_b[:], 1.0), writes=[t_const])
        op("pool", lambda g: g.memset(nones_b[:], -1.0), writes=[t_const])
        op("pool", lambda g: g.memset(c_one[:], 1.0), writes=[t_const])
        op("pool", lambda g: g.memset(c_eps[:], LN_EPS), writes=[t_const])
        op("pool", lambda g: g.memset(c_nhalf[:], -0.5), writes=[t_const])
        op("pool", lambda g: g.memset(zrow[:], 0.0), writes=[t_const])
        op("pool", lambda g: g.memset(zi[:], 0), writes=[t_const])
        op("pool", lambda g: g.iota(tokid[:], pattern=[[128, NT]], base=0, channel_multiplier=1), writes=[t_const])
        op("pool", lambda g: g.iota(ecol[:], pattern=[[C, NE]], base=0, channel_multiplier=0,
                                    allow_small_or_imprecise_dtypes=True), writes=[t_const])
        t_ys = kb.tok("yslots")
        t_tl = kb.tok("toklist")
        dma("sp", lambda q: q.dma_start(out=yslots[NSLOT:NSLOT + 128, :], in_=zrow[:]), reads=[t_const],
            writes=[t_ys], key=t_ys, join=True)

        t_xres = [kb.tok("xres0"), kb.tok("xres1")]
        t_x1res = kb.tok("x1res")
        t_x1bf = kb.tok("x1bf")
        t_qkv = kb.tok("qkv")
        t_out = kb.tok("out")

        def layer_norm_tile(h, g_bc, b_bc, xo, stat, t_h, t_xo, t_stat, t_gb, eng2="pool"):
            def s1(v):
                v.bn_stats(out=stat[:, 0:6], in_=h[:, 0:512])
                return v.bn_stats(out=stat[:, 6:12], in_=h[:, 512:1024])
            op("dve", s1, reads=[t_h], writes=[t_stat])
            op("dve", lambda v: v.bn_aggr(out=stat[:, 12:14], in_=stat[:, 0:12]), reads=[t_stat], writes=[t_stat])
            op("pool", lambda g: g.tensor_tensor(out=stat[:, 14:15], in0=stat[:, 13:14], in1=c_eps[:], op=ALU.add),
               reads=[t_stat, t_const], writes=[t_stat])
            op("pool", lambda g: g.tensor_tensor(out=stat[:, 15:16], in0=stat[:, 14:15], in1=c_nhalf[:], op=ALU.pow),
               reads=[t_stat, t_const], writes=[t_stat])
            op("dve", lambda v: v.tensor_scalar(out=xo[:], in0=h[:], scalar1=stat[:, 12:13], scalar2=stat[:, 15:16],
                                                op0=ALU.subtract, op1=ALU.mult),
               reads=[t_h, t_stat], writes=[t_xo])
            op(eng2, lambda g: g.tensor_tensor(out=xo[:], in0=xo[:], in1=g_bc[:], op=ALU.mult),
               reads=[t_gb], writes=[t_xo])
            op(eng2, lambda g: g.tensor_tensor(out=xo[:], in0=xo[:], in1=b_bc[:], op=ALU.add),
               reads=[t_gb], writes=[t_xo])

        def bcast_load(dst, src_row, t_dst, q="sp"):
            n = dst.shape[-1] if hasattr(dst, "shape") else None
            dma(q, lambda e: e.dma_start(out=dst[:], in_=src_row.partition_broadcast(128)), writes=[t_dst], key=t_dst)

        def phase_xT(xin, t_xin, xT, t_xT):
            with contextlib.ExitStack() as sc:
                xs = [sb(f"xs{i}", [128, D], scope=sc) for i in range(2)]
                xb = [sb(f"xb{i}", [128, D], BF16, scope=sc) for i in range(2)]
                pT = [ps(f"pT{i}", [128, 8, 128], BF16, scope=sc) for i in range(2)]
                t_xs = [kb.tok("xs") for _ in range(2)]
                t_xb = [kb.tok("xb") for _ in range(2)]
                t_pT = [kb.tok("pT") for _ in range(2)]
                for i in range(NT):
                    b = i % 2
                    dma("sp", lambda q: q.dma_start(out=xs[b][:], in_=xin[i * 128:(i + 1) * 128, :]),
                        reads=[t_xin], writes=[t_xs[b]], key=t_xs[b])
                    op("act", lambda a: a.copy(out=xb[b][:], in_=xs[b][:]), reads=[t_xs[b]], writes=[t_xb[b]])

                    def tr(pe):
                        for c in range(8):
                            r = pe.transpose(out=pT[b][:, c, :], in_=xb[b][:, c * 128:(c + 1) * 128], identity=identb[:])
                        return r
                    op("pe", tr, reads=[t_xb[b], t_const], writes=[t_pT[b]])
                    op("dve", lambda v: v.tensor_copy(out=xT[:, :, i * 128:(i + 1) * 128], in_=pT[b][:]),
                       reads=[t_pT[b]], writes=[t_xT])
                kb.barrier()

        def phase_conv(j, xT, t_xT, mT, t_mT):
            with contextlib.ExitStack() as sc:
                wc = [sb(f"wc{i}", [128, 8, 384], BF16, scope=sc) for i in range(2)]
                t_wc = [kb.tok("wc") for _ in range(2)]
                cw = sb("cw", [128, 8, 3], scope=sc)
                cwr = sb("cwr", [3, D], scope=sc)
                cwb = sb("cwb", [3, D], BF16, scope=sc)
                t_cw = kb.tok("cw")
                pcw = ps("pcw", [128, 8, 4], BF16, scope=sc)
                t_pcw = kb.tok("pcw")
                pB = [ps(f"pB{i}", [128, 512], scope=sc) for i in range(2)]
                pC = [ps(f"pC{i}", [128, 512], scope=sc) for i in range(2)]
                pH = [ps(f"pH{i}", [128, 512], scope=sc) for i in range(2)]
                t_pB = [kb.tok("pB") for _ in range(2)]
                t_pC = [kb.tok("pC") for _ in range(2)]
                t_pH = [kb.tok("pH") for _ in range(2)]
                ctmp = [sb(f"ctmp{i}", [128, 512], scope=sc) for i in range(2)]
                t_ct = [kb.tok("ct") for _ in range(2)]
                u = [sb(f"u{i}", [128, 514], scope=sc) for i in range(2)]
                t_u = [kb.tok("u") for _ in range(2)]
                cv = [sb(f"cv{i}", [128, 512], scope=sc) for i in range(2)]
                t_cv = [kb.tok("cv") for _ in range(2)]
                cwlo = sb("cwlo", [3, D], BF16, scope=sc)
                cwt = sb("cwt", [3, D], scope=sc)
                dma("sp", lambda q: q.dma_start(out=cwr[:], in_=conv_w[j]), writes=[t_cw], key=t_cw)
                op("dve", lambda v: v.tensor_copy(out=cwb[:], in_=cwr[:]), reads=[t_cw], writes=[t_cw])
                op("dve", lambda v: v.tensor_copy(out=cwt[:], in_=cwb[:]), reads=[t_cw], writes=[t_cw])
                op("dve", lambda v: v.tensor_tensor(out=cwt[:], in0=cwr[:], in1=cwt[:], op=ALU.subtract), writes=[t_cw])
                op("dve", lambda v: v.tensor_copy(out=cwlo[:], in_=cwt[:]), writes=[t_cw])
                for part, src in ((0, cwb), (1, cwlo)):
                    def tr(pe):
                        for c in range(8):
                            r = pe.transpose(out=pcw[:, c, 0:3], in_=src[:, c * 128:(c + 1) * 128], identity=identb[0:3, 0:3])
                        return r
                    op("pe", tr, reads=[t_cw, t_const], writes=[t_pcw])
                    if part == 0:
                        op("dve", lambda v: v.tensor_copy(out=cw[:], in_=pcw[:, :, 0:3]), reads=[t_pcw], writes=[t_cw])
                    else:
                        op("dve", lambda v: v.tensor_tensor(out=cw[:], in0=cw[:], in1=pcw[:, :, 0:3], op=ALU.add),
                           reads=[t_pcw], writes=[t_cw])
                wsrc = conv_w_in[j].rearrange("(kc p) n -> p kc n", p=128)

                def load_w(c):
                    b = c % 2
                    for g in range(3):
                        dma("pool", lambda q: q.dma_start(out=wc[b][:, :, g * 128:(g + 1) * 128],
                                                          in_=wsrc[:, :, g * 1024 + c * 128:g * 1024 + (c + 1) * 128]),
                            writes=[t_wc[b]], key=t_wc[b], join=(g > 0))
                load_w(0)
                it = 0
                for c in range(8):
                    if c + 1 < 8:
                        load_w(c + 1)
                    wb_ = wc[c % 2]
                    for tq in range(NQ):
                        b = it % 2
                        it += 1
                        for (pp, tp, g) in ((pB[b], t_pB[b], 0), (pC[b], t_pC[b], 1), (pH[b], t_pH[b], 2)):
                            def mm(pe):
                                for kc in range(8):
                                    r = pe.matmul(pp[:], lhsT=wb_[:, kc, g * 128:(g + 1) * 128],
                                                  rhs=xT[:, kc, tq * 512:(tq + 1) * 512], start=(kc == 0), stop=(kc == 7))
                                return r
                            op("pe", mm, reads=[t_wc[c % 2], t_xT], writes=[tp])
                        op("act", lambda a: a.copy(out=ctmp[b][:], in_=pC[b][:]), reads=[t_pC[b]], writes=[t_ct[b]])
                        if tq == 0:
                            op("pool", lambda g_: g_.memset(u[b][:, 0:2], 0.0), writes=[t_u[b]])
                        else:
                            op("pool", lambda g_: g_.tensor_copy(out=u[b][:, 0:2], in_=u[1 - b][:, 512:514]),
                               reads=[t_u[1 - b]], writes=[t_u[b]])
                        op("dve", lambda v: v.tensor_tensor(out=u[b][:, 2:514], in0=ctmp[b][:], in1=pH[b][:], op=ALU.mult),
                           reads=[t_ct[b], t_pH[b]], writes=[t_u[b]])
                        op("dve", lambda v: v.tensor_scalar(out=cv[b][:], in0=u[b][:, 2:514], scalar1=cw[:, c, 2:3],
                                                            scalar2=None, op0=ALU.mult),
                           reads=[t_u[b], t_cw], writes=[t_cv[b]])
                        op("dve", lambda v: v.scalar_tensor_tensor(out=cv[b][:], in0=u[b][:, 1:513], scalar=cw[:, c, 1:2],
                                                                   in1=cv[b][:], op0=ALU.mult, op1=ALU.add),
                           reads=[t_u[b], t_cw], writes=[t_cv[b]])
                        op("dve", lambda v: v.scalar_tensor_tensor(out=cv[b][:], in0=u[b][:, 0:512], scalar=cw[:, c, 0:1],
                                                                   in1=cv[b][:], op0=ALU.mult, op1=ALU.add),
                           reads=[t_u[b], t_cw], writes=[t_cv[b]])
                        op("dve", lambda v: v.tensor_tensor(out=mT[:, c, tq * 512:(tq + 1) * 512], in0=cv[b][:],
                                                            in1=pB[b][:], op=ALU.mult),
                           reads=[t_cv[b], t_pB[b]], writes=[t_mT])
                kb.barrier()

        def phase_qkv(kind, w_in_d, xT, t_xT, ncum, refB, t_cum):
            with contextlib.ExitStack() as sc:
                wsrc = w_in_d.rearrange("(kc p) n -> p kc n", p=128)
                wq = [sb(f"wq{i}", [128, 8, 512], BF16, scope=sc) for i in range(2)]; t_wq = [kb.tok("wq") for _ in range(2)]
                wv = sb("wv", [128, 8, D], BF16, scope=sc); t_wv = kb.tok("wv")
                stg = [sb(f"stg{i}", [128, T], BF16, scope=sc) for i in range(2)]; t_stg = [kb.tok("stg") for _ in range(2)]
                vst = [sb(f"vst{i}", [128, D], BF16, scope=sc) for i in range(2)]; t_vst = [kb.tok("vst") for _ in range(2)]
                pq = [ps(f"pq{i}", [128, 512], scope=sc) for i in range(2)]; t_pq = [kb.tok("pq") for _ in range(2)]
                pv = [ps(f"pv{i}", [128, 512], scope=sc) for i in range(2)]; t_pv = [kb.tok("pv") for _ in range(2)]
                dma("pool", lambda q: q.dma_start(out=wv[:], in_=wsrc[:, :, 2048:3072]), writes=[t_wv], key=t_wv)

                def load_g(g):
                    dma("pool", lambda q: q.dma_start(out=wq[g % 2][:], in_=wsrc[:, :, g * 512:(g + 1) * 512]),
                        writes=[t_wq[g % 2]], key=t_wq[g % 2])
                load_g(0)
                it = 0
                for g in range(4):
                    if g + 1 < 4:
                        load_g(g + 1)
                    for cc in range(4):
                        c16 = g * 4 + cc
                        sbuf_ = stg[c16 % 2]; ts_ = t_stg[c16 % 2]
                        for tq in range(NQ):
                            b = it % 2
                            it += 1

                            def mm(pe):
                                for kc in range(8):
                                    r = pe.matmul(pq[b][:], lhsT=wq[g % 2][:, kc, cc * 128:(cc + 1) * 128],
                                                  rhs=xT[:, kc, tq * 512:(tq + 1) * 512], start=(kc == 0), stop=(kc == 7))
                                return r
                            op("pe", mm, reads=[t_wq[g % 2], t_xT], writes=[t_pq[b]])
                            scl = 0.125 if c16 < 8 else 1.0
                            if it % 2 == 0:
                                op("act", lambda a: a.mul(out=sbuf_[:, tq * 512:(tq + 1) * 512], in_=pq[b][:], mul=scl),
                                   reads=[t_pq[b]], writes=[ts_])
                            else:
                                op("dve", lambda v: v.tensor_scalar(out=sbuf_[:, tq * 512:(tq + 1) * 512], in0=pq[b][:], scalar1=scl,
                                                                    scalar2=None, op0=ALU.mult), reads=[t_pq[b]], writes=[ts_])
                        dst = qT_d[c16] if c16 < 8 else kT_d[c16 - 8]
                        dma("sp", lambda q: q.dma_start(out=dst, in_=sbuf_[:]), reads=[ts_], writes=[t_qkv], key=ts_, join=True)
                for i in range(NT):
                    b = i % 2
                    for hh in range(2):
                        def mmv(pe):
                            for kc in range(8):
                                r = pe.matmul(pv[hh][:], lhsT=xT[:, kc, i * 128:(i + 1) * 128],
                                              rhs=wv[:, kc, hh * 512:(hh + 1) * 512], start=(kc == 0), stop=(kc == 7))
                            return r
                        op("pe", mmv, reads=[t_xT, t_wv], writes=[t_pv[hh]])
                        if hh == 0:
                            op("act", lambda a: a.copy(out=vst[b][:, 0:512], in_=pv[hh][:]), reads=[t_pv[hh]], writes=[t_vst[b]])
                        else:
                            op("dve", lambda v: v.tensor_copy(out=vst[b][:, 512:1024], in_=pv[hh][:]), reads=[t_pv[hh]], writes=[t_vst[b]])
                    dma("sp", lambda q: q.dma_start(out=v_d[i * 128:(i + 1) * 128, :], in_=vst[b][:]), reads=[t_vst[b]],
                        writes=[t_qkv], key=t_vst[b], join=True)
                if kind == 2:
                    wf = sb("wf", [128, 8, 16], BF16, scope=sc); t_wf = kb.tok("wf")
                    bf_bc = sb("bf_bc", [128, 16], scope=sc)
                    basef = sb("basef", [128, 16], scope=sc); t_bf = kb.tok("basef")
                    fl = [sb(f"fl{i}", [128, 64], scope=sc) for i in range(2)]; t_fl = [kb.tok("fl") for _ in range(2)]
                    flb = [sb(f"flb{i}", [128, 32], BF16, scope=sc) for i in range(2)]; t_flb = [kb.tok("flb") for _ in range(2)]
                    pf = ps("pf", [128, 16], scope=sc); t_pf = kb.tok("pf")
                    pc = ps("pc", [128, 2, 16], scope=sc); t_pc = kb.tok("pc")
                    ptr = ps("ptr", [128, 3, 128], BF16, scope=sc); t_ptr = kb.tok("ptr")
                    nsp = [sb(f"nsp{i}", [128, 3, 16], BF16, scope=sc) for i in range(2)]; t_nsp = [kb.tok("nsp") for _ in range(2)]
                    nr = [sb(f"nr{i}", [128, 48], scope=sc) for i in range(2)]
                    dma("pool", lambda q: q.dma_start(out=wf[:], in_=wsrc[:, :, 3072:3088]), writes=[t_wf], key=t_wf)
                    dma("sp", lambda q: q.dma_start(out=bf_bc[:], in_=fox_b_f[0].partition_broadcast(128)), writes=[t_wf], key=t_wf, join=True)
                    op("pool", lambda g_: g_.memset(basef[:], 0.0), writes=[t_bf])
                    for i in range(NT):
                        b = i % 2
                        F = fl[b]

                        def mmf(pe):
                            for kc in range(8):
                                r = pe.matmul(pf[:], lhsT=xT[:, kc, i * 128:(i + 1) * 128], rhs=wf[:, kc, :], start=(kc == 0), stop=(kc == 7))
                            return r
                        op("pe", mmf, reads=[t_xT, t_wf], writes=[t_pf])
                        op("dve", lambda v: v.tensor_tensor(out=F[:, 0:16], in0=pf[:], in1=bf_bc[:], op=ALU.add), reads=[t_pf, t_wf], writes=[t_fl[b]])
                        op("act", lambda a: a.activation(out=F[:, 16:32], in_=F[:, 0:16], func=AF.Exp, scale=-1.0), writes=[t_fl[b]])
                        op("act", lambda a: a.activation(out=F[:, 32:48], in_=F[:, 16:32], func=AF.Ln, bias=c_one[:], scale=1.0),
                           reads=[t_const], writes=[t_fl[b]])
                        op("dve", lambda v: v.tensor_copy(out=flb[b][:, 0:16], in_=F[:, 32:48]), reads=[t_fl[b]], writes=[t_flb[b]])
                        op("dve", lambda v: v.tensor_copy(out=F[:, 48:64], in_=flb[b][:, 0:16]), writes=[t_fl[b]])
                        op("dve", lambda v: v.tensor_tensor(out=F[:, 48:64], in0=F[:, 32:48], in1=F[:, 48:64], op=ALU.subtract), writes=[t_fl[b]])
                        op("dve", lambda v: v.tensor_copy(out=flb[b][:, 16:32], in_=F[:, 48:64]), reads=[t_fl[b]], writes=[t_flb[b]])

                        def mmc(pe):
                            pe.matmul(pc[:, 0, :], lhsT=mask_le[:], rhs=flb[b][:, 0:16], start=True, stop=False)
                            pe.matmul(pc[:, 0, :], lhsT=mask_le[:], rhs=flb[b][:, 16:32], start=False, stop=True)
                            pe.matmul(pc[:, 1, :], lhsT=ones_b[:], rhs=flb[b][:, 0:16], start=True, stop=False)
                            return pe.matmul(pc[:, 1, :], lhsT=ones_b[:], rhs=flb[b][:, 16:32], start=False, stop=True)
                        op("pe", mmc, reads=[t_flb[b], t_const], writes=[t_pc])
                        op("dve", lambda v: v.tensor_tensor(out=ncum[:, i, :], in0=pc[:, 0, :], in1=basef[:], op=ALU.add),
                           reads=[t_pc, t_bf], writes=[t_cum])
                        op("dve", lambda v: v.tensor_tensor(out=basef[:], in0=basef[:], in1=pc[:, 1, :], op=ALU.add),
                           reads=[t_pc], writes=[t_bf])
                        N3 = nsp[b]
                        R_ = nr[b]
                        op("dve", lambda v: v.tensor_copy(out=N3[:, 0, :], in_=ncum[:, i, :]), reads=[t_cum], writes=[t_nsp[b]])
                        op("dve", lambda v: v.tensor_copy(out=R_[:, 0:16], in_=N3[:, 0, :]), writes=[t_nsp[b]])
                        op("dve", lambda v: v.tensor_tensor(out=R_[:, 16:32], in0=ncum[:, i, :], in1=R_[:, 0:16], op=ALU.subtract), writes=[t_nsp[b]])
                        op("dve", lambda v: v.tensor_copy(out=N3[:, 1, :], in_=R_[:, 16:32]), writes=[t_nsp[b]])
                        op("dve", lambda v: v.tensor_copy(out=R_[:, 0:16], in_=N3[:, 1, :]), writes=[t_nsp[b]])
                        op("dve", lambda v: v.tensor_tensor(out=R_[:, 32:48], in0=R_[:, 16:32], in1=R_[:, 0:16], op=ALU.subtract), writes=[t_nsp[b]])
                        op("dve", lambda v: v.tensor_copy(out=N3[:, 2, :], in_=R_[:, 32:48]), writes=[t_nsp[b]])

                        def trn(pe):
                            for g_ in range(3):
                                r = pe.transpose(out=ptr[0:16, g_, :], in_=N3[:, g_, :], identity=identb[:])
                            return r
                        op("pe", trn, reads=[t_nsp[b], t_const], writes=[t_ptr])
                        op("dve", lambda v: v.tensor_scalar(out=refB[0:16, :, i * 128:(i + 1) * 128], in0=ptr[0:16, :, :], scalar1=-1.0,
                                                            scalar2=None, op0=ALU.mult), reads=[t_ptr], writes=[t_cum])
                    dma("sp", lambda q: q.dma_start(out=nrow_d, in_=refB[0:16, :, :]), reads=[t_cum], writes=[t_qkv], key=t_cum, join=True)
                kb.barrier()

        def phase_attn(kind, mT, t_mT, ncum, refB, t_cum):
            with contextlib.ExitStack() as sc:
                KQ = 67 if kind == 2 else 64
                qs = [sb(f"qs{i}", [128, T], BF16, scope=sc) for i in range(2)]
                ks = [sb(f"ks{i}", [128, T], BF16, scope=sc) for i in range(2)]
                vs = [sb(f"vs{i}", [128, NT, 128], BF16, scope=sc) for i in range(2)]
                t_q = [kb.tok("q") for _ in range(2)]; t_k = [kb.tok("k") for _ in range(2)]; t_v = [kb.tok("v") for _ in range(2)]
                NB = 3
                ex = [sb(f"ex{i}", [128, 512], scope=sc) for i in range(NB)]; t_ex = [kb.tok("ex") for _ in range(NB)]
                spb = [sb(f"spb{i}", [128, 512], BF16, scope=sc) for i in range(NB)]; t_sp = [kb.tok("sp") for _ in range(NB)]
                wt = [sb(f"wt{i}", [128, 512], BF16, scope=sc) for i in range(NB)]; t_wt = [kb.tok("wt") for _ in range(NB)]
                acc = sb("acc", [128, 512], BF16, scope=sc); t_acc = kb.tok("acc")
                rden = sb("rden", [128, 512], scope=sc); t_rden = kb.tok("rden")
                pst = [ps(f"pst{i}", [128, 512], scope=sc) for i in range(2)]; t_pst = [kb.tok("pst") for _ in range(2)]
                parg = [ps(f"parg{i}", [128, 512], scope=sc) for i in range(2)]; t_parg = [kb.tok("parg") for _ in range(2)]
                po = [ps(f"po{i}", [128, 512], scope=sc) for i in range(2)]; t_po = [kb.tok("po") for _ in range(2)]
                pden = ps("pden", [128, 512], scope=sc); t_pden = kb.tok("pden")
                vsrc = v_d.rearrange("(j p) d -> p j d", p=128)
                if kind == 2:
                    for b in range(2):
                        op("pool", lambda g: g.memset(ks[b][64:67, :], 1.0), writes=[t_k[b]])

                def load_h(hd):
                    b = hd % 2
                    c, hh = hd // 2, hd % 2
                    dma("sp", lambda q: q.dma_start(out=qs[b][0:64, :], in_=qT_d[c, hh * 64:(hh + 1) * 64, :]), reads=[t_qkv],
                        writes=[t_q[b]], key=t_q[b])
                    if kind == 2:
                        dma("sp", lambda q: q.dma_start(out=qs[b][64:67, :], in_=nrow_d[hd]), reads=[t_qkv],
                            writes=[t_q[b]], key=t_q[b], join=True)
                    dma("sp", lambda q: q.dma_start(out=ks[b][0:64, :], in_=kT_d[c, hh * 64:(hh + 1) * 64, :]), reads=[t_qkv],
                        writes=[t_k[b]], key=t_k[b], join=True)
                    if hh == 0:
                        dma("sp", lambda q: q.dma_start(out=vs[c % 2][:], in_=vsrc[:, :, c * 128:(c + 1) * 128]), reads=[t_qkv],
                            writes=[t_v[c % 2]], key=t_v[c % 2])
                load_h(0)
                it = 0
                grp = 0
                for hd in range(16):
                    c, hh = hd // 2, hd % 2
                    hb = hd % 2
                    cb = c % 2
                    P0 = hh * 64
                    if hd + 1 < 16:
                        load_h(hd + 1)
                    for tq in range(NQ):
                        jmax = 4 * tq + 3
                        pob = po[grp % 2]; t_pob = t_po[grp % 2]
                        grp += 1
                        if kind != 2:
                            op("pool", lambda g: g.memset(acc[:], 0.0), writes=[t_acc])
                        op("pe", lambda pe: pe.matmul(pob[:], lhsT=zeros_b[:], rhs=zsrc[:], start=True, stop=False),
                           reads=[t_const], writes=[t_pob])
                        if kind == 2:
                            op("pe", lambda pe: pe.matmul(pden[:], lhsT=zeros_b[:], rhs=zsrc[:], start=True, stop=False),
                               reads=[t_const], writes=[t_pden])
                        order = list(range(jmax + 1)) if kind == 2 else list(range(jmax, -1, -1))
                        ntile = len(order)
                        info = {}

                        def stage1(nj):
                            j = order[nj]
                            b2 = (it + nj) % 2
                            b3 = (it + nj) % NB
                            col0 = max(0, j - 4 * tq) * 128
                            diag = j >= 4 * tq
                            q0 = tq * 512 + col0
                            info[nj] = (j, b2, b3, col0, diag, q0)

                            def mms(pe):
                                r = pe.matmul(pst[b2][:, col0:512], lhsT=ks[hb][0:KQ, j * 128:(j + 1) * 128],
                                              rhs=qs[hb][0:KQ, q0:(tq + 1) * 512], start=True, stop=not (kind == 2 and diag))
                                if kind == 2 and diag:
                                    r = pe.matmul(pst[b2][:, col0:col0 + 128], lhsT=identb[:], rhs=negmask_gt[:], start=False, stop=True)
                                return r
                            op("pe", mms, reads=[t_q[hb], t_k[hb], t_const], writes=[t_pst[b2]])
                            if kind == 2:
                                op("act", lambda a: a.activation(out=wt[b3][:, col0:512], in_=pst[b2][:, col0:512], func=AF.Exp,
                                                                 bias=ncum[:, j, hd:hd + 1], scale=1.0),
                                   reads=[t_pst[b2], t_cum], writes=[t_wt[b3]])
                            else:
                                op("act", lambda a: a.activation(out=ex[b3][:, col0:512], in_=pst[b2][:, col0:512], func=AF.Exp),
                                   reads=[t_pst[b2]], writes=[t_ex[b3]])
                                op("act", lambda a: a.activation(out=spb[b3][:, col0:512], in_=ex[b3][:, col0:512], func=AF.Ln,
                                                                 bias=c_one[:], scale=1.0), reads=[t_ex[b3], t_const], writes=[t_sp[b3]])
                                if diag:
                                    op("pool", lambda g: g.tensor_tensor(out=spb[b3][:, col0:col0 + 128], in0=spb[b3][:, col0:col0 + 128],
                                                                         in1=mask_lt[:], op=ALU.mult), reads=[t_const], writes=[t_sp[b3]])

                        def stage2(nj):
                            (j, b2, b3, col0, diag, q0) = info[nj]

                            def mma(pe):
                                pe.matmul(parg[b2][:, col0:512], lhsT=ks[hb][0:64, j * 128:(j + 1) * 128],
                                          rhs=qs[hb][0:64, q0:(tq + 1) * 512], start=True, stop=False)
                                pe.matmul(parg[b2][:, col0:512], lhsT=ntri_ge[:], rhs=spb[b3][:, col0:512], start=False, stop=False)
                                return pe.matmul(parg[b2][:, col0:512], lhsT=nones_b[:], rhs=acc[:, col0:512], start=False, stop=True)
                            op("pe", mma, reads=[t_q[hb], t_k[hb], t_sp[b3], t_acc, t_const], writes=[t_parg[b2]])
                            op("pool", lambda g: g.tensor_tensor(out=acc[:, col0:512], in0=acc[:, col0:512], in1=spb[b3][:, col0:512],
                                                                 op=ALU.add), reads=[t_sp[b3]], writes=[t_acc])
                            op("act", lambda a: a.activation(out=wt[b3][:, col0:512], in_=parg[b2][:, col0:512], func=AF.Exp),
                               reads=[t_parg[b2]], writes=[t_wt[b3]])
                            if diag:
                                op("pool", lambda g: g.tensor_tensor(out=wt[b3][:, col0:col0 + 128], in0=wt[b3][:, col0:col0 + 128],
                                                                     in1=mask_lt[:], op=ALU.mult), reads=[t_const], writes=[t_wt[b3]])

                        def stage3(nj):
                            (j, b2, b3, col0, diag, q0) = info[nj]
                            last = (nj == ntile - 1)

                            def mmo(pe):
                                r = pe.matmul(pob[:, col0:512], lhsT=vs[cb][:, j, :], rhs=wt[b3][:, col0:512], start=False, stop=last)
                                if kind == 2:
                                    r = pe.matmul(pden[:, col0:512], lhsT=ones_b[:], rhs=wt[b3][:, col0:512], start=False, stop=last)
                                return r
                            op("pe", mmo, reads=[t_wt[b3], t_v[cb], t_const], writes=[t_pob] + ([t_pden] if kind == 2 else []))

                        if kind == 2:
                            for n in range(ntile + 1):
                                if n < ntile:
                                    stage1(n)
                                if n >= 1:
                                    stage3(n - 1)
                        else:
                            for n in range(ntile + 2):
                                if n < ntile:
                                    stage1(n)
                                if 1 <= n <= ntile:
                                    stage2(n - 1)
                                if n >= 2:
                                    stage3(n - 2)
                        it += ntile
                        osl = mT[P0:P0 + 64, c, tq * 512:(tq + 1) * 512]
                        if kind == 2:
                            op("dve", lambda v: v.reciprocal(out=rden[P0:P0 + 64, :], in_=pden[P0:P0 + 64, :]), reads=[t_pden], writes=[t_rden])
                            op("dve", lambda v: v.tensor_tensor(out=osl, in0=pob[P0:P0 + 64, :], in1=rden[P0:P0 + 64, :], op=ALU.mult),
                               reads=[t_pob, t_rden], writes=[t_mT])
                        else:
                            op("dve", lambda v: v.tensor_copy(out=osl, in_=pob[P0:P0 + 64, :]), reads=[t_pob], writes=[t_mT])
                kb.barrier()

        def phase_outproj_ln1_route(li, w_out_d, xin, t_xin, mT, t_mT, gates, dests, t_route):
            with contextlib.ExitStack() as sc:
                wo = sb("wo", [128, 8, D], BF16, scope=sc); t_wo = kb.tok("wo")
                g_bc = sb("g_bc", [128, D], scope=sc); b_bc = sb("b_bc", [128, D], scope=sc); t_gb = kb.tok("gb")
                wr32 = sb("wr32", [128, 8, NE], scope=sc)
                wrh = sb("wrh", [128, 8, NE], BF16, scope=sc)
                wrl = sb("wrl", [128, 8, NE], BF16, scope=sc)
                wrt = sb("wrt", [128, 8, NE], scope=sc)
                rb_bc = sb("rb_bc", [128, NE], scope=sc)
                t_wr = kb.tok("wr")
                base = sb("base", [128, NE], scope=sc); t_base = kb.tok("base")
                xs = [sb(f"xs{i}", [128, D], scope=sc) for i in range(2)]; t_xs = [kb.tok("xs") for _ in range(2)]
                h = [sb(f"h{i}", [128, D], scope=sc) for i in range(2)]; t_h = [kb.tok("h") for _ in range(2)]
                x1 = [sb(f"x1_{i}", [128, D], scope=sc) for i in range(2)]; t_x1 = [kb.tok("x1") for _ in range(2)]
                x1h = [sb(f"x1h{i}", [128, D], BF16, scope=sc) for i in range(2)]; t_x1h = [kb.tok("x1h") for _ in range(2)]
                x1l = [sb(f"x1l{i}", [128, D], BF16, scope=sc) for i in range(2)]; t_x1l = [kb.tok("x1l") for _ in range(2)]
                x1t = [sb(f"x1t{i}", [128, D], scope=sc) for i in range(2)]
                xTh = [sb(f"xTh{i}", [128, 8, 128], BF16, scope=sc) for i in range(2)]; t_xTh = [kb.tok("xTh") for _ in range(2)]
                xTl = [sb(f"xTl{i}", [128, 8, 128], BF16, scope=sc) for i in range(2)]; t_xTl = [kb.tok("xTl") for _ in range(2)]
                stat = [sb(f"stat{i}", [128, 16], scope=sc) for i in range(2)]; t_stat = [kb.tok("stat") for _ in range(2)]
                rt = [sb(f"rt{i}", [128, 8 * NE], scope=sc) for i in range(2)]; t_rt = [kb.tok("rt") for _ in range(2)]
                mkb = [sb(f"mkb{i}", [128, NE], BF16, scope=sc) for i in range(2)]; t_mkb = [kb.tok("mkb") for _ in range(2)]
                sm = [sb(f"sm{i}", [128, 32], scope=sc) for i in range(2)]
                desti = sb("desti", [128, NT, 4], I32, scope=sc)
                py = [ps(f"py{i}", [128, D], scope=sc) for i in range(2)]; t_py = [kb.tok("py") for _ in range(2)]
                pTh = ps("pTh", [128, 8, 128], BF16, scope=sc); t_pTh = kb.tok("pTh")
                pTl = ps("pTl", [128, 8, 128], BF16, scope=sc); t_pTl = kb.tok("pTl")
                plog = ps("plog", [128, NE], scope=sc); t_plog = kb.tok("plog")
                pcnt = ps("pcnt", [128, 2, NE], scope=sc); t_pcnt = kb.tok("pcnt")

                dma("pool", lambda q: q.dma_start(out=wo[:], in_=w_out_d.rearrange("(kc p) n -> p kc n", p=128)),
                    writes=[t_wo], key=t_wo)
                dma("sp", lambda q: q.dma_start(out=g_bc[:], in_=ln1_g[li].partition_broadcast(128)), writes=[t_gb], key=t_gb)
                dma("sp", lambda q: q.dma_start(out=b_bc[:], in_=ln1_b[li].partition_broadcast(128)), writes=[t_gb], key=t_gb, join=True)
                dma("sp", lambda q: q.dma_start(out=wr32[:], in_=router_w[li].rearrange("(kc p) n -> p kc n", p=128)),
                    writes=[t_wr], key=t_wr)
                dma("sp", lambda q: q.dma_start(out=rb_bc[:], in_=router_b[li].partition_broadcast(128)), writes=[t_wr], key=t_wr, join=True)
                op("dve", lambda v: v.tensor_copy(out=wrh[:], in_=wr32[:]), reads=[t_wr], writes=[t_wr])
                op("dve", lambda v: v.tensor_copy(out=wrt[:], in_=wrh[:]), writes=[t_wr])
                op("dve", lambda v: v.tensor_tensor(out=wrt[:], in0=wr32[:], in1=wrt[:], op=ALU.subtract), writes=[t_wr])
                op("dve", lambda v: v.tensor_copy(out=wrl[:], in_=wrt[:]), writes=[t_wr])
                op("pool", lambda g: g.memset(base[:], 0.0), writes=[t_base])
                dma("sp", lambda q: q.dma_start(out=toklist.rearrange("(p r) o -> p (r o)", p=128), in_=zi[:]),
                    reads=[t_const], writes=[t_tl], key=t_tl)

                for i in range(NT):
                    b = i % 2
                    dma("sp", lambda q: q.dma_start(out=xs[b][:], in_=xin[i * 128:(i + 1) * 128, :]),
                        reads=[t_xin], writes=[t_xs[b]], key=t_xs[b])

                    def mm(pe):
                        for hh in range(2):
                            for c in range(8):
                                r = pe.matmul(py[b][:, hh * 512:(hh + 1) * 512], lhsT=mT[:, c, i * 128:(i + 1) * 128],
                                              rhs=wo[:, c, hh * 512:(hh + 1) * 512], start=(c == 0), stop=(c == 7))
                        return r
                    op("pe", mm, reads=[t_mT, t_wo], writes=[t_py[b]])
                    op("dve", lambda v: v.scalar_tensor_tensor(out=h[b][:], in0=xs[b][:], scalar=ALPHA, in1=py[b][:],
                                                               op0=ALU.mult, op1=ALU.add),
                       reads=[t_xs[b], t_py[b]], writes=[t_h[b]])
                    layer_norm_tile(h[b], g_bc, b_bc, x1[b], stat[b], t_h[b], t_x1[b], t_stat[b], t_gb)
                    dma("sp", lambda q: q.dma_start(out=x1res[i * 128:(i + 1) * 128, :], in_=x1[b][:]),
                        reads=[t_x1[b]], writes=[t_x1res], key=t_x1[b], join=True)
                    op("act", lambda a: a.copy(out=x1h[b][:], in_=x1[b][:]), reads=[t_x1[b]], writes=[t_x1h[b]])
                    dma("sp", lambda q: q.dma_start(out=x1bf[i * 128:(i + 1) * 128, :], in_=x1h[b][:]),
                        reads=[t_x1h[b]], writes=[t_x1bf], key=t_x1h[b], join=True)
                    op("pool", lambda g: g.tensor_copy(out=x1t[b][:], in_=x1h[b][:]), reads=[t_x1h[b]], writes=[t_x1l[b]])
                    op("pool", lambda g: g.tensor_tensor(out=x1t[b][:], in0=x1[b][:], in1=x1t[b][:], op=ALU.subtract),
                       reads=[t_x1[b]], writes=[t_x1l[b]])
                    op("pool", lambda g: g.tensor_copy(out=x1l[b][:], in_=x1t[b][:]), writes=[t_x1l[b]])
                    for (src, tsrc, pp, tpp, dst, tdst) in ((x1h[b], t_x1h[b], pTh, t_pTh, xTh[b], t_xTh[b]),
                                                            (x1l[b], t_x1l[b], pTl, t_pTl, xTl[b], t_xTl[b])):
                        def tr(pe):
                            for c in range(8):
                                r = pe.transpose(out=pp[:, c, :], in_=src[:, c * 128:(c + 1) * 128], identity=identb[:])
                            return r
                        op("pe", tr, reads=[tsrc, t_const], writes=[tpp])
                        op("act", lambda a: a.copy(out=dst[:], in_=pp[:]), reads=[tpp], writes=[tdst])

                    def mmr(pe):
                        n = 0
                        for (xa, wa) in ((xTh[b], wrh), (xTh[b], wrl), (xTl[b], wrh)):
                            for c in range(8):
                                r = pe.matmul(plog[:], lhsT=xa[:, c, :], rhs=wa[:, c, :], start=(n == 0), stop=(n == 23))
                                n += 1
                        return r
                    op("pe", mmr, reads=[t_xTh[b], t_xTl[b], t_wr], writes=[t_plog])
                    R = rt[b]
                    L = R[:, 0:32]; MK = R[:, 32:64]; EX = R[:, 64:96]; G = R[:, 96:128]
                    POS = R[:, 128:160]; TMP = R[:, 160:192]; OH = R[:, 192:224]; TMP2 = R[:, 224:256]
                    S = sm[b]
                    top8 = S[:, 0:8]; nmax = S[:, 8:9]; den = S[:, 9:10]; rden = S[:, 10:11]
                    dstf = S[:, 12:16]
                    tr_ = t_rt[b]
                    op("dve", lambda v: v.tensor_tensor(out=L, in0=plog[:], in1=rb_bc[:], op=ALU.add),
                       reads=[t_plog, t_wr], writes=[tr_])
                    op("dve", lambda v: v.max(out=top8, in_=L), writes=[tr_])
                    op("dve", lambda v: v.tensor_scalar(out=MK, in0=L, scalar1=S[:, 3:4], scalar2=None, op0=ALU.is_ge), writes=[tr_])
                    op("dve", lambda v: v.tensor_scalar(out=nmax, in0=S[:, 0:1], scalar1=-1.0, scalar2=None, op0=ALU.mult), writes=[tr_])
                    op("act", lambda a: a.activation(out=EX, in_=L, func=AF.Exp, bias=nmax, scale=1.0), writes=[tr_])
                    op("dve", lambda v: v.tensor_tensor(out=EX, in0=EX, in1=MK, op=ALU.mult), writes=[tr_])
                    op("dve", lambda v: v.reduce_sum(out=den, in_=EX, axis=AX.X), writes=[tr_])
                    op("dve", lambda v: v.reciprocal(out=rden, in_=den), writes=[tr_])
                    op("dve", lambda v: v.tensor_scalar(out=G, in0=EX, scalar1=rden, scalar2=None, op0=ALU.mult), writes=[tr_])
                    op("dve", lambda v: v.tensor_copy(out=mkb[b][:], in_=MK), reads=[tr_], writes=[t_mkb[b]])

                    def mmc(pe):
                        pe.matmul(pcnt[:, 0, :], lhsT=tri_lt[:], rhs=mkb[b][:], start=True, stop=True)
                        return pe.matmul(pcnt[:, 1, :], lhsT=ones_b[:], rhs=mkb[b][:], start=True, stop=True)
                    op("pe", mmc, reads=[t_mkb[b], t_const], writes=[t_pcnt])
                    op("dve", lambda v: v.tensor_tensor(out=POS, in0=pcnt[:, 0, :], in1=base[:], op=ALU.add),
                       reads=[t_pcnt, t_base], writes=[tr_])
                    op("dve", lambda v: v.tensor_tensor(out=base[:], in0=base[:], in1=pcnt[:, 1, :], op=ALU.add),
                       reads=[t_pcnt], writes=[t_base])
                    op("dve", lambda v: v.tensor_scalar(out=TMP, in0=POS, scalar1=float(C), scalar2=None, op0=ALU.is_lt), writes=[tr_])
                    op("dve", lambda v: v.tensor_tensor(out=POS, in0=POS, in1=ecol[:], op=ALU.add), reads=[t_const], writes=[tr_])
                    op("dve", lambda v: v.tensor_scalar(out=POS, in0=POS, scalar1=float(-NSLOT), scalar2=None, op0=ALU.add), writes=[tr_])
                    op("dve", lambda v: v.tensor_tensor(out=POS, in0=POS, in1=TMP, op=ALU.mult), writes=[tr_])
                    op("dve", lambda v: v.tensor_scalar(out=POS, in0=POS, scalar1=float(NSLOT), scalar2=None, op0=ALU.add), writes=[tr_])
                    for k in range(4):
                        op("dve", lambda v: v.tensor_scalar(out=OH, in0=L, scalar1=S[:, k:k + 1], scalar2=None, op0=ALU.is_equal), writes=[tr_])
                        op("dve", lambda v: v.tensor_tensor(out=TMP2, in0=OH, in1=POS, op=ALU.mult), writes=[tr_])
                        op("dve", lambda v: v.reduce_sum(out=dstf[:, k:k + 1], in_=TMP2, axis=AX.X), writes=[tr_])
                        op("dve", lambda v: v.tensor_tensor(out=TMP2, in0=OH, in1=G, op=ALU.mult), writes=[tr_])
                        op("dve", lambda v: v.reduce_sum(out=gates[:, i, k:k + 1], in_=TMP2, axis=AX.X), writes=[tr_, t_route])
                    op("dve", lambda v: v.tensor_copy(out=dests[:, i, :], in_=dstf), reads=[tr_], writes=[t_route])
                    for k in range(4):
                        dma("pool", lambda q: q.indirect_dma_start(
                            out=toklist[:, :], out_offset=bass.IndirectOffsetOnAxis(ap=dests[:, i, k:k + 1], axis=0),
                            in_=tokid[:, i:i + 1], in_offset=None), reads=[t_route, t_const], writes=[t_tl], key=t_tl, join=True)
                kb.barrier()

        def phase_experts(li):
            with contextlib.ExitStack() as sc:
                wg = [sb(f"wg{i}", [128, 8, D], BF16, scope=sc) for i in range(2)]
                wu = [sb(f"wu{i}", [128, 8, D], BF16, scope=sc) for i in range(2)]
                wd = [sb(f"wd{i}", [128, 8, D], BF16, scope=sc) for i in range(2)]
                t_wg = [kb.tok("wg") for _ in range(2)]; t_wu = [kb.tok("wu") for _ in range(2)]; t_wd = [kb.tok("wd") for _ in range(2)]
                bd = [sb(f"bd{i}", [128, D], scope=sc) for i in range(2)]; t_bd = [kb.tok("bd") for _ in range(2)]
                braw = sb("braw", [NE, 2, D], scope=sc)
                brb = sb("brb", [NE, 2, D], BF16, scope=sc)
                bT = sb("bT", [128, 2, 8, NE], scope=sc)
                t_b = kb.tok("bias")
                idx = [sb(f"idx{i}", [128, CR], I32, scope=sc) for i in range(2)]; t_idx = [kb.tok("idx") for _ in range(2)]
                xg = [sb(f"xg{i}", [128, CR, D], BF16, scope=sc) for i in range(2)]; t_xg = [kb.tok("xg") for _ in range(2)]
                xeT = sb("xeT", [128, 8, C], BF16, scope=sc); t_xeT = kb.tok("xeT")
                aT = sb("aT", [128, 8, C], BF16, scope=sc); t_aT = kb.tok("aT")
                gt = [sb(f"gt{i}", [128, 512], scope=sc) for i in range(2)]; t_gt = [kb.tok("gt") for _ in range(2)]
                sg = [sb(f"sg{i}", [128, 512], scope=sc) for i in range(2)]; t_sg = [kb.tok("sg") for _ in range(2)]
                ut = [sb(f"ut{i}", [128, 512], scope=sc) for i in range(2)]; t_ut = [kb.tok("ut") for _ in range(2)]
                yo = [sb(f"yo{i}", [128, D], scope=sc) for i in range(2)]; t_yo = [kb.tok("yo") for _ in range(2)]
                pT = ps("pT", [128, 8, 128], BF16, scope=sc); t_pT = kb.tok("pT")
                pg = [ps(f"pg{i}", [128, 512], scope=sc) for i in range(2)]; t_pg = [kb.tok("pg") for _ in range(2)]
                pu = [ps(f"pu{i}", [128, 512], scope=sc) for i in range(2)]; t_pu = [kb.tok("pu") for _ in range(2)]
                py = ps("py", [128, 1024], scope=sc); t_py = kb.tok("py")
                pbt = pT[:, :, 0:NE]; t_pbt = t_pT

                dma("sp", lambda q: q.dma_start(out=braw[:, 0, :], in_=exp_b_gate[li]), writes=[t_b], key=t_b)
                dma("sp", lambda q: q.dma_start(out=braw[:, 1, :], in_=exp_b_up[li]), writes=[t_b], key=t_b, join=True)
                op("dve", lambda v: v.tensor_copy(out=brb[:], in_=braw[:]), reads=[t_b], writes=[t_b])
                for g in range(2):
                    def tr(pe):
                        for c in range(8):
                            r = pe.transpose(out=pT[:, c, 0:NE], in_=brb[:, g, c * 128:(c + 1) * 128], identity=identb[0:NE, 0:NE])
                        return r
                    op("pe", tr, reads=[t_b, t_const], writes=[t_pbt])
                    op("dve", lambda v: v.tensor_copy(out=bT[:, g, :, :], in_=pT[:, :, 0:NE]), reads=[t_pbt], writes=[t_b])

                def load_w(e):
                    b = e % 2
                    for (dst, td, src) in ((wg[b], t_wg[b], exp_w_gate), (wu[b], t_wu[b], exp_w_up), (wd[b], t_wd[b], exp_w_down)):
                        dma("pool", lambda q: q.dma_start(out=dst[:], in_=src[li, e].rearrange("(kc p) n -> p kc n", p=128)),
                            writes=[td], key=td)
                    dma("sp", lambda q: q.dma_start(out=bd[b][:], in_=exp_b_down[li, e].partition_broadcast(128)),
                        writes=[t_bd[b]], key=t_bd[b])

                def load_x(e):
                    b = e % 2
                    dma("sp", lambda q: q.dma_start(out=idx[b][:], in_=toklist[e * C:(e + 1) * C, :].rearrange("(p r) o -> p (r o)", p=128)),
                        reads=[t_tl], writes=[t_idx[b]], key=t_idx[b])
                    for r in range(CR):
                        dma("pool", lambda q: q.indirect_dma_start(
                            out=xg[b][:, r, :], out_offset=None, in_=x1bf[:, :],
                            in_offset=bass.IndirectOffsetOnAxis(ap=idx[b][:, r:r + 1], axis=0)),
                            reads=[t_idx[b], t_x1bf], writes=[t_xg[b]], key=t_xg[b], join=(r > 0))

                load_x(0)
                load_w(0)
                it = 0
                for e in range(NE):
                    b = e % 2
                    if e + 1 < NE:
                        load_x(e + 1)
                        load_w(e + 1)
                    for r in range(CR):
                        def tr(pe):
                            for c in range(8):
                                rr = pe.transpose(out=pT[:, c, :], in_=xg[b][:, r, c * 128:(c + 1) * 128], identity=identb[:])
                            return rr
                        op("pe", tr, reads=[t_xg[b], t_const], writes=[t_pT])
                        op("act", lambda a: a.copy(out=xeT[:, :, r * 128:(r + 1) * 128], in_=pT[:]), reads=[t_pT], writes=[t_xeT])
                    segs = [(s0, min(512, C - s0)) for s0 in range(0, C, 512)]
                    for fc in range(8):
                        for (s0, sn) in segs:
                            bb = it % 2
                            it += 1
                            for (pp, tp, w_, tw) in ((pg[bb], t_pg[bb], wg[b], t_wg[b]), (pu[bb], t_pu[bb], wu[b], t_wu[b])):
                                def mm(pe):
                                    for kc in range(8):
                                        rr = pe.matmul(pp[:, 0:sn], lhsT=w_[:, kc, fc * 128:(fc + 1) * 128],
                                                       rhs=xeT[:, kc, s0:s0 + sn], start=(kc == 0), stop=(kc == 7))
                                    return rr
                                op("pe", mm, reads=[tw, t_xeT], writes=[tp])
                            G_, S_, U_ = gt[bb][:, 0:sn], sg[bb][:, 0:sn], ut[bb][:, 0:sn]
                            op("dve", lambda v: v.tensor_scalar(out=G_, in0=pg[bb][:, 0:sn], scalar1=bT[:, 0, fc, e:e + 1], scalar2=7.0,
                                                                op0=ALU.add, op1=ALU.min), reads=[t_pg[bb], t_b], writes=[t_gt[bb]])
                            op("act", lambda a: a.activation(out=S_, in_=G_, func=AF.Silu, scale=1.702),
                               reads=[t_gt[bb]], writes=[t_sg[bb]])
                            op("dve", lambda v: v.tensor_scalar(out=U_, in0=pu[bb][:, 0:sn], scalar1=bT[:, 1, fc, e:e + 1], scalar2=7.0,
                                                                op0=ALU.add, op1=ALU.min), reads=[t_pu[bb], t_b], writes=[t_ut[bb]])
                            op("dve", lambda v: v.tensor_scalar(out=U_, in0=U_, scalar1=-7.0, scalar2=1.0 / 1.702,
                                                                op0=ALU.max, op1=ALU.mult), writes=[t_ut[bb]])
                            op("dve", lambda v: v.scalar_tensor_tensor(out=aT[:, fc, s0:s0 + sn], in0=U_, scalar=1.0 / 1.702, in1=S_,
                                                                       op0=ALU.add, op1=ALU.mult),
                               reads=[t_sg[bb], t_ut[bb]], writes=[t_aT])
                    for r in range(CR):
                        yb = (e * CR + r) % 2

                        def mmd(pe):
                            for hh in range(2):
                                for fc in range(8):
                                    rr = pe.matmul(py[:, hh * 512:(hh + 1) * 512], lhsT=aT[:, fc, r * 128:(r + 1) * 128],
                                                   rhs=wd[b][:, fc, hh * 512:(hh + 1) * 512], start=(fc == 0), stop=(fc == 7))
                            return rr
                        op("pe", mmd, reads=[t_aT, t_wd[b]], writes=[t_py])
                        op("dve", lambda v: v.tensor_tensor(out=yo[yb][:], in0=py[:], in1=bd[b][:], op=ALU.add),
                           reads=[t_py, t_bd[b]], writes=[t_yo[yb]])
                        ydst = yslots[e * C:(e + 1) * C, :].rearrange("(p r) d -> p r d", p=128)
                        dma("sp", lambda q: q.dma_start(out=ydst[:, r, :], in_=yo[yb][:]),
                            reads=[t_yo[yb]], writes=[t_ys], key=t_yo[yb], join=True)
                kb.barrier()

        def phase_combine_ln2_ple(li, gates, dests, t_route, xout, t_xout):
            with contextlib.ExitStack() as sc:
                wpg = sb("wpg", [128, 8, D], BF16, scope=sc); wpp = sb("wpp", [128, 2, D], BF16, scope=sc); t_w = kb.tok("wple")
                g_bc = sb("g_bc", [128, D], scope=sc); b_bc = sb("b_bc", [128, D], scope=sc)
                bpg = sb("bpg", [128, D], scope=sc); t_gb = kb.tok("gb")
                yk = [[sb(f"yk{i}_{k}", [128, D], scope=sc) for k in range(4)] for i in range(2)]
                t_yk = [[kb.tok("yk") for k in range(4)] for i in range(2)]
                xs = [sb(f"xs{i}", [128, D], scope=sc) for i in range(2)]; t_xs = [kb.tok("xs") for _ in range(2)]
                h = [sb(f"h{i}", [128, D], scope=sc) for i in range(2)]; t_h = [kb.tok("h") for _ in range(2)]
                x2 = [sb(f"x2_{i}", [128, D], scope=sc) for i in range(2)]; t_x2 = [kb.tok("x2") for _ in range(2)]
                x2b = [sb(f"x2b{i}", [128, D], BF16, scope=sc) for i in range(2)]; t_x2b = [kb.tok("x2b") for _ in range(2)]
                x2T = [sb(f"x2T{i}", [128, 8, 128], BF16, scope=sc) for i in range(2)]; t_x2T = [kb.tok("x2T") for _ in range(2)]
                pp32 = [sb(f"pp32_{i}", [128, PLE], scope=sc) for i in range(2)]; t_pp = [kb.tok("pp") for _ in range(2)]
                ppb = [sb(f"ppb{i}", [128, PLE], BF16, scope=sc) for i in range(2)]; t_ppb = [kb.tok("ppb") for _ in range(2)]
                ppT = [sb(f"ppT{i}", [128, 2, 128], BF16, scope=sc) for i in range(2)]; t_ppT = [kb.tok("ppT") for _ in range(2)]
                stat = [sb(f"stat{i}", [128, 16], scope=sc) for i in range(2)]; t_stat = [kb.tok("stat") for _ in range(2)]
                gs = [sb(f"gs{i}", [128, D], scope=sc) for i in range(2)]; t_gs = [kb.tok("gs") for _ in range(2)]
                xo = [sb(f"xo{i}", [128, D], scope=sc) for i in range(2)]; t_xo = [kb.tok("xo") for _ in range(2)]
                pT = ps("pT", [128, 8, 128], BF16, scope=sc); t_pT = kb.tok("pT")
                pT2 = ps("pT2", [128, 2, 128], BF16, scope=sc); t_pT2 = kb.tok("pT2")
                pgt = ps("pgt", [128, D], scope=sc); t_pgt = kb.tok("pgt")
                ppj = ps("ppj", [128, D], scope=sc); t_ppj = kb.tok("ppj")
                dma("pool", lambda q: q.dma_start(out=wpg[:], in_=ple_w_gate[li].rearrange("(kc p) n -> p kc n", p=128)), writes=[t_w], key=t_w)
                dma("pool", lambda q: q.dma_start(out=wpp[:], in_=ple_w_proj[li].rearrange("(kc p) n -> p kc n", p=128)), writes=[t_w], key=t_w, join=True)
                dma("sp", lambda q: q.dma_start(out=g_bc[:], in_=ln2_g[li].partition_broadcast(128)), writes=[t_gb], key=t_gb)
                dma("sp", lambda q: q.dma_start(out=b_bc[:], in_=ln2_b[li].partition_broadcast(128)), writes=[t_gb], key=t_gb, join=True)
                dma("sp", lambda q: q.dma_start(out=bpg[:], in_=ple_b_gate[li].partition_broadcast(128)), writes=[t_gb], key=t_gb, join=True)
                for i in range(NT):
                    b = i % 2
                    dma("sp", lambda q: q.dma_start(out=xs[b][:], in_=x1res[i * 128:(i + 1) * 128, :]),
                        reads=[t_x1res], writes=[t_xs[b]], key=t_xs[b])
                    dma("sp", lambda q: q.dma_start(out=pp32[b][:], in_=p_in[li, i * 128:(i + 1) * 128, :]),
                        writes=[t_pp[b]], key=t_pp[b])
                    for k in range(4):
                        dma("pool", lambda q: q.indirect_dma_start(
                            out=yk[b][k][:], out_offset=None, in_=yslots[:, :],
                            in_offset=bass.IndirectOffsetOnAxis(ap=dests[:, i, k:k + 1], axis=0)),
                            reads=[t_route, t_ys], writes=[t_yk[b][k]], key=t_yk[b][k])
                    op("act", lambda a: a.mul(out=h[b][:], in_=xs[b][:], mul=ALPHA), reads=[t_xs[b]], writes=[t_h[b]])
                    for k in range(4):
                        op("dve", lambda v: v.scalar_tensor_tensor(out=h[b][:], in0=yk[b][k][:], scalar=gates[:, i, k:k + 1],
                                                                   in1=h[b][:], op0=ALU.mult, op1=ALU.add),
                           reads=[t_yk[b][k], t_route], writes=[t_h[b]])
                    layer_norm_tile(h[b], g_bc, b_bc, x2[b], stat[b], t_h[b], t_x2[b], t_stat[b], t_gb)
                    op("act", lambda a: a.copy(out=x2b[b][:], in_=x2[b][:]), reads=[t_x2[b]], writes=[t_x2b[b]])
                    op("act", lambda a: a.copy(out=ppb[b][:], in_=pp32[b][:]), reads=[t_pp[b]], writes=[t_ppb[b]])

                    def tr(pe):
                        for c in range(8):
                            r = pe.transpose(out=pT[:, c, :], in_=x2b[b][:, c * 128:(c + 1) * 128], identity=identb[:])
                        return r
                    op("pe", tr, reads=[t_x2b[b], t_const], writes=[t_pT])
                    op("act", lambda a: a.copy(out=x2T[b][:], in_=pT[:]), reads=[t_pT], writes=[t_x2T[b]])

                    def tr2(pe):
                        for c in range(2):
                            r = pe.transpose(out=pT2[:, c, :], in_=ppb[b][:, c * 128:(c + 1) * 128], identity=identb[:])
                        return r
                    op("pe", tr2, reads=[t_ppb[b], t_const], writes=[t_pT2])
                    op("act", lambda a: a.copy(out=ppT[b][:], in_=pT2[:]), reads=[t_pT2], writes=[t_ppT[b]])

                    def mmg(pe):
                        for hh in range(2):
                            for c in range(8):
                                r = pe.matmul(pgt[:, hh * 512:(hh + 1) * 512], lhsT=x2T[b][:, c, :], rhs=wpg[:, c, hh * 512:(hh + 1) * 512],
                                              start=(c == 0), stop=(c == 7))
                        return r
                    op("pe", mmg, reads=[t_x2T[b], t_w], writes=[t_pgt])

                    def mmp(pe):
                        for hh in range(2):
                            for c in range(2):
                                r = pe.matmul(ppj[:, hh * 512:(hh + 1) * 512], lhsT=ppT[b][:, c, :], rhs=wpp[:, c, hh * 512:(hh + 1) * 512],
                                              start=(c == 0), stop=(c == 1))
                        return r
                    op("pe", mmp, reads=[t_ppT[b], t_w], writes=[t_ppj])
                    op("dve", lambda v: v.tensor_tensor(out=gs[b][:], in0=pgt[:], in1=bpg[:], op=ALU.add),
                       reads=[t_pgt, t_gb], writes=[t_gs[b]])
                    op("act", lambda a: a.activation(out=gs[b][:], in_=gs[b][:], func=AF.Sigmoid), writes=[t_gs[b]])
                    op("dve", lambda v: v.tensor_tensor(out=xo[b][:], in0=ppj[:], in1=gs[b][:], op=ALU.mult),
                       reads=[t_ppj, t_gs[b]], writes=[t_xo[b]])
                    op("pool", lambda g: g.tensor_tensor(out=xo[b][:], in0=xo[b][:], in1=x2[b][:], op=ALU.add),
                       reads=[t_x2[b]], writes=[t_xo[b]])
                    dma("sp", lambda q: q.dma_start(out=xout[i * 128:(i + 1) * 128, :], in_=xo[b][:]),
                        reads=[t_xo[b]], writes=[t_xout], key=t_xo[b], join=True)
                kb.barrier()

        gates = sb("gates", [128, NT, 4])
        dests = sb("dests", [128, NT, 4], I32)
        t_route = kb.tok("route")
        xin, t_xin = x_in, kb.tok("xin")
        nl = len(layers)
        for n, li in enumerate(layers):
            kind, j = KINDS[li], JIDX[li]
            last = (n == nl - 1)
            xout, t_xout = (out_d, t_out) if last else (xres[n % 2], t_xres[n % 2])
            with contextlib.ExitStack() as lsc:
                t_mT = kb.tok("mT")
                if kind == 0:
                    mT = sb("mT", [128, 8, T], BF16, scope=lsc)
                    with contextlib.ExitStack() as asc:
                        xT = sb("xT", [128, 8, T], BF16, scope=asc); t_xT = kb.tok("xT")
                        phase_xT(xin, t_xin, xT, t_xT)
                        phase_conv(j, xT, t_xT, mT, t_mT)
                    w_out_d = conv_w_out[j]
                else:
                    ncum = sb("ncum", [128, NT, 16], scope=lsc)
                    refB = sb("refB", [16, 3, T], BF16, scope=lsc)
                    t_cum = kb.tok("cum")
                    w_in_d = sb_w_in[0] if kind == 1 else fox_w_in[0]
                    with contextlib.ExitStack() as asc:
                        xT = sb("xT", [128, 8, T], BF16, scope=asc); t_xT = kb.tok("xT")
                        phase_xT(xin, t_xin, xT, t_xT)
                        phase_qkv(kind, w_in_d, xT, t_xT, ncum, refB, t_cum)
                    mT = sb("mT", [128, 8, T], BF16, scope=lsc)
                    phase_attn(kind, mT, t_mT, ncum, refB, t_cum)
                    w_out_d = sb_w_out[0] if kind == 1 else fox_w_out[0]
                phase_outproj_ln1_route(li, w_out_d, xin, t_xin, mT, t_mT, gates, dests, t_route)
            phase_experts(li)
            phase_combine_ln2_ple(li, gates, dests, t_route, xout, t_xout)
            xin, t_xin = xout, t_xout
        kb.barrier()
    return nc


_NAMES = ["x", "p", "conv_w_in", "conv_w", "conv_w_out", "sb_w_in", "sb_w_out", "fox_w_in", "fox_b_f", "fox_w_out",
          "ln1_g", "ln1_b", "ln2_g", "ln2_b", "router_w", "router_b", "exp_w_gate", "exp_b_gate", "exp_w_up",
          "exp_b_up", "exp_w_down", "exp_b_down", "ple_w_proj", "ple_w_gate", "ple_b_gate"]


def kernel(**inputs):
    B = inputs["x"].shape[0]
    T = inputs["x"].shape[1]
    nc = build_program(T=T)
    in_maps = []
    shared = {k: np.ascontiguousarray(inputs[k], dtype=np.float32) for k in _NAMES if k not in ("x", "p")}
    for b in range(B):
        m = dict(shared)
        m["x"] = np.ascontiguousarray(inputs["x"][b], dtype=np.float32)
        m["p"] = np.ascontiguousarray(inputs["p"][:, b], dtype=np.float32)
        in_maps.append(m)
    res = run_bass_kernel_spmd(nc, in_maps, core_ids=list(range(B)))
    return np.stack([np.asarray(r["out"]) for r in res.results], axis=0).astype(np.float32)
```

```python
import contextlib
import numpy as np
import concourse.bass as bass
import concourse.mybir as mybir
from concourse.bass_utils import run_bass_kernel_spmd

F32 = mybir.dt.float32
BF16 = mybir.dt.bfloat16
I32 = mybir.dt.int32
AF = mybir.ActivationFunctionType
ALU = mybir.AluOpType
AX = mybir.AxisListType

D = 1024
NE = 32
PLE = 256
ALPHA = 8 ** 0.25
LN_EPS = 1e-5
KINDS = (0, 1, 2, 0)
JIDX = (0, 0, 0, 1)


class Tok:
    __slots__ = ("name", "w", "r", "dkey")

    def __init__(self, name):
        self.name = name
        self.w = {}
        self.r = {}
        self.dkey = None


class KB:
    def __init__(self, nc, es):
        self.nc = nc
        self.es = es
        self.eng = dict(pe=nc.tensor, dve=nc.vector, act=nc.scalar, pool=nc.gpsimd, sp=nc.sync)
        self.sems, self.cnt, self.isdma = {}, {}, {}
        self.waited = {k: {} for k in self.eng}
        for k in self.eng:
            self._newsem(k, False)
        self.ntok = 0
        self.free_slots = []
        for i in range(64):
            self._newsem(f"d{i}", True)
            self.free_slots.append(f"d{i}")
        self.live = []
        self.mark = {}

    def _newsem(self, key, isdma):
        self.sems[key] = self.es.enter_context(self.nc.semaphore("s_" + key))
        self.cnt[key] = 0
        self.isdma[key] = isdma

    def tok(self, name="t"):
        self.ntok += 1
        return Tok(f"{name}{self.ntok}")

    def _wait(self, e, evs):
        need = {}
        for ev in evs:
            for k, v in ev.items():
                if self.isdma[k]:
                    if v <= self.mark.get(k, 0):
                        continue
                    v = self.cnt[k]
                if v > need.get(k, 0):
                    need[k] = v
        for k, v in need.items():
            if self.waited[e].get(k, 0) >= v:
                continue
            self.eng[e].wait_ge(self.sems[k], v)
            self.waited[e][k] = v

    def op(self, e, emit, reads=(), writes=()):
        evs = [t.w for t in reads] + [t.w for t in writes] + [t.r for t in writes]
        self._wait(e, evs)
        inst = emit(self.eng[e])
        self.cnt[e] += 1
        inst.then_inc(self.sems[e], 1)
        v = self.cnt[e]
        for t in reads:
            t.r[e] = v
        for t in writes:
            t.w = {e: v}
            t.r = {}
        return inst

    def dma(self, q, emit, reads=(), writes=(), key=None, join=False):
        evs = [t.w for t in reads] + [t.r for t in writes]
        if not join:
            evs += [t.w for t in writes]
        self._wait(q, evs)
        if key.dkey is None:
            key.dkey = self.free_slots.pop(0)
            self.live.append(key)
        k = key.dkey
        inst = emit(self.eng[q])
        self.cnt[k] += 16
        inst.then_inc(self.sems[k], 16)
        v = self.cnt[k]
        for t in reads:
            t.r[k] = v
        for t in writes:
            if join:
                t.w[k] = v
            else:
                t.w = {k: v}
                t.r = {}
        return inst

    def barrier(self):
        allev = [dict(self.cnt)]
        for e in self.eng:
            self._wait(e, allev)
        self.mark = {k: v for k, v in self.cnt.items() if self.isdma[k]}
        for t in self.live:
            self.free_slots.append(t.dkey)
            t.dkey = None
        self.live = []


def _cfg_caps(T):
    return {4096: 640, 512: 128, 1024: 256}[T]


def build_program(T=4096, layers=(0, 1, 2, 3), dbg=False):
    C = _cfg_caps(T)
    NT = T // 128
    NQ = T // 512
    CR = C // 128
    NSLOT = NE * C
    nc = bass.Bass("TRN2", target_bir_lowering=False)

    def din(name, shape, dt=F32):
        return nc.dram_tensor(name, list(shape), dt, kind="ExternalInput").ap()

    def dscr(name, shape, dt=F32):
        return nc.dram_tensor(name, list(shape), dt).ap()

    x_in = din("x", [T, D])
    p_in = din("p", [4, T, PLE])
    conv_w_in = din("conv_w_in", [2, D, 3 * D])
    conv_w = din("conv_w", [2, 3, D])
    conv_w_out = din("conv_w_out", [2, D, D])
    sb_w_in = din("sb_w_in", [1, D, 3 * D])
    sb_w_out = din("sb_w_out", [1, D, D])
    fox_w_in = din("fox_w_in", [1, D, 3 * D + 16])
    fox_b_f = din("fox_b_f", [1, 16])
    fox_w_out = din("fox_w_out", [1, D, D])
    ln1_g = din("ln1_g", [4, D]); ln1_b = din("ln1_b", [4, D])
    ln2_g = din("ln2_g", [4, D]); ln2_b = din("ln2_b", [4, D])
    router_w = din("router_w", [4, D, NE]); router_b = din("router_b", [4, NE])
    exp_w_gate = din("exp_w_gate", [4, NE, D, D]); exp_b_gate = din("exp_b_gate", [4, NE, D])
    exp_w_up = din("exp_w_up", [4, NE, D, D]); exp_b_up = din("exp_b_up", [4, NE, D])
    exp_w_down = din("exp_w_down", [4, NE, D, D]); exp_b_down = din("exp_b_down", [4, NE, D])
    ple_w_proj = din("ple_w_proj", [4, PLE, D]); ple_w_gate = din("ple_w_gate", [4, D, D])
    ple_b_gate = din("ple_b_gate", [4, D])
    out_d = nc.dram_tensor("out", [T, D], F32, kind="ExternalOutput").ap()

    xres = [dscr("xres0", [T, D]), dscr("xres1", [T, D])]
    x1res = dscr("x1res", [T, D])
    x1bf = dscr("x1bf", [T, D], BF16)
    yslots = dscr("yslots", [NSLOT + 128, D])
    toklist = dscr("toklist", [NSLOT + 128, 1], I32)
    qT_d = dscr("qT_d", [8, 128, T], BF16)
    kT_d = dscr("kT_d", [8, 128, T], BF16)
    v_d = dscr("v_d", [T, D], BF16)
    nrow_d = dscr("nrow_d", [16, 3, T], BF16)
    dbg_d = None
    if dbg:
        dbg_d = nc.dram_tensor("dbg", [T, D], F32, kind="ExternalOutput").ap()

    es = contextlib.ExitStack()
    with es:
        kb = KB(nc, es)
        op, dma = kb.op, kb.dma

        uid = [0]

        def sb(name, shape, dt=F32, scope=es):
            uid[0] += 1
            return scope.enter_context(nc.sbuf_tensor(f"{name}_{uid[0]}", list(shape), dt))

        def ps(name, shape, dt=F32, scope=es):
            uid[0] += 1
            return scope.enter_context(nc.psum_tensor(f"{name}_{uid[0]}", list(shape), dt))

        t_const = kb.tok("const")
        ident32 = sb("ident32", [128, 128])
        identb = sb("identb", [128, 128], BF16)
        ones_b = sb("ones_b", [128, 128], BF16)
        tri_lt = sb("tri_lt", [128, 128], BF16)
        ntri_ge = sb("ntri_ge", [128, 128], BF16)
        nones_b = sb("nones_b", [128, 128], BF16)
        mask_lt = sb("mask_lt", [128, 128], BF16)
        mask_le = sb("mask_le", [128, 128], BF16)
        c_one = sb("c_one", [128, 1])
        c_eps = sb("c_eps", [128, 1])
        c_nhalf = sb("c_nhalf", [128, 1])
        tokid = sb("tokid", [128, NT], I32)
        ecol = sb("ecol", [128, NE])
        zrow = sb("zrow", [128, D])
        zi = sb("zi", [128, (NSLOT + 128) // 128], I32)
        tmp32 = sb("tmp32", [128, 128])

        def mk_tri(dst, cmp, mult_p, mult_f, fillv, inv):
            def e1(g):
                return g.memset(tmp32[:], inv)
            op("pool", e1, writes=[t_const])

            def e2(g):
                return g.affine_select(out=tmp32[:], in_=tmp32[:], pattern=[[mult_f, 128]], compare_op=cmp,
                                       fill=fillv, base=0, channel_multiplier=mult_p)
            op("pool", e2, writes=[t_const])
            op("dve", lambda v: v.tensor_copy(out=dst[:], in_=tmp32[:]), reads=[t_const], writes=[t_const])

        mk_tri(ident32, ALU.not_equal, 1, -1, 1.0, 0.0)
        mk_tri(identb, ALU.not_equal, 1, -1, 1.0, 0.0)
        mk_tri(tri_lt, ALU.is_gt, -1, 1, 0.0, 1.0)
        mk_tri(mask_lt, ALU.is_gt, -1, 1, 0.0, 1.0)
        mk_tri(mask_le, ALU.is_ge, -1, 1, 0.0, 1.0)
        mk_tri(ntri_ge, ALU.is_ge, 1, -1, 0.0, -1.0)
        zeros_b = sb("zeros_b", [128, 128], BF16)
        sel0 = sb("sel0", [128, 128], BF16)
        mk_tri(sel0, ALU.is_equal, 1, 0, 0.0, 1.0)
        negmask_gt = sb("negmask_gt", [128, 128], BF16)
        mk_tri(negmask_gt, ALU.is_gt, 1, -1, 0.0, -30000.0)
        selh = sb("selh", [16, 16, 128], BF16)
        selt = sb("selt", [16, 16, 128])
        op("pool", lambda g: g.memset(selt[:], 1.0), writes=[t_const])
        op("pool", lambda g: g.affine_select(out=selt[:], in_=selt[:], pattern=[[-1, 16], [0, 128]], compare_op=ALU.is_equal,
                                             fill=0.0, base=0, channel_multiplier=1), writes=[t_const])
        op("dve", lambda v: v.tensor_copy(out=selh[:], in_=selt[:]), reads=[t_const], writes=[t_const])
        zsrc = sb("zsrc", [128, 512], BF16)
        op("pool", lambda g: g.memset(zsrc[:], 0.0), writes=[t_const])
        op("pool", lambda g: g.memset(zeros_b[:], 0.0), writes=[t_const])
        op("pool", lambda g: g.memset(ones_b[:], 1.0), writes=[t_const])
        op("pool", lambda g: g.memset(nones_b[:], -1.0), writes=[t_const])
        op("pool", lambda g: g.memset(c_one[:], 1.0), writes=[t_const])
        op("pool", lambda g: g.memset(c_eps[:], LN_EPS), writes=[t_const])
        op("pool", lambda g: g.memset(c_nhalf[:], -0.5), writes=[t_const])
        op("pool", lambda g: g.memset(zrow[:], 0.0), writes=[t_const])
        op("pool", lambda g: g.memset(zi[:], 0), writes=[t_const])
        op("pool", lambda g: g.iota(tokid[:], pattern=[[128, NT]], base=0, channel_multiplier=1), writes=[t_const])
        op("pool", lambda g: g.iota(ecol[:], pattern=[[C, NE]], base=0, channel_multiplier=0,
                                    allow_small_or_imprecise_dtypes=True), writes=[t_const])
        t_ys = kb.tok("yslots")
        t_tl = kb.tok("toklist")
        dma("sp", lambda q: q.dma_start(out=yslots[NSLOT:NSLOT + 128, :], in_=zrow[:]), reads=[t_const],
            writes=[t_ys], key=t_ys, join=True)

        t_xres = [kb.tok("xres0"), kb.tok("xres1")]
        t_x1res = kb.tok("x1res")
        t_x1bf = kb.tok("x1bf")
        t_qkv = kb.tok("qkv")
        t_out = kb.tok("out")

        def layer_norm_tile(h, g_bc, b_bc, xo, stat, t_h, t_xo, t_stat, t_gb, eng2="pool"):
            def s1(v):
                v.bn_stats(out=stat[:, 0:6], in_=h[:, 0:512])
                return v.bn_stats(out=stat[:, 6:12], in_=h[:, 512:1024])
            op("dve", s1, reads=[t_h], writes=[t_stat])
            op("dve", lambda v: v.bn_aggr(out=stat[:, 12:14], in_=stat[:, 0:12]), reads=[t_stat], writes=[t_stat])
            op("pool", lambda g: g.tensor_tensor(out=stat[:, 14:15], in0=stat[:, 13:14], in1=c_eps[:], op=ALU.add),
               reads=[t_stat, t_const], writes=[t_stat])
            op("pool", lambda g: g.tensor_tensor(out=stat[:, 15:16], in0=stat[:, 14:15], in1=c_nhalf[:], op=ALU.pow),
               reads=[t_stat, t_const], writes=[t_stat])
            op("dve", lambda v: v.tensor_scalar(out=xo[:], in0=h[:], scalar1=stat[:, 12:13], scalar2=stat[:, 15:16],
                                                op0=ALU.subtract, op1=ALU.mult),
               reads=[t_h, t_stat], writes=[t_xo])
            op(eng2, lambda g: g.tensor_tensor(out=xo[:], in0=xo[:], in1=g_bc[:], op=ALU.mult),
               reads=[t_gb], writes=[t_xo])
            op(eng2, lambda g: g.tensor_tensor(out=xo[:], in0=xo[:], in1=b_bc[:], op=ALU.add),
               reads=[t_gb], writes=[t_xo])

        def bcast_load(dst, src_row, t_dst, q="sp"):
            n = dst.shape[-1] if hasattr(dst, "shape") else None
            dma(q, lambda e: e.dma_start(out=dst[:], in_=src_row.partition_broadcast(128)), writes=[t_dst], key=t_dst)

        def phase_xT(xin, t_xin, xT, t_xT):
            with contextlib.ExitStack() as sc:
                xs = [sb(f"xs{i}", [128, D], scope=sc) for i in range(2)]
                xb = [sb(f"xb{i}", [128, D], BF16, scope=sc) for i in range(2)]
                pT = [ps(f"pT{i}", [128, 8, 128], BF16, scope=sc) for i in range(2)]
                t_xs = [kb.tok("xs") for _ in range(2)]
                t_xb = [kb.tok("xb") for _ in range(2)]
                t_pT = [kb.tok("pT") for _ in range(2)]
                for i in range(NT):
                    b = i % 2
                    dma("sp", lambda q: q.dma_start(out=xs[b][:], in_=xin[i * 128:(i + 1) * 128, :]),
                        reads=[t_xin], writes=[t_xs[b]], key=t_xs[b])
                    op("act", lambda a: a.copy(out=xb[b][:], in_=xs[b][:]), reads=[t_xs[b]], writes=[t_xb[b]])

                    def tr(pe):
                        for c in range(8):
                            r = pe.transpose(out=pT[b][:, c, :], in_=xb[b][:, c * 128:(c + 1) * 128], identity=identb[:])
                        return r
                    op("pe", tr, reads=[t_xb[b], t_const], writes=[t_pT[b]])
                    op("dve", lambda v: v.tensor_copy(out=xT[:, :, i * 128:(i + 1) * 128], in_=pT[b][:]),
                       reads=[t_pT[b]], writes=[t_xT])
                kb.barrier()

        def phase_conv(j, xT, t_xT, mT, t_mT):
            with contextlib.ExitStack() as sc:
                wc = [sb(f"wc{i}", [128, 8, 384], BF16, scope=sc) for i in range(2)]
                t_wc = [kb.tok("wc") for _ in range(2)]
                cw = sb("cw", [128, 8, 3], scope=sc)
                cwr = sb("cwr", [3, D], scope=sc)
                cwb = sb("cwb", [3, D], BF16, scope=sc)
                t_cw = kb.tok("cw")
                pcw = ps("pcw", [128, 8, 4], BF16, scope=sc)
                t_pcw = kb.tok("pcw")
                pB = [ps(f"pB{i}", [128, 512], scope=sc) for i in range(2)]
                pC = [ps(f"pC{i}", [128, 512], scope=sc) for i in range(2)]
                pH = [ps(f"pH{i}", [128, 512], scope=sc) for i in range(2)]
                t_pB = [kb.tok("pB") for _ in range(2)]
                t_pC = [kb.tok("pC") for _ in range(2)]
                t_pH = [kb.tok("pH") for _ in range(2)]
                ctmp = [sb(f"ctmp{i}", [128, 512], scope=sc) for i in range(2)]
                t_ct = [kb.tok("ct") for _ in range(2)]
                u = [sb(f"u{i}", [128, 514], scope=sc) for i in range(2)]
                t_u = [kb.tok("u") for _ in range(2)]
                cv = [sb(f"cv{i}", [128, 512], scope=sc) for i in range(2)]
                t_cv = [kb.tok("cv") for _ in range(2)]
                cwlo = sb("cwlo", [3, D], BF16, scope=sc)
                cwt = sb("cwt", [3, D], scope=sc)
                dma("sp", lambda q: q.dma_start(out=cwr[:], in_=conv_w[j]), writes=[t_cw], key=t_cw)
                op("dve", lambda v: v.tensor_copy(out=cwb[:], in_=cwr[:]), reads=[t_cw], writes=[t_cw])
                op("dve", lambda v: v.tensor_copy(out=cwt[:], in_=cwb[:]), reads=[t_cw], writes=[t_cw])
                op("dve", lambda v: v.tensor_tensor(out=cwt[:], in0=cwr[:], in1=cwt[:], op=ALU.subtract), writes=[t_cw])
                op("dve", lambda v: v.tensor_copy(out=cwlo[:], in_=cwt[:]), writes=[t_cw])
                for part, src in ((0, cwb), (1, cwlo)):
                    def tr(pe):
                        for c in range(8):
                            r = pe.transpose(out=pcw[:, c, 0:3], in_=src[:, c * 128:(c + 1) * 128], identity=identb[0:3, 0:3])
                        return r
                    op("pe", tr, reads=[t_cw, t_const], writes=[t_pcw])
                    if part == 0:
                        op("dve", lambda v: v.tensor_copy(out=cw[:], in_=pcw[:, :, 0:3]), reads=[t_pcw], writes=[t_cw])
                    else:
                        op("dve", lambda v: v.tensor_tensor(out=cw[:], in0=cw[:], in1=pcw[:, :, 0:3], op=ALU.add),
                           reads=[t_pcw], writes=[t_cw])
                wsrc = conv_w_in[j].rearrange("(kc p) n -> p kc n", p=128)

                def load_w(c):
                    b = c % 2
                    for g in range(3):
                        dma("pool", lambda q: q.dma_start(out=wc[b][:, :, g * 128:(g + 1) * 128],
                                                          in_=wsrc[:, :, g * 1024 + c * 128:g * 1024 + (c + 1) * 128]),
                            writes=[t_wc[b]], key=t_wc[b], join=(g > 0))
                load_w(0)
                it = 0
                for c in range(8):
                    if c + 1 < 8:
                        load_w(c + 1)
                    wb_ = wc[c % 2]
                    for tq in range(NQ):
                        b = it % 2
                        it += 1
                        for (pp, tp, g) in ((pB[b], t_pB[b], 0), (pC[b], t_pC[b], 1), (pH[b], t_pH[b], 2)):
                            def mm(pe):
                                for kc in range(8):
                                    r = pe.matmul(pp[:], lhsT=wb_[:, kc, g * 128:(g + 1) * 128],
                                                  rhs=xT[:, kc, tq * 512:(tq + 1) * 512], start=(kc == 0), stop=(kc == 7))
                                return r
                            op("pe", mm, reads=[t_wc[c % 2], t_xT], writes=[tp])
                        op("act", lambda a: a.copy(out=ctmp[b][:], in_=pC[b][:]), reads=[t_pC[b]], writes=[t_ct[b]])
                        if tq == 0:
                            op("pool", lambda g_: g_.memset(u[b][:, 0:2], 0.0), writes=[t_u[b]])
                        else:
                            op("pool", lambda g_: g_.tensor_copy(out=u[b][:, 0:2], in_=u[1 - b][:, 512:514]),
                               reads=[t_u[1 - b]], writes=[t_u[b]])
                        op("dve", lambda v: v.tensor_tensor(out=u[b][:, 2:514], in0=ctmp[b][:], in1=pH[b][:], op=ALU.mult),
                           reads=[t_ct[b], t_pH[b]], writes=[t_u[b]])
                        op("dve", lambda v: v.tensor_scalar(out=cv[b][:], in0=u[b][:, 2:514], scalar1=cw[:, c, 2:3],
                                                            scalar2=None, op0=ALU.mult),
                           reads=[t_u[b], t_cw], writes=[t_cv[b]])
                        op("dve", lambda v: v.scalar_tensor_tensor(out=cv[b][:], in0=u[b][:, 1:513], scalar=cw[:, c, 1:2],
                                                                   in1=cv[b][:], op0=ALU.mult, op1=ALU.add),
                           reads=[t_u[b], t_cw], writes=[t_cv[b]])
                        op("dve", lambda v: v.scalar_tensor_tensor(out=cv[b][:], in0=u[b][:, 0:512], scalar=cw[:, c, 0:1],
                                                                   in1=cv[b][:], op0=ALU.mult, op1=ALU.add),
                           reads=[t_u[b], t_cw], writes=[t_cv[b]])
                        op("dve", lambda v: v.tensor_tensor(out=mT[:, c, tq * 512:(tq + 1) * 512], in0=cv[b][:],
                                                            in1=pB[b][:], op=ALU.mult),
                           reads=[t_cv[b], t_pB[b]], writes=[t_mT])
                kb.barrier()

        def phase_qkv(kind, w_in_d, xT, t_xT, ncum, refB, t_cum):
            with contextlib.ExitStack() as sc:
                wsrc = w_in_d.rearrange("(kc p) n -> p kc n", p=128)
                wq = [sb(f"wq{i}", [128, 8, 512], BF16, scope=sc) for i in range(2)]; t_wq = [kb.tok("wq") for _ in range(2)]
                wv = sb("wv", [128, 8, D], BF16, scope=sc); t_wv = kb.tok("wv")
                stg = [sb(f"stg{i}", [128, T], BF16, scope=sc) for i in range(2)]; t_stg = [kb.tok("stg") for _ in range(2)]
                vst = [sb(f"vst{i}", [128, D], BF16, scope=sc) for i in range(2)]; t_vst = [kb.tok("vst") for _ in range(2)]
                pq = [ps(f"pq{i}", [128, 512], scope=sc) for i in range(2)]; t_pq = [kb.tok("pq") for _ in range(2)]
                pv = [ps(f"pv{i}", [128, 512], scope=sc) for i in range(2)]; t_pv = [kb.tok("pv") for _ in range(2)]
                dma("pool", lambda q: q.dma_start(out=wv[:], in_=wsrc[:, :, 2048:3072]), writes=[t_wv], key=t_wv)

                def load_g(g):
                    dma("pool", lambda q: q.dma_start(out=wq[g % 2][:], in_=wsrc[:, :, g * 512:(g + 1) * 512]),
                        writes=[t_wq[g % 2]], key=t_wq[g % 2])
                load_g(0)
                it = 0
                for g in range(4):
                    if g + 1 < 4:
                        load_g(g + 1)
                    for cc in range(4):
                        c16 = g * 4 + cc
                        sbuf_ = stg[c16 % 2]; ts_ = t_stg[c16 % 2]
                        for tq in range(NQ):
                            b = it % 2
                            it += 1

                            def mm(pe):
                                for kc in range(8):
                                    r = pe.matmul(pq[b][:], lhsT=wq[g % 2][:, kc, cc * 128:(cc + 1) * 128],
                                                  rhs=xT[:, kc, tq * 512:(tq + 1) * 512], start=(kc == 0), stop=(kc == 7))
                                return r
                            op("pe", mm, reads=[t_wq[g % 2], t_xT], writes=[t_pq[b]])
                            scl = 0.125 if c16 < 8 else 1.0
                            if it % 2 == 0:
                                op("act", lambda a: a.mul(out=sbuf_[:, tq * 512:(tq + 1) * 512], in_=pq[b][:], mul=scl),
                                   reads=[t_pq[b]], writes=[ts_])
                            else:
                                op("dve", lambda v: v.tensor_scalar(out=sbuf_[:, tq * 512:(tq + 1) * 512], in0=pq[b][:], scalar1=scl,
                                                                    scalar2=None, op0=ALU.mult), reads=[t_pq[b]], writes=[ts_])
                        dst = qT_d[c16] if c16 < 8 else kT_d[c16 - 8]
                        dma("sp", lambda q: q.dma_start(out=dst, in_=sbuf_[:]), reads=[ts_], writes=[t_qkv], key=ts_, join=True)
                for i in range(NT):
                    b = i % 2
                    for hh in range(2):
                        def mmv(pe):
                            for kc in range(8):
                                r = pe.matmul(pv[hh][:], lhsT=xT[:, kc, i * 128:(i + 1) * 128],
                                              rhs=wv[:, kc, hh * 512:(hh + 1) * 512], start=(kc == 0), stop=(kc == 7))
                            return r
                        op("pe", mmv, reads=[t_xT, t_wv], writes=[t_pv[hh]])
                        if hh == 0:
                            op("act", lambda a: a.copy(out=vst[b][:, 0:512], in_=pv[hh][:]), reads=[t_pv[hh]], writes=[t_vst[b]])
                        else:
                            op("dve", lambda v: v.tensor_copy(out=vst[b][:, 512:1024], in_=pv[hh][:]), reads=[t_pv[hh]], writes=[t_vst[b]])
                    dma("sp", lambda q: q.dma_start(out=v_d[i * 128:(i + 1) * 128, :], in_=vst[b][:]), reads=[t_vst[b]],
                        writes=[t_qkv], key=t_vst[b], join=True)
                if kind == 2:
                    wf = sb("wf", [128, 8, 16], BF16, scope=sc); t_wf = kb.tok("wf")
                    bf_bc = sb("bf_bc", [128, 16], scope=sc)
                    basef = sb("basef", [128, 16], scope=sc); t_bf = kb.tok("basef")
                    fl = [sb(f"fl{i}", [128, 64], scope=sc) for i in range(2)]; t_fl = [kb.tok("fl") for _ in range(2)]
                    flb = [sb(f"flb{i}", [128, 32], BF16, scope=sc) for i in range(2)]; t_flb = [kb.tok("flb") for _ in range(2)]
                    pf = ps("pf", [128, 16], scope=sc); t_pf = kb.tok("pf")
                    pc = ps("pc", [128, 2, 16], scope=sc); t_pc = kb.tok("pc")
                    ptr = ps("ptr", [128, 3, 128], BF16, scope=sc); t_ptr = kb.tok("ptr")
                    nsp = [sb(f"nsp{i}", [128, 3, 16], BF16, scope=sc) for i in range(2)]; t_nsp = [kb.tok("nsp") for _ in range(2)]
                    nr = [sb(f"nr{i}", [128, 48], scope=sc) for i in range(2)]
                    dma("pool", lambda q: q.dma_start(out=wf[:], in_=wsrc[:, :, 3072:3088]), writes=[t_wf], key=t_wf)
                    dma("sp", lambda q: q.dma_start(out=bf_bc[:], in_=fox_b_f[0].partition_broadcast(128)), writes=[t_wf], key=t_wf, join=True)
                    op("pool", lambda g_: g_.memset(basef[:], 0.0), writes=[t_bf])
                    for i in range(NT):
                        b = i % 2
                        F = fl[b]

                        def mmf(pe):
                            for kc in range(8):
                                r = pe.matmul(pf[:], lhsT=xT[:, kc, i * 128:(i + 1) * 128], rhs=wf[:, kc, :], start=(kc == 0), stop=(kc == 7))
                            return r
                        op("pe", mmf, reads=[t_xT, t_wf], writes=[t_pf])
                        op("dve", lambda v: v.tensor_tensor(out=F[:, 0:16], in0=pf[:], in1=bf_bc[:], op=ALU.add), reads=[t_pf, t_wf], writes=[t_fl[b]])
                        op("act", lambda a: a.activation(out=F[:, 16:32], in_=F[:, 0:16], func=AF.Exp, scale=-1.0), writes=[t_fl[b]])
                        op("act", lambda a: a.activation(out=F[:, 32:48], in_=F[:, 16:32], func=AF.Ln, bias=c_one[:], scale=1.0),
                           reads=[t_const], writes=[t_fl[b]])
                        op("dve", lambda v: v.tensor_copy(out=flb[b][:, 0:16], in_=F[:, 32:48]), reads=[t_fl[b]], writes=[t_flb[b]])
                        op("dve", lambda v: v.tensor_copy(out=F[:, 48:64], in_=flb[b][:, 0:16]), writes=[t_fl[b]])
                        op("dve", lambda v: v.tensor_tensor(out=F[:, 48:64], in0=F[:, 32:48], in1=F[:, 48:64], op=ALU.subtract), writes=[t_fl[b]])
                        op("dve", lambda v: v.tensor_copy(out=flb[b][:, 16:32], in_=F[:, 48:64]), reads=[t_fl[b]], writes=[t_flb[b]])

                        def mmc(pe):
                            pe.matmul(pc[:, 0, :], lhsT=mask_le[:], rhs=flb[b][:, 0:16], start=True, stop=False)
                            pe.matmul(pc[:, 0, :], lhsT=mask_le[:], rhs=flb[b][:, 16:32], start=False, stop=True)
                            pe.matmul(pc[:, 1, :], lhsT=ones_b[:], rhs=flb[b][:, 0:16], start=True, stop=False)
                            return pe.matmul(pc[:, 1, :], lhsT=ones_b[:], rhs=flb[b][:, 16:32], start=False, stop=True)
                        op("pe", mmc, reads=[t_flb[b], t_const], writes=[t_pc])
                        op("dve", lambda v: v.tensor_tensor(out=ncum[:, i, :], in0=pc[:, 0, :], in1=basef[:], op=ALU.add),
                           reads=[t_pc, t_bf], writes=[t_cum])
                        op("dve", lambda v: v.tensor_tensor(out=basef[:], in0=basef[:], in1=pc[:, 1, :], op=ALU.add),
                           reads=[t_pc], writes=[t_bf])
                        N3 = nsp[b]
                        R_ = nr[b]
                        op("dve", lambda v: v.tensor_copy(out=N3[:, 0, :], in_=ncum[:, i, :]), reads=[t_cum], writes=[t_nsp[b]])
                        op("dve", lambda v: v.tensor_copy(out=R_[:, 0:16], in_=N3[:, 0, :]), writes=[t_nsp[b]])
                        op("dve", lambda v: v.tensor_tensor(out=R_[:, 16:32], in0=ncum[:, i, :], in1=R_[:, 0:16], op=ALU.subtract), writes=[t_nsp[b]])
                        op("dve", lambda v: v.tensor_copy(out=N3[:, 1, :], in_=R_[:, 16:32]), writes=[t_nsp[b]])
                        op("dve", lambda v: v.tensor_copy(out=R_[:, 0:16], in_=N3[:, 1, :]), writes=[t_nsp[b]])
                        op("dve", lambda v: v.tensor_tensor(out=R_[:, 32:48], in0=R_[:, 16:32], in1=R_[:, 0:16], op=ALU.subtract), writes=[t_nsp[b]])
                        op("dve", lambda v: v.tensor_copy(out=N3[:, 2, :], in_=R_[:, 32:48]), writes=[t_nsp[b]])

                        def trn(pe):
                            for g_ in range(3):
                                r = pe.transpose(out=ptr[0:16, g_, :], in_=N3[:, g_, :], identity=identb[:])
                            return r
                        op("pe", trn, reads=[t_nsp[b], t_const], writes=[t_ptr])
                        op("dve", lambda v: v.tensor_scalar(out=refB[0:16, :, i * 128:(i + 1) * 128], in0=ptr[0:16, :, :], scalar1=-1.0,
                                                            scalar2=None, op0=ALU.mult), reads=[t_ptr], writes=[t_cum])
                    dma("sp", lambda q: q.dma_start(out=nrow_d, in_=refB[0:16, :, :]), reads=[t_cum], writes=[t_qkv], key=t_cum, join=True)
                kb.barrier()

        def phase_attn(kind, mT, t_mT, ncum, refB, t_cum):
            with contextlib.ExitStack() as sc:
                KQ = 67 if kind == 2 else 64
                qs = [sb(f"qs{i}", [128, T], BF16, scope=sc) for i in range(2)]
                ks = [sb(f"ks{i}", [128, T], BF16, scope=sc) for i in range(2)]
                vs = [sb(f"vs{i}", [128, NT, 128], BF16, scope=sc) for i in range(2)]
                t_q = [kb.tok("q") for _ in range(2)]; t_k = [kb.tok("k") for _ in range(2)]; t_v = [kb.tok("v") for _ in range(2)]
                NB = 3
                ex = [sb(f"ex{i}", [128, 512], scope=sc) for i in range(NB)]; t_ex = [kb.tok("ex") for _ in range(NB)]
                spb = [sb(f"spb{i}", [128, 512], BF16, scope=sc) for i in range(NB)]; t_sp = [kb.tok("sp") for _ in range(NB)]
                wt = [sb(f"wt{i}", [128, 512], BF16, scope=sc) for i in range(NB)]; t_wt = [kb.tok("wt") for _ in range(NB)]
                acc = sb("acc", [128, 512], BF16, scope=sc); t_acc = kb.tok("acc")
                rden = sb("rden", [128, 512], scope=sc); t_rden = kb.tok("rden")
                pst = [ps(f"pst{i}", [128, 512], scope=sc) for i in range(2)]; t_pst = [kb.tok("pst") for _ in range(2)]
                parg = [ps(f"parg{i}", [128, 512], scope=sc) for i in range(2)]; t_parg = [kb.tok("parg") for _ in range(2)]
                po = [ps(f"po{i}", [128, 512], scope=sc) for i in range(2)]; t_po = [kb.tok("po") for _ in range(2)]
                pden = ps("pden", [128, 512], scope=sc); t_pden = kb.tok("pden")
                pfill = pden; t_fill = kb.tok("fill")
                vsrc = v_d.rearrange("(j p) d -> p j d", p=128)
                if kind == 2:
                    for b in range(2):
                        op("pool", lambda g: g.memset(ks[b][64:67, :], 1.0), writes=[t_k[b]])

                def load_h(hd):
                    b = hd % 2
                    c, hh = hd // 2, hd % 2
                    dma("sp", lambda q: q.dma_start(out=qs[b][0:64, :], in_=qT_d[c, hh * 64:(hh + 1) * 64, :]), reads=[t_qkv],
                        writes=[t_q[b]], key=t_q[b])
                    if kind == 2:
                        dma("sp", lambda q: q.dma_start(out=qs[b][64:67, :], in_=nrow_d[hd]), reads=[t_qkv],
                            writes=[t_q[b]], key=t_q[b], join=True)
                    dma("sp", lambda q: q.dma_start(out=ks[b][0:64, :], in_=kT_d[c, hh * 64:(hh + 1) * 64, :]), reads=[t_qkv],
                        writes=[t_k[b]], key=t_k[b], join=True)
                    if hh == 0:
                        dma("sp", lambda q: q.dma_start(out=vs[c % 2][:], in_=vsrc[:, :, c * 128:(c + 1) * 128]), reads=[t_qkv],
                            writes=[t_v[c % 2]], key=t_v[c % 2])
                load_h(0)
                it = 0
                grp = 0
                for hd in range(16):
                    c, hh = hd // 2, hd % 2
                    hb = hd % 2
                    cb = c % 2
                    P0 = hh * 64
                    if hd + 1 < 16:
                        load_h(hd + 1)
                    for tq in range(NQ):
                        jmax = 4 * tq + 3
                        pob = po[grp % 2]; t_pob = t_po[grp % 2]
                        grp += 1
                        if kind != 2:
                            op("pool", lambda g: g.memset(acc[:], 0.0), writes=[t_acc])
                        op("pe", lambda pe: pe.matmul(pob[:], lhsT=zeros_b[:], rhs=zsrc[:], start=True, stop=False),
                           reads=[t_const], writes=[t_pob])
                        if kind == 2:
                            op("pe", lambda pe: pe.matmul(pden[:], lhsT=zeros_b[:], rhs=zsrc[:], start=True, stop=False),
                               reads=[t_const], writes=[t_pden])
                        order = list(range(jmax + 1)) if kind == 2 else list(range(jmax, -1, -1))
                        ntile = len(order)
                        info = {}

                        def stage1(nj):
                            j = order[nj]
                            b2 = (it + nj) % 2
                            b3 = (it + nj) % NB
                            col0 = max(0, j - 4 * tq) * 128
                            diag = j >= 4 * tq
                            q0 = tq * 512 + col0
                            info[nj] = (j, b2, b3, col0, diag, q0)

                            def mms(pe):
                                r = pe.matmul(pst[b2][:, col0:512], lhsT=ks[hb][0:KQ, j * 128:(j + 1) * 128],
                                              rhs=qs[hb][0:KQ, q0:(tq + 1) * 512], start=True, stop=not (kind == 2 and diag))
                                if kind == 2 and diag:
                                    r = pe.matmul(pst[b2][:, col0:col0 + 128], lhsT=identb[:], rhs=negmask_gt[:], start=False, stop=True)
                                return r
                            op("pe", mms, reads=[t_q[hb], t_k[hb], t_const], writes=[t_pst[b2]])
                            if kind == 2:
                                op("act", lambda a: a.activation(out=wt[b3][:, col0:512], in_=pst[b2][:, col0:512], func=AF.Exp,
                                                                 bias=ncum[:, j, hd:hd + 1], scale=1.0),
                                   reads=[t_pst[b2], t_cum], writes=[t_wt[b3]])
                            else:
                                op("act", lambda a: a.activation(out=ex[b3][:, col0:512], in_=pst[b2][:, col0:512], func=AF.Exp),
                                   reads=[t_pst[b2]], writes=[t_ex[b3]])
                                op("act", lambda a: a.activation(out=spb[b3][:, col0:512], in_=ex[b3][:, col0:512], func=AF.Ln,
                                                                 bias=c_one[:], scale=1.0), reads=[t_ex[b3], t_const], writes=[t_sp[b3]])
                                if diag:
                                    op("pool", lambda g: g.tensor_tensor(out=spb[b3][:, col0:col0 + 128], in0=spb[b3][:, col0:col0 + 128],
                                                                         in1=mask_lt[:], op=ALU.mult), reads=[t_const], writes=[t_sp[b3]])

                        def stage2(nj):
                            (j, b2, b3, col0, diag, q0) = info[nj]

                            def mma(pe):
                                pe.matmul(parg[b2][:, col0:512], lhsT=ks[hb][0:64, j * 128:(j + 1) * 128],
                                          rhs=qs[hb][0:64, q0:(tq + 1) * 512], start=True, stop=False)
                                pe.matmul(parg[b2][:, col0:512], lhsT=ntri_ge[:], rhs=spb[b3][:, col0:512], start=False, stop=False)
                                return pe.matmul(parg[b2][:, col0:512], lhsT=nones_b[:], rhs=acc[:, col0:512], start=False, stop=True)
                            op("pe", mma, reads=[t_q[hb], t_k[hb], t_sp[b3], t_acc, t_const], writes=[t_parg[b2]])

                            def fill(pe):
                                pe.matmul(pfill[:], lhsT=zeros_b[:], rhs=zsrc[:], start=True, stop=True)
                                return pe.matmul(pfill[:], lhsT=zeros_b[:], rhs=zsrc[:], start=True, stop=True)
                            op("pe", fill, reads=[t_const], writes=[t_fill])
                            op("pool", lambda g: g.tensor_tensor(out=acc[:, col0:512], in0=acc[:, col0:512], in1=spb[b3][:, col0:512],
                                                                 op=ALU.add), reads=[t_sp[b3]], writes=[t_acc])
                            op("act", lambda a: a.activation(out=wt[b3][:, col0:512], in_=parg[b2][:, col0:512], func=AF.Exp),
                               reads=[t_parg[b2]], writes=[t_wt[b3]])
                            if diag:
                                op("pool", lambda g: g.tensor_tensor(out=wt[b3][:, col0:col0 + 128], in0=wt[b3][:, col0:col0 + 128],
                                                                     in1=mask_lt[:], op=ALU.mult), reads=[t_const], writes=[t_wt[b3]])

                        def stage3(nj):
                            (j, b2, b3, col0, diag, q0) = info[nj]
                            last = (nj == ntile - 1)

                            def mmo(pe):
                                r = pe.matmul(pob[:, col0:512], lhsT=vs[cb][:, j, :], rhs=wt[b3][:, col0:512], start=False, stop=last)
                                if kind == 2:
                                    r = pe.matmul(pden[:, col0:512], lhsT=ones_b[:], rhs=wt[b3][:, col0:512], start=False, stop=last)
                                return r
                            op("pe", mmo, reads=[t_wt[b3], t_v[cb], t_const], writes=[t_pob] + ([t_pden] if kind == 2 else []))

                        if kind == 2:
                            for n in range(ntile + 1):
                                if n < ntile:
                                    stage1(n)
                                if n >= 1:
                                    stage3(n - 1)
                        else:
                            for n in range(ntile + 2):
                                if n < ntile:
                                    stage1(n)
                                if 1 <= n <= ntile:
                                    stage2(n - 1)
                                if n >= 2:
                                    stage3(n - 2)
                        it += ntile
                        osl = mT[P0:P0 + 64, c, tq * 512:(tq + 1) * 512]
                        if kind == 2:
                            op("dve", lambda v: v.reciprocal(out=rden[P0:P0 + 64, :], in_=pden[P0:P0 + 64, :]), reads=[t_pden], writes=[t_rden])
                            op("dve", lambda v: v.tensor_tensor(out=osl, in0=pob[P0:P0 + 64, :], in1=rden[P0:P0 + 64, :], op=ALU.mult),
                               reads=[t_pob, t_rden], writes=[t_mT])
                        else:
                            op("dve", lambda v: v.tensor_copy(out=osl, in_=pob[P0:P0 + 64, :]), reads=[t_pob], writes=[t_mT])
                kb.barrier()

        def phase_outproj_ln1_route(li, w_out_d, xin, t_xin, mT, t_mT, gates, dests, t_route):
            with contextlib.ExitStack() as sc:
                wo = sb("wo", [128, 8, D], BF16, scope=sc); t_wo = kb.tok("wo")
                g_bc = sb("g_bc", [128, D], scope=sc); b_bc = sb("b_bc", [128, D], scope=sc); t_gb = kb.tok("gb")
                wr32 = sb("wr32", [128, 8, NE], scope=sc)
                wrh = sb("wrh", [128, 8, NE], BF16, scope=sc)
                wrl = sb("wrl", [128, 8, NE], BF16, scope=sc)
                wrt = sb("wrt", [128, 8, NE], scope=sc)
                rb_bc = sb("rb_bc", [128, NE], scope=sc)
                t_wr = kb.tok("wr")
                base = sb("base", [128, NE], scope=sc); t_base = kb.tok("base")
                xs = [sb(f"xs{i}", [128, D], scope=sc) for i in range(2)]; t_xs = [kb.tok("xs") for _ in range(2)]
                h = [sb(f"h{i}", [128, D], scope=sc) for i in range(2)]; t_h = [kb.tok("h") for _ in range(2)]
                x1 = [sb(f"x1_{i}", [128, D], scope=sc) for i in range(2)]; t_x1 = [kb.tok("x1") for _ in range(2)]
                x1h = [sb(f"x1h{i}", [128, D], BF16, scope=sc) for i in range(2)]; t_x1h = [kb.tok("x1h") for _ in range(2)]
                x1l = [sb(f"x1l{i}", [128, D], BF16, scope=sc) for i in range(2)]; t_x1l = [kb.tok("x1l") for _ in range(2)]
                x1t = [sb(f"x1t{i}", [128, D], scope=sc) for i in range(2)]
                xTh = [sb(f"xTh{i}", [128, 8, 128], BF16, scope=sc) for i in range(2)]; t_xTh = [kb.tok("xTh") for _ in range(2)]
                xTl = [sb(f"xTl{i}", [128, 8, 128], BF16, scope=sc) for i in range(2)]; t_xTl = [kb.tok("xTl") for _ in range(2)]
                stat = [sb(f"stat{i}", [128, 16], scope=sc) for i in range(2)]; t_stat = [kb.tok("stat") for _ in range(2)]
                rt = [sb(f"rt{i}", [128, 8 * NE], scope=sc) for i in range(2)]; t_rt = [kb.tok("rt") for _ in range(2)]
                mkb = [sb(f"mkb{i}", [128, NE], BF16, scope=sc) for i in range(2)]; t_mkb = [kb.tok("mkb") for _ in range(2)]
                sm = [sb(f"sm{i}", [128, 32], scope=sc) for i in range(2)]
                desti = sb("desti", [128, NT, 4], I32, scope=sc)
                py = [ps(f"py{i}", [128, D], scope=sc) for i in range(2)]; t_py = [kb.tok("py") for _ in range(2)]
                pTh = ps("pTh", [128, 8, 128], BF16, scope=sc); t_pTh = kb.tok("pTh")
                pTl = ps("pTl", [128, 8, 128], BF16, scope=sc); t_pTl = kb.tok("pTl")
                plog = ps("plog", [128, NE], scope=sc); t_plog = kb.tok("plog")
                pcnt = ps("pcnt", [128, 2, NE], scope=sc); t_pcnt = kb.tok("pcnt")

                dma("pool", lambda q: q.dma_start(out=wo[:], in_=w_out_d.rearrange("(kc p) n -> p kc n", p=128)),
                    writes=[t_wo], key=t_wo)
                dma("sp", lambda q: q.dma_start(out=g_bc[:], in_=ln1_g[li].partition_broadcast(128)), writes=[t_gb], key=t_gb)
                dma("sp", lambda q: q.dma_start(out=b_bc[:], in_=ln1_b[li].partition_broadcast(128)), writes=[t_gb], key=t_gb, join=True)
                dma("sp", lambda q: q.dma_start(out=wr32[:], in_=router_w[li].rearrange("(kc p) n -> p kc n", p=128)),
                    writes=[t_wr], key=t_wr)
                dma("sp", lambda q: q.dma_start(out=rb_bc[:], in_=router_b[li].partition_broadcast(128)), writes=[t_wr], key=t_wr, join=True)
                op("dve", lambda v: v.tensor_copy(out=wrh[:], in_=wr32[:]), reads=[t_wr], writes=[t_wr])
                op("dve", lambda v: v.tensor_copy(out=wrt[:], in_=wrh[:]), writes=[t_wr])
                op("dve", lambda v: v.tensor_tensor(out=wrt[:], in0=wr32[:], in1=wrt[:], op=ALU.subtract), writes=[t_wr])
                op("dve", lambda v: v.tensor_copy(out=wrl[:], in_=wrt[:]), writes=[t_wr])
                op("pool", lambda g: g.memset(base[:], 0.0), writes=[t_base])
                dma("sp", lambda q: q.dma_start(out=toklist.rearrange("(p r) o -> p (r o)", p=128), in_=zi[:]),
                    reads=[t_const], writes=[t_tl], key=t_tl)

                for i in range(NT):
                    b = i % 2
                    dma("sp", lambda q: q.dma_start(out=xs[b][:], in_=xin[i * 128:(i + 1) * 128, :]),
                        reads=[t_xin], writes=[t_xs[b]], key=t_xs[b])

                    def mm(pe):
                        for hh in range(2):
                            for c in range(8):
                                r = pe.matmul(py[b][:, hh * 512:(hh + 1) * 512], lhsT=mT[:, c, i * 128:(i + 1) * 128],
                                              rhs=wo[:, c, hh * 512:(hh + 1) * 512], start=(c == 0), stop=(c == 7))
                        return r
                    op("pe", mm, reads=[t_mT, t_wo], writes=[t_py[b]])
                    op("dve", lambda v: v.scalar_tensor_tensor(out=h[b][:], in0=xs[b][:], scalar=ALPHA, in1=py[b][:],
                                                               op0=ALU.mult, op1=ALU.add),
                       reads=[t_xs[b], t_py[b]], writes=[t_h[b]])
                    layer_norm_tile(h[b], g_bc, b_bc, x1[b], stat[b], t_h[b], t_x1[b], t_stat[b], t_gb)
                    dma("sp", lambda q: q.dma_start(out=x1res[i * 128:(i + 1) * 128, :], in_=x1[b][:]),
                        reads=[t_x1[b]], writes=[t_x1res], key=t_x1[b], join=True)
                    op("act", lambda a: a.copy(out=x1h[b][:], in_=x1[b][:]), reads=[t_x1[b]], writes=[t_x1h[b]])
                    dma("sp", lambda q: q.dma_start(out=x1bf[i * 128:(i + 1) * 128, :], in_=x1h[b][:]),
                        reads=[t_x1h[b]], writes=[t_x1bf], key=t_x1h[b], join=True)
                    op("pool", lambda g: g.tensor_copy(out=x1t[b][:], in_=x1h[b][:]), reads=[t_x1h[b]], writes=[t_x1l[b]])
                    op("pool", lambda g: g.tensor_tensor(out=x1t[b][:], in0=x1[b][:], in1=x1t[b][:], op=ALU.subtract),
                       reads=[t_x1[b]], writes=[t_x1l[b]])
                    op("pool", lambda g: g.tensor_copy(out=x1l[b][:], in_=x1t[b][:]), writes=[t_x1l[b]])
                    for (src, tsrc, pp, tpp, dst, tdst) in ((x1h[b], t_x1h[b], pTh, t_pTh, xTh[b], t_xTh[b]),
                                                            (x1l[b], t_x1l[b], pTl, t_pTl, xTl[b], t_xTl[b])):
                        def tr(pe):
                            for c in range(8):
                                r = pe.transpose(out=pp[:, c, :], in_=src[:, c * 128:(c + 1) * 128], identity=identb[:])
                            return r
                        op("pe", tr, reads=[tsrc, t_const], writes=[tpp])
                        op("act", lambda a: a.copy(out=dst[:], in_=pp[:]), reads=[tpp], writes=[tdst])

                    def mmr(pe):
                        n = 0
                        for (xa, wa) in ((xTh[b], wrh), (xTh[b], wrl), (xTl[b], wrh)):
                            for c in range(8):
                                r = pe.matmul(plog[:], lhsT=xa[:, c, :], rhs=wa[:, c, :], start=(n == 0), stop=(n == 23))
                                n += 1
                        return r
                    op("pe", mmr, reads=[t_xTh[b], t_xTl[b], t_wr], writes=[t_plog])
                    R = rt[b]
                    L = R[:, 0:32]; MK = R[:, 32:64]; EX = R[:, 64:96]; G = R[:, 96:128]
                    POS = R[:, 128:160]; TMP = R[:, 160:192]; OH = R[:, 192:224]; TMP2 = R[:, 224:256]
                    S = sm[b]
                    top8 = S[:, 0:8]; nmax = S[:, 8:9]; den = S[:, 9:10]; rden = S[:, 10:11]
                    dstf = S[:, 12:16]
                    tr_ = t_rt[b]
                    op("dve", lambda v: v.tensor_tensor(out=L, in0=plog[:], in1=rb_bc[:], op=ALU.add),
                       reads=[t_plog, t_wr], writes=[tr_])
                    op("dve", lambda v: v.max(out=top8, in_=L), writes=[tr_])
                    op("dve", lambda v: v.tensor_scalar(out=MK, in0=L, scalar1=S[:, 3:4], scalar2=None, op0=ALU.is_ge), writes=[tr_])
                    op("dve", lambda v: v.tensor_scalar(out=nmax, in0=S[:, 0:1], scalar1=-1.0, scalar2=None, op0=ALU.mult), writes=[tr_])
                    op("act", lambda a: a.activation(out=EX, in_=L, func=AF.Exp, bias=nmax, scale=1.0), writes=[tr_])
                    op("dve", lambda v: v.tensor_tensor(out=EX, in0=EX, in1=MK, op=ALU.mult), writes=[tr_])
                    op("dve", lambda v: v.reduce_sum(out=den, in_=EX, axis=AX.X), writes=[tr_])
                    op("dve", lambda v: v.reciprocal(out=rden, in_=den), writes=[tr_])
                    op("dve", lambda v: v.tensor_scalar(out=G, in0=EX, scalar1=rden, scalar2=None, op0=ALU.mult), writes=[tr_])
                    op("dve", lambda v: v.tensor_copy(out=mkb[b][:], in_=MK), reads=[tr_], writes=[t_mkb[b]])

                    def mmc(pe):
                        pe.matmul(pcnt[:, 0, :], lhsT=tri_lt[:], rhs=mkb[b][:], start=True, stop=True)
                        return pe.matmul(pcnt[:, 1, :], lhsT=ones_b[:], rhs=mkb[b][:], start=True, stop=True)
                    op("pe", mmc, reads=[t_mkb[b], t_const], writes=[t_pcnt])
                    op("dve", lambda v: v.tensor_tensor(out=POS, in0=pcnt[:, 0, :], in1=base[:], op=ALU.add),
                       reads=[t_pcnt, t_base], writes=[tr_])
                    op("dve", lambda v: v.tensor_tensor(out=base[:], in0=base[:], in1=pcnt[:, 1, :], op=ALU.add),
                       reads=[t_pcnt], writes=[t_base])
                    op("dve", lambda v: v.tensor_scalar(out=TMP, in0=POS, scalar1=float(C), scalar2=None, op0=ALU.is_lt), writes=[tr_])
                    op("dve", lambda v: v.tensor_tensor(out=POS, in0=POS, in1=ecol[:], op=ALU.add), reads=[t_const], writes=[tr_])
                    op("dve", lambda v: v.tensor_scalar(out=POS, in0=POS, scalar1=float(-NSLOT), scalar2=None, op0=ALU.add), writes=[tr_])
                    op("dve", lambda v: v.tensor_tensor(out=POS, in0=POS, in1=TMP, op=ALU.mult), writes=[tr_])
                    op("dve", lambda v: v.tensor_scalar(out=POS, in0=POS, scalar1=float(NSLOT), scalar2=None, op0=ALU.add), writes=[tr_])
                    for k in range(4):
                        op("dve", lambda v: v.tensor_scalar(out=OH, in0=L, scalar1=S[:, k:k + 1], scalar2=None, op0=ALU.is_equal), writes=[tr_])
                        op("dve", lambda v: v.tensor_tensor(out=TMP2, in0=OH, in1=POS, op=ALU.mult), writes=[tr_])
                        op("dve", lambda v: v.reduce_sum(out=dstf[:, k:k + 1], in_=TMP2, axis=AX.X), writes=[tr_])
                        op("dve", lambda v: v.tensor_tensor(out=TMP2, in0=OH, in1=G, op=ALU.mult), writes=[tr_])
                        op("dve", lambda v: v.reduce_sum(out=gates[:, i, k:k + 1], in_=TMP2, axis=AX.X), writes=[tr_, t_route])
                    op("dve", lambda v: v.tensor_copy(out=dests[:, i, :], in_=dstf), reads=[tr_], writes=[t_route])
                    for k in range(4):
                        dma("pool", lambda q: q.indirect_dma_start(
                            out=toklist[:, :], out_offset=bass.IndirectOffsetOnAxis(ap=dests[:, i, k:k + 1], axis=0),
                            in_=tokid[:, i:i + 1], in_offset=None), reads=[t_route, t_const], writes=[t_tl], key=t_tl, join=True)
                kb.barrier()

        def phase_experts(li):
            with contextlib.ExitStack() as sc:
                wg = [sb(f"wg{i}", [128, 8, D], BF16, scope=sc) for i in range(2)]
                wu = [sb(f"wu{i}", [128, 8, D], BF16, scope=sc) for i in range(2)]
                wd = [sb(f"wd{i}", [128, 8, D], BF16, scope=sc) for i in range(2)]
                t_wg = [kb.tok("wg") for _ in range(2)]; t_wu = [kb.tok("wu") for _ in range(2)]; t_wd = [kb.tok("wd") for _ in range(2)]
                bd = [sb(f"bd{i}", [128, D], scope=sc) for i in range(2)]; t_bd = [kb.tok("bd") for _ in range(2)]
                braw = sb("braw", [NE, 2, D], scope=sc)
                brb = sb("brb", [NE, 2, D], BF16, scope=sc)
                bT = sb("bT", [128, 2, 8, NE], scope=sc)
                t_b = kb.tok("bias")
                idx = [sb(f"idx{i}", [128, CR], I32, scope=sc) for i in range(2)]; t_idx = [kb.tok("idx") for _ in range(2)]
                xg = [sb(f"xg{i}", [128, CR, D], BF16, scope=sc) for i in range(2)]; t_xg = [kb.tok("xg") for _ in range(2)]
                xeT = sb("xeT", [128, 8, C], BF16, scope=sc); t_xeT = kb.tok("xeT")
                aT = sb("aT", [128, 8, C], BF16, scope=sc); t_aT = kb.tok("aT")
                gt = [sb(f"gt{i}", [128, 512], scope=sc) for i in range(2)]; t_gt = [kb.tok("gt") for _ in range(2)]
                sg = [sb(f"sg{i}", [128, 512], scope=sc) for i in range(2)]; t_sg = [kb.tok("sg") for _ in range(2)]
                ut = [sb(f"ut{i}", [128, 512], scope=sc) for i in range(2)]; t_ut = [kb.tok("ut") for _ in range(2)]
                yo = [sb(f"yo{i}", [128, D], scope=sc) for i in range(2)]; t_yo = [kb.tok("yo") for _ in range(2)]
                pT = ps("pT", [128, 8, 128], BF16, scope=sc); t_pT = kb.tok("pT")
                pTs = [pT, ps("pTb", [128, 8, 128], BF16, scope=sc)]; t_pTs = [t_pT, kb.tok("pTb")]
                pg = [ps(f"pg{i}", [128, 512], scope=sc) for i in range(2)]; t_pg = [kb.tok("pg") for _ in range(2)]
                pu = [ps(f"pu{i}", [128, 512], scope=sc) for i in range(2)]; t_pu = [kb.tok("pu") for _ in range(2)]
                py = ps("py", [128, 1024], scope=sc); t_py = kb.tok("py")
                t_pyh = [kb.tok("pyh0"), kb.tok("pyh1")]
                t_yoh = [kb.tok("yoh") for _ in range(2)]
                ytmp = [sb(f"ytmp{i}", [128, 512], scope=sc) for i in range(2)]; t_ytmp = [kb.tok("ytmp") for _ in range(2)]
                pbt = pT[:, :, 0:NE]; t_pbt = t_pT

                dma("sp", lambda q: q.dma_start(out=braw[:, 0, :], in_=exp_b_gate[li]), writes=[t_b], key=t_b)
                dma("sp", lambda q: q.dma_start(out=braw[:, 1, :], in_=exp_b_up[li]), writes=[t_b], key=t_b, join=True)
                op("dve", lambda v: v.tensor_copy(out=brb[:], in_=braw[:]), reads=[t_b], writes=[t_b])
                for g in range(2):
                    def tr(pe):
                        for c in range(8):
                            r = pe.transpose(out=pT[:, c, 0:NE], in_=brb[:, g, c * 128:(c + 1) * 128], identity=identb[0:NE, 0:NE])
                        return r
                    op("pe", tr, reads=[t_b, t_const], writes=[t_pbt])
                    op("dve", lambda v: v.tensor_copy(out=bT[:, g, :, :], in_=pT[:, :, 0:NE]), reads=[t_pbt], writes=[t_b])

                def load_w(e):
                    b = e % 2
                    for (dst, td, src) in ((wg[b], t_wg[b], exp_w_gate), (wu[b], t_wu[b], exp_w_up), (wd[b], t_wd[b], exp_w_down)):
                        dma("pool", lambda q: q.dma_start(out=dst[:], in_=src[li, e].rearrange("(kc p) n -> p kc n", p=128)),
                            writes=[td], key=td)
                    dma("sp", lambda q: q.dma_start(out=bd[b][:], in_=exp_b_down[li, e].partition_broadcast(128)),
                        writes=[t_bd[b]], key=t_bd[b])

                def load_x(e):
                    b = e % 2
                    dma("sp", lambda q: q.dma_start(out=idx[b][:], in_=toklist[e * C:(e + 1) * C, :].rearrange("(p r) o -> p (r o)", p=128)),
                        reads=[t_tl], writes=[t_idx[b]], key=t_idx[b])
                    for r in range(CR):
                        dma("pool", lambda q: q.indirect_dma_start(
                            out=xg[b][:, r, :], out_offset=None, in_=x1bf[:, :],
                            in_offset=bass.IndirectOffsetOnAxis(ap=idx[b][:, r:r + 1], axis=0)),
                            reads=[t_idx[b], t_x1bf], writes=[t_xg[b]], key=t_xg[b], join=(r > 0))

                load_x(0)
                load_w(0)
                it = 0
                for e in range(NE):
                    b = e % 2
                    if e + 1 < NE:
                        load_x(e + 1)
                        load_w(e + 1)
                    for r in range(CR):
                        pTr, t_pTr = pTs[r % 2], t_pTs[r % 2]

                        def tr(pe):
                            for c in range(8):
                                rr = pe.transpose(out=pTr[:, c, :], in_=xg[b][:, r, c * 128:(c + 1) * 128], identity=identb[:])
                            return rr
                        op("pe", tr, reads=[t_xg[b], t_const], writes=[t_pTr])
                        if r % 2 == 0:
                            op("act", lambda a: a.copy(out=xeT[:, :, r * 128:(r + 1) * 128], in_=pTr[:]), reads=[t_pTr], writes=[t_xeT])
                        else:
                            op("dve", lambda v: v.tensor_copy(out=xeT[:, :, r * 128:(r + 1) * 128], in_=pTr[:]), reads=[t_pTr], writes=[t_xeT])
                    segs = [(s0, min(512, C - s0)) for s0 in range(0, C, 512)]
                    for fc in range(8):
                        for (s0, sn) in segs:
                            bb = it % 2
                            it += 1
                            for (pp, tp, w_, tw) in ((pg[bb], t_pg[bb], wg[b], t_wg[b]), (pu[bb], t_pu[bb], wu[b], t_wu[b])):
                                def mm(pe):
                                    for kc in range(8):
                                        rr = pe.matmul(pp[:, 0:sn], lhsT=w_[:, kc, fc * 128:(fc + 1) * 128],
                                                       rhs=xeT[:, kc, s0:s0 + sn], start=(kc == 0), stop=(kc == 7))
                                    return rr
                                op("pe", mm, reads=[tw, t_xeT], writes=[tp])
                            G_, S_, U_ = gt[bb][:, 0:sn], sg[bb][:, 0:sn], ut[bb][:, 0:sn]
                            op("dve", lambda v: v.tensor_scalar(out=G_, in0=pg[bb][:, 0:sn], scalar1=bT[:, 0, fc, e:e + 1], scalar2=7.0,
                                                                op0=ALU.add, op1=ALU.min), reads=[t_pg[bb], t_b], writes=[t_gt[bb]])
                            op("act", lambda a: a.activation(out=S_, in_=G_, func=AF.Silu, scale=1.702),
                               reads=[t_gt[bb]], writes=[t_sg[bb]])
                            op("dve", lambda v: v.tensor_scalar(out=U_, in0=pu[bb][:, 0:sn], scalar1=bT[:, 1, fc, e:e + 1], scalar2=7.0,
                                                                op0=ALU.add, op1=ALU.min), reads=[t_pu[bb], t_b], writes=[t_ut[bb]])
                            op("dve", lambda v: v.tensor_scalar(out=U_, in0=U_, scalar1=-7.0, scalar2=1.0 / 1.702,
                                                                op0=ALU.max, op1=ALU.mult), writes=[t_ut[bb]])
                            op("dve", lambda v: v.scalar_tensor_tensor(out=aT[:, fc, s0:s0 + sn], in0=U_, scalar=1.0 / 1.702, in1=S_,
                                                                       op0=ALU.add, op1=ALU.mult),
                               reads=[t_sg[bb], t_ut[bb]], writes=[t_aT])
                    for r in range(CR):
                        yb = (e * CR + r) % 2

                        for hh in range(2):
                            def mmd(pe):
                                for fc in range(8):
                                    rr = pe.matmul(py[:, hh * 512:(hh + 1) * 512], lhsT=aT[:, fc, r * 128:(r + 1) * 128],
                                                   rhs=wd[b][:, fc, hh * 512:(hh + 1) * 512], start=(fc == 0), stop=(fc == 7))
                                return rr
                            op("pe", mmd, reads=[t_aT, t_wd[b]], writes=[t_pyh[hh]])
                            if hh == 0:
                                op("dve", lambda v: v.tensor_tensor(out=yo[yb][:, 0:512], in0=py[:, 0:512], in1=bd[b][:, 0:512], op=ALU.add),
                                   reads=[t_pyh[hh], t_bd[b]], writes=[t_yo[yb]])
                            else:
                                op("pool", lambda g: g.tensor_copy(out=yo[yb][:, 512:1024], in_=bd[b][:, 512:1024]), reads=[t_bd[b]], writes=[t_yoh[yb]])
                                op("act", lambda a: a.copy(out=ytmp[yb][:], in_=py[:, 512:1024]),
                                   reads=[t_pyh[hh]], writes=[t_ytmp[yb]])
                                op("pool", lambda g: g.tensor_tensor(out=yo[yb][:, 512:1024], in0=yo[yb][:, 512:1024], in1=ytmp[yb][:], op=ALU.add),
                                   reads=[t_ytmp[yb], t_yoh[yb]], writes=[t_yoh[yb]])
                        ydst = yslots[e * C:(e + 1) * C, :].rearrange("(p r) d -> p r d", p=128)
                        dma("sp", lambda q: q.dma_start(out=ydst[:, r, :], in_=yo[yb][:]),
                            reads=[t_yo[yb], t_yoh[yb]], writes=[t_ys], key=t_yo[yb], join=True)
                kb.barrier()

        def phase_combine_ln2_ple(li, gates, dests, t_route, xout, t_xout):
            with contextlib.ExitStack() as sc:
                wpg = sb("wpg", [128, 8, D], BF16, scope=sc); wpp = sb("wpp", [128, 2, D], BF16, scope=sc); t_w = kb.tok("wple")
                g_bc = sb("g_bc", [128, D], scope=sc); b_bc = sb("b_bc", [128, D], scope=sc)
                bpg = sb("bpg", [128, D], scope=sc); t_gb = kb.tok("gb")
                yk = [[sb(f"yk{i}_{k}", [128, D], scope=sc) for k in range(4)] for i in range(2)]
                t_yk = [[kb.tok("yk") for k in range(4)] for i in range(2)]
                xs = [sb(f"xs{i}", [128, D], scope=sc) for i in range(2)]; t_xs = [kb.tok("xs") for _ in range(2)]
                h = [sb(f"h{i}", [128, D], scope=sc) for i in range(2)]; t_h = [kb.tok("h") for _ in range(2)]
                x2 = [sb(f"x2_{i}", [128, D], scope=sc) for i in range(2)]; t_x2 = [kb.tok("x2") for _ in range(2)]
                x2b = [sb(f"x2b{i}", [128, D], BF16, scope=sc) for i in range(2)]; t_x2b = [kb.tok("x2b") for _ in range(2)]
                x2T = [sb(f"x2T{i}", [128, 8, 128], BF16, scope=sc) for i in range(2)]; t_x2T = [kb.tok("x2T") for _ in range(2)]
                pp32 = [sb(f"pp32_{i}", [128, PLE], scope=sc) for i in range(2)]; t_pp = [kb.tok("pp") for _ in range(2)]
                ppb = [sb(f"ppb{i}", [128, PLE], BF16, scope=sc) for i in range(2)]; t_ppb = [kb.tok("ppb") for _ in range(2)]
                ppT = [sb(f"ppT{i}", [128, 2, 128], BF16, scope=sc) for i in range(2)]; t_ppT = [kb.tok("ppT") for _ in range(2)]
                stat = [sb(f"stat{i}", [128, 16], scope=sc) for i in range(2)]; t_stat = [kb.tok("stat") for _ in range(2)]
                gs = [sb(f"gs{i}", [128, D], scope=sc) for i in range(2)]; t_gs = [kb.tok("gs") for _ in range(2)]
                xo = [sb(f"xo{i}", [128, D], scope=sc) for i in range(2)]; t_xo = [kb.tok("xo") for _ in range(2)]
                pT = ps("pT", [128, 8, 128], BF16, scope=sc); t_pT = kb.tok("pT")
                pT2 = ps("pT2", [128, 2, 128], BF16, scope=sc); t_pT2 = kb.tok("pT2")
                pgt = ps("pgt", [128, D], scope=sc); t_pgt = kb.tok("pgt")
                ppj = ps("ppj", [128, D], scope=sc); t_ppj = kb.tok("ppj")
                dma("pool", lambda q: q.dma_start(out=wpg[:], in_=ple_w_gate[li].rearrange("(kc p) n -> p kc n", p=128)), writes=[t_w], key=t_w)
                dma("pool", lambda q: q.dma_start(out=wpp[:], in_=ple_w_proj[li].rearrange("(kc p) n -> p kc n", p=128)), writes=[t_w], key=t_w, join=True)
                dma("sp", lambda q: q.dma_start(out=g_bc[:], in_=ln2_g[li].partition_broadcast(128)), writes=[t_gb], key=t_gb)
                dma("sp", lambda q: q.dma_start(out=b_bc[:], in_=ln2_b[li].partition_broadcast(128)), writes=[t_gb], key=t_gb, join=True)
                dma("sp", lambda q: q.dma_start(out=bpg[:], in_=ple_b_gate[li].partition_broadcast(128)), writes=[t_gb], key=t_gb, join=True)
                for i in range(NT):
                    b = i % 2
                    dma("sp", lambda q: q.dma_start(out=xs[b][:], in_=x1res[i * 128:(i + 1) * 128, :]),
                        reads=[t_x1res], writes=[t_xs[b]], key=t_xs[b])
                    dma("sp", lambda q: q.dma_start(out=pp32[b][:], in_=p_in[li, i * 128:(i + 1) * 128, :]),
                        writes=[t_pp[b]], key=t_pp[b])
                    for k in range(4):
                        dma("pool", lambda q: q.indirect_dma_start(
                            out=yk[b][k][:], out_offset=None, in_=yslots[:, :],
                            in_offset=bass.IndirectOffsetOnAxis(ap=dests[:, i, k:k + 1], axis=0)),
                            reads=[t_route, t_ys], writes=[t_yk[b][k]], key=t_yk[b][k])
                    op("act", lambda a: a.mul(out=h[b][:], in_=xs[b][:], mul=ALPHA), reads=[t_xs[b]], writes=[t_h[b]])
                    for k in range(4):
                        op("dve", lambda v: v.scalar_tensor_tensor(out=h[b][:], in0=yk[b][k][:], scalar=gates[:, i, k:k + 1],
                                                                   in1=h[b][:], op0=ALU.mult, op1=ALU.add),
                           reads=[t_yk[b][k], t_route], writes=[t_h[b]])
                    layer_norm_tile(h[b], g_bc, b_bc, x2[b], stat[b], t_h[b], t_x2[b], t_stat[b], t_gb)
                    op("act", lambda a: a.copy(out=x2b[b][:], in_=x2[b][:]), reads=[t_x2[b]], writes=[t_x2b[b]])
                    op("act", lambda a: a.copy(out=ppb[b][:], in_=pp32[b][:]), reads=[t_pp[b]], writes=[t_ppb[b]])

                    def tr(pe):
                        for c in range(8):
                            r = pe.transpose(out=pT[:, c, :], in_=x2b[b][:, c * 128:(c + 1) * 128], identity=identb[:])
                        return r
                    op("pe", tr, reads=[t_x2b[b], t_const], writes=[t_pT])
                    op("act", lambda a: a.copy(out=x2T[b][:], in_=pT[:]), reads=[t_pT], writes=[t_x2T[b]])

                    def tr2(pe):
                        for c in range(2):
                            r = pe.transpose(out=pT2[:, c, :], in_=ppb[b][:, c * 128:(c + 1) * 128], identity=identb[:])
                        return r
                    op("pe", tr2, reads=[t_ppb[b], t_const], writes=[t_pT2])
                    op("act", lambda a: a.copy(out=ppT[b][:], in_=pT2[:]), reads=[t_pT2], writes=[t_ppT[b]])

                    def mmg(pe):
                        for hh in range(2):
                            for c in range(8):
                                r = pe.matmul(pgt[:, hh * 512:(hh + 1) * 512], lhsT=x2T[b][:, c, :], rhs=wpg[:, c, hh * 512:(hh + 1) * 512],
                                              start=(c == 0), stop=(c == 7))
                        return r
                    op("pe", mmg, reads=[t_x2T[b], t_w], writes=[t_pgt])

                    def mmp(pe):
                        for hh in range(2):
                            for c in range(2):
                                r = pe.matmul(ppj[:, hh * 512:(hh + 1) * 512], lhsT=ppT[b][:, c, :], rhs=wpp[:, c, hh * 512:(hh + 1) * 512],
                                              start=(c == 0), stop=(c == 1))
                        return r
                    op("pe", mmp, reads=[t_ppT[b], t_w], writes=[t_ppj])
                    op("dve", lambda v: v.tensor_tensor(out=gs[b][:], in0=pgt[:], in1=bpg[:], op=ALU.add),
                       reads=[t_pgt, t_gb], writes=[t_gs[b]])
                    op("act", lambda a: a.activation(out=gs[b][:], in_=gs[b][:], func=AF.Sigmoid), writes=[t_gs[b]])
                    op("dve", lambda v: v.tensor_tensor(out=xo[b][:], in0=ppj[:], in1=gs[b][:], op=ALU.mult),
                       reads=[t_ppj, t_gs[b]], writes=[t_xo[b]])
                    op("pool", lambda g: g.tensor_tensor(out=xo[b][:], in0=xo[b][:], in1=x2[b][:], op=ALU.add),
                       reads=[t_x2[b]], writes=[t_xo[b]])
                    dma("sp", lambda q: q.dma_start(out=xout[i * 128:(i + 1) * 128, :], in_=xo[b][:]),
                        reads=[t_xo[b]], writes=[t_xout], key=t_xo[b], join=True)
                kb.barrier()

        gates = sb("gates", [128, NT, 4])
        dests = sb("dests", [128, NT, 4], I32)
        t_route = kb.tok("route")
        xin, t_xin = x_in, kb.tok("xin")
        nl = len(layers)
        for n, li in enumerate(layers):
            kind, j = KINDS[li], JIDX[li]
            last = (n == nl - 1)
            xout, t_xout = (out_d, t_out) if last else (xres[n % 2], t_xres[n % 2])
            with contextlib.ExitStack() as lsc:
                t_mT = kb.tok("mT")
                if kind == 0:
                    mT = sb("mT", [128, 8, T], BF16, scope=lsc)
                    with contextlib.ExitStack() as asc:
                        xT = sb("xT", [128, 8, T], BF16, scope=asc); t_xT = kb.tok("xT")
                        phase_xT(xin, t_xin, xT, t_xT)
                        phase_conv(j, xT, t_xT, mT, t_mT)
                    w_out_d = conv_w_out[j]
                else:
                    ncum = sb("ncum", [128, NT, 16], scope=lsc)
                    refB = sb("refB", [16, 3, T], BF16, scope=lsc)
                    t_cum = kb.tok("cum")
                    w_in_d = sb_w_in[0] if kind == 1 else fox_w_in[0]
                    with contextlib.ExitStack() as asc:
                        xT = sb("xT", [128, 8, T], BF16, scope=asc); t_xT = kb.tok("xT")
                        phase_xT(xin, t_xin, xT, t_xT)
                        phase_qkv(kind, w_in_d, xT, t_xT, ncum, refB, t_cum)
                    mT = sb("mT", [128, 8, T], BF16, scope=lsc)
                    phase_attn(kind, mT, t_mT, ncum, refB, t_cum)
                    w_out_d = sb_w_out[0] if kind == 1 else fox_w_out[0]
                phase_outproj_ln1_route(li, w_out_d, xin, t_xin, mT, t_mT, gates, dests, t_route)
            phase_experts(li)
            phase_combine_ln2_ple(li, gates, dests, t_route, xout, t_xout)
            xin, t_xin = xout, t_xout
        kb.barrier()
    return nc


_NAMES = ["x", "p", "conv_w_in", "conv_w", "conv_w_out", "sb_w_in", "sb_w_out", "fox_w_in", "fox_b_f", "fox_w_out",
          "ln1_g", "ln1_b", "ln2_g", "ln2_b", "router_w", "router_b", "exp_w_gate", "exp_b_gate", "exp_w_up",
          "exp_b_up", "exp_w_down", "exp_b_down", "ple_w_proj", "ple_w_gate", "ple_b_gate"]


def kernel(**inputs):
    B = inputs["x"].shape[0]
    T = inputs["x"].shape[1]
    nc = build_program(T=T)
    in_maps = []
    shared = {k: np.ascontiguousarray(inputs[k], dtype=np.float32) for k in _NAMES if k not in ("x", "p")}
    for b in range(B):
        m = dict(shared)
        m["x"] = np.ascontiguousarray(inputs["x"][b], dtype=np.float32)
        m["p"] = np.ascontiguousarray(inputs["p"][:, b], dtype=np.float32)
        in_maps.append(m)
    res = run_bass_kernel_spmd(nc, in_maps, core_ids=list(range(B)))
    return np.stack([np.asarray(r["out"]) for r in res.results], axis=0).astype(np.float32)
```

```python
import contextlib
import numpy as np
import concourse.bass as bass
import concourse.mybir as mybir
from concourse.bass_utils import run_bass_kernel_spmd

F32 = mybir.dt.float32
BF16 = mybir.dt.bfloat16
I32 = mybir.dt.int32
AF = mybir.ActivationFunctionType
ALU = mybir.AluOpType
AX = mybir.AxisListType

D = 1024
NE = 32
PLE = 256
ALPHA = 8 ** 0.25
LN_EPS = 1e-5
KINDS = (0, 1, 2, 0)
JIDX = (0, 0, 0, 1)


class Tok:
    __slots__ = ("name", "w", "r", "dkey")

    def __init__(self, name):
        self.name = name
        self.w = {}
        self.r = {}
        self.dkey = None


class KB:
    def __init__(self, nc, es):
        self.nc = nc
        self.es = es
        self.eng = dict(pe=nc.tensor, dve=nc.vector, act=nc.scalar, pool=nc.gpsimd, sp=nc.sync)
        self.sems, self.cnt, self.isdma = {}, {}, {}
        self.waited = {k: {} for k in self.eng}
        for k in self.eng:
            self._newsem(k, False)
        self.ntok = 0
        self.free_slots = []
        for i in range(64):
            self._newsem(f"d{i}", True)
            self.free_slots.append(f"d{i}")
        self.live = []
        self.mark = {}

    def _newsem(self, key, isdma):
        self.sems[key] = self.es.enter_context(self.nc.semaphore("s_" + key))
        self.cnt[key] = 0
        self.isdma[key] = isdma

    def tok(self, name="t"):
        self.ntok += 1
        return Tok(f"{name}{self.ntok}")

    def _wait(self, e, evs):
        need = {}
        for ev in evs:
            for k, v in ev.items():
                if self.isdma[k]:
                    if v <= self.mark.get(k, 0):
                        continue
                    v = self.cnt[k]
                if v > need.get(k, 0):
                    need[k] = v
        for k, v in need.items():
            if self.waited[e].get(k, 0) >= v:
                continue
            self.eng[e].wait_ge(self.sems[k], v)
            self.waited[e][k] = v

    def op(self, e, emit, reads=(), writes=()):
        evs = [t.w for t in reads] + [t.w for t in writes] + [t.r for t in writes]
        self._wait(e, evs)
        inst = emit(self.eng[e])
        self.cnt[e] += 1
        inst.then_inc(self.sems[e], 1)
        v = self.cnt[e]
        for t in reads:
            t.r[e] = v
        for t in writes:
            t.w = {e: v}
            t.r = {}
        return inst

    def dma(self, q, emit, reads=(), writes=(), key=None, join=False):
        evs = [t.w for t in reads] + [t.r for t in writes]
        if not join:
            evs += [t.w for t in writes]
        self._wait(q, evs)
        if key.dkey is None:
            key.dkey = self.free_slots.pop(0)
            self.live.append(key)
        k = key.dkey
        inst = emit(self.eng[q])
        self.cnt[k] += 16
        inst.then_inc(self.sems[k], 16)
        v = self.cnt[k]
        for t in reads:
            t.r[k] = v
        for t in writes:
            if join:
                t.w[k] = v
            else:
                t.w = {k: v}
                t.r = {}
        return inst

    def barrier(self):
        allev = [dict(self.cnt)]
        for e in self.eng:
            self._wait(e, allev)
        self.mark = {k: v for k, v in self.cnt.items() if self.isdma[k]}
        for t in self.live:
            self.free_slots.append(t.dkey)
            t.dkey = None
        self.live = []


def _cfg_caps(T):
    return {4096: 640, 512: 128, 1024: 256}[T]


def build_program(T=4096, layers=(0, 1, 2, 3), dbg=False):
    C = _cfg_caps(T)
    NT = T // 128
    NQ = T // 512
    CR = C // 128
    NSLOT = NE * C
    nc = bass.Bass("TRN2", target_bir_lowering=False)

    def din(name, shape, dt=F32):
        return nc.dram_tensor(name, list(shape), dt, kind="ExternalInput").ap()

    def dscr(name, shape, dt=F32):
        return nc.dram_tensor(name, list(shape), dt).ap()

    x_in = din("x", [T, D])
    p_in = din("p", [4, T, PLE])
    conv_w_in = din("conv_w_in", [2, D, 3 * D])
    conv_w = din("conv_w", [2, 3, D])
    conv_w_out = din("conv_w_out", [2, D, D])
    sb_w_in = din("sb_w_in", [1, D, 3 * D])
    sb_w_out = din("sb_w_out", [1, D, D])
    fox_w_in = din("fox_w_in", [1, D, 3 * D + 16])
    fox_b_f = din("fox_b_f", [1, 16])
    fox_w_out = din("fox_w_out", [1, D, D])
    ln1_g = din("ln1_g", [4, D]); ln1_b = din("ln1_b", [4, D])
    ln2_g = din("ln2_g", [4, D]); ln2_b = din("ln2_b", [4, D])
    router_w = din("router_w", [4, D, NE]); router_b = din("router_b", [4, NE])
    exp_w_gate = din("exp_w_gate", [4, NE, D, D]); exp_b_gate = din("exp_b_gate", [4, NE, D])
    exp_w_up = din("exp_w_up", [4, NE, D, D]); exp_b_up = din("exp_b_up", [4, NE, D])
    exp_w_down = din("exp_w_down", [4, NE, D, D]); exp_b_down = din("exp_b_down", [4, NE, D])
    ple_w_proj = din("ple_w_proj", [4, PLE, D]); ple_w_gate = din("ple_w_gate", [4, D, D])
    ple_b_gate = din("ple_b_gate", [4, D])
    out_d = nc.dram_tensor("out", [T, D], F32, kind="ExternalOutput").ap()

    xres = [dscr("xres0", [T, D]), dscr("xres1", [T, D])]
    x1res = dscr("x1res", [T, D])
    x1bf = dscr("x1bf", [T, D], BF16)
    yslots = dscr("yslots", [NSLOT + 128, D])
    toklist = dscr("toklist", [NSLOT + 128, 1], I32)
    qT_d = dscr("qT_d", [8, 128, T], BF16)
    kT_d = dscr("kT_d", [8, 128, T], BF16)
    v_d = dscr("v_d", [T, D], BF16)
    nrow_d = dscr("nrow_d", [16, 3, T], BF16)
    dbg_d = None
    if dbg:
        dbg_d = nc.dram_tensor("dbg", [T, D], F32, kind="ExternalOutput").ap()

    es = contextlib.ExitStack()
    with es:
        kb = KB(nc, es)
        op, dma = kb.op, kb.dma

        uid = [0]

        def sb(name, shape, dt=F32, scope=es):
            uid[0] += 1
            return scope.enter_context(nc.sbuf_tensor(f"{name}_{uid[0]}", list(shape), dt))

        def ps(name, shape, dt=F32, scope=es):
            uid[0] += 1
            return scope.enter_context(nc.psum_tensor(f"{name}_{uid[0]}", list(shape), dt))

        t_const = kb.tok("const")
        ident32 = sb("ident32", [128, 128])
        identb = sb("identb", [128, 128], BF16)
        ones_b = sb("ones_b", [128, 128], BF16)
        tri_lt = sb("tri_lt", [128, 128], BF16)
        ntri_ge = sb("ntri_ge", [128, 128], BF16)
        nones_b = sb("nones_b", [128, 128], BF16)
        mask_lt = sb("mask_lt", [128, 128], BF16)
        mask_le = sb("mask_le", [128, 128], BF16)
        c_one = sb("c_one", [128, 1])
        c_eps = sb("c_eps", [128, 1])
        c_nhalf = sb("c_nhalf", [128, 1])
        tokid = sb("tokid", [128, NT], I32)
        ecol = sb("ecol", [128, NE])
        zrow = sb("zrow", [128, D])
        zi = sb("zi", [128, (NSLOT + 128) // 128], I32)
        tmp32 = sb("tmp32", [128, 128])

        def mk_tri(dst, cmp, mult_p, mult_f, fillv, inv):
            def e1(g):
                return g.memset(tmp32[:], inv)
            op("pool", e1, writes=[t_const])

            def e2(g):
                return g.affine_select(out=tmp32[:], in_=tmp32[:], pattern=[[mult_f, 128]], compare_op=cmp,
                                       fill=fillv, base=0, channel_multiplier=mult_p)
            op("pool", e2, writes=[t_const])
            op("dve", lambda v: v.tensor_copy(out=dst[:], in_=tmp32[:]), reads=[t_const], writes=[t_const])

        mk_tri(ident32, ALU.not_equal, 1, -1, 1.0, 0.0)
        mk_tri(identb, ALU.not_equal, 1, -1, 1.0, 0.0)
        mk_tri(tri_lt, ALU.is_gt, -1, 1, 0.0, 1.0)
        mk_tri(mask_lt, ALU.is_gt, -1, 1, 0.0, 1.0)
        mk_tri(mask_le, ALU.is_ge, -1, 1, 0.0, 1.0)
        mk_tri(ntri_ge, ALU.is_ge, 1, -1, 0.0, -1.0)
        zeros_b = sb("zeros_b", [128, 128], BF16)
        sel0 = sb("sel0", [128, 128], BF16)
        mk_tri(sel0, ALU.is_equal, 1, 0, 0.0, 1.0)
        negmask_gt = sb("negmask_gt", [128, 128], BF16)
        mk_tri(negmask_gt, ALU.is_gt, 1, -1, 0.0, -30000.0)
        selh = sb("selh", [16, 16, 128], BF16)
        selt = sb("selt", [16, 16, 128])
        op("pool", lambda g: g.memset(selt[:], 1.0), writes=[t_const])
        op("pool", lambda g: g.affine_select(out=selt[:], in_=selt[:], pattern=[[-1, 16], [0, 128]], compare_op=ALU.is_equal,
                                             fill=0.0, base=0, channel_multiplier=1), writes=[t_const])
        op("dve", lambda v: v.tensor_copy(out=selh[:], in_=selt[:]), reads=[t_const], writes=[t_const])
        zsrc = sb("zsrc", [128, 512], BF16)
        op("pool", lambda g: g.memset(zsrc[:], 0.0), writes=[t_const])
        op("pool", lambda g: g.memset(zeros_b[:], 0.0), writes=[t_const])
        op("pool", lambda g: g.memset(ones_b[:], 1.0), writes=[t_const])
        op("pool", lambda g: g.memset(nones_b[:], -1.0), writes=[t_const])
        op("pool", lambda g: g.memset(c_one[:], 1.0), writes=[t_const])
        op("pool", lambda g: g.memset(c_eps[:], LN_EPS), writes=[t_const])
        op("pool", lambda g: g.memset(c_nhalf[:], -0.5), writes=[t_const])
        op("pool", lambda g: g.memset(zrow[:], 0.0), writes=[t_const])
        op("pool", lambda g: g.memset(zi[:], 0), writes=[t_const])
        op("pool", lambda g: g.iota(tokid[:], pattern=[[128, NT]], base=0, channel_multiplier=1), writes=[t_const])
        op("pool", lambda g: g.iota(ecol[:], pattern=[[C, NE]], base=0, channel_multiplier=0,
                                    allow_small_or_imprecise_dtypes=True), writes=[t_const])
        t_ys = kb.tok("yslots")
        t_tl = kb.tok("toklist")
        dma("sp", lambda q: q.dma_start(out=yslots[NSLOT:NSLOT + 128, :], in_=zrow[:]), reads=[t_const],
            writes=[t_ys], key=t_ys, join=True)

        t_xres = [kb.tok("xres0"), kb.tok("xres1")]
        t_x1res = kb.tok("x1res")
        t_x1bf = kb.tok("x1bf")
        t_qkv = kb.tok("qkv")
        t_out = kb.tok("out")

        def layer_norm_tile(h, g_bc, b_bc, xo, stat, t_h, t_xo, t_stat, t_gb, eng2="pool"):
            def s1(v):
                v.bn_stats(out=stat[:, 0:6], in_=h[:, 0:512])
                return v.bn_stats(out=stat[:, 6:12], in_=h[:, 512:1024])
            op("dve", s1, reads=[t_h], writes=[t_stat])
            op("dve", lambda v: v.bn_aggr(out=stat[:, 12:14], in_=stat[:, 0:12]), reads=[t_stat], writes=[t_stat])
            op("pool", lambda g: g.tensor_tensor(out=stat[:, 14:15], in0=stat[:, 13:14], in1=c_eps[:], op=ALU.add),
               reads=[t_stat, t_const], writes=[t_stat])
            op("pool", lambda g: g.tensor_tensor(out=stat[:, 15:16], in0=stat[:, 14:15], in1=c_nhalf[:], op=ALU.pow),
               reads=[t_stat, t_const], writes=[t_stat])
            op("dve", lambda v: v.tensor_scalar(out=xo[:], in0=h[:], scalar1=stat[:, 12:13], scalar2=stat[:, 15:16],
                                                op0=ALU.subtract, op1=ALU.mult),
               reads=[t_h, t_stat], writes=[t_xo])
            op(eng2, lambda g: g.tensor_tensor(out=xo[:], in0=xo[:], in1=g_bc[:], op=ALU.mult),
               reads=[t_gb], writes=[t_xo])
            op(eng2, lambda g: g.tensor_tensor(out=xo[:], in0=xo[:], in1=b_bc[:], op=ALU.add),
               reads=[t_gb], writes=[t_xo])

        def bcast_load(dst, src_row, t_dst, q="sp"):
            n = dst.shape[-1] if hasattr(dst, "shape") else None
            dma(q, lambda e: e.dma_start(out=dst[:], in_=src_row.partition_broadcast(128)), writes=[t_dst], key=t_dst)

        def phase_xT(xin, t_xin, xT, t_xT):
            with contextlib.ExitStack() as sc:
                xs = [sb(f"xs{i}", [128, D], scope=sc) for i in range(2)]
                xb = [sb(f"xb{i}", [128, D], BF16, scope=sc) for i in range(2)]
                pT = [ps(f"pT{i}", [128, 8, 128], BF16, scope=sc) for i in range(2)]
                t_xs = [kb.tok("xs") for _ in range(2)]
                t_xb = [kb.tok("xb") for _ in range(2)]
                t_pT = [kb.tok("pT") for _ in range(2)]
                for i in range(NT):
                    b = i % 2
                    dma("sp", lambda q: q.dma_start(out=xs[b][:], in_=xin[i * 128:(i + 1) * 128, :]),
                        reads=[t_xin], writes=[t_xs[b]], key=t_xs[b])
                    op("act", lambda a: a.copy(out=xb[b][:], in_=xs[b][:]), reads=[t_xs[b]], writes=[t_xb[b]])

                    def tr(pe):
                        for c in range(8):
                            r = pe.transpose(out=pT[b][:, c, :], in_=xb[b][:, c * 128:(c + 1) * 128], identity=identb[:])
                        return r
                    op("pe", tr, reads=[t_xb[b], t_const], writes=[t_pT[b]])
                    op("dve", lambda v: v.tensor_copy(out=xT[:, :, i * 128:(i + 1) * 128], in_=pT[b][:]),
                       reads=[t_pT[b]], writes=[t_xT])
                kb.barrier()

        def phase_conv(j, xT, t_xT, mT, t_mT):
            with contextlib.ExitStack() as sc:
                wc = [sb(f"wc{i}", [128, 8, 384], BF16, scope=sc) for i in range(2)]
                t_wc = [kb.tok("wc") for _ in range(2)]
                cw = sb("cw", [128, 8, 3], scope=sc)
                cwr = sb("cwr", [3, D], scope=sc)
                cwb = sb("cwb", [3, D], BF16, scope=sc)
                t_cw = kb.tok("cw")
                pcw = ps("pcw", [128, 8, 4], BF16, scope=sc)
                t_pcw = kb.tok("pcw")
                pB = [ps(f"pB{i}", [128, 512], scope=sc) for i in range(2)]
                pC = [ps(f"pC{i}", [128, 512], scope=sc) for i in range(2)]
                pH = [ps(f"pH{i}", [128, 512], scope=sc) for i in range(2)]
                t_pB = [kb.tok("pB") for _ in range(2)]
                t_pC = [kb.tok("pC") for _ in range(2)]
                t_pH = [kb.tok("pH") for _ in range(2)]
                ctmp = [sb(f"ctmp{i}", [128, 512], scope=sc) for i in range(2)]
                t_ct = [kb.tok("ct") for _ in range(2)]
                u = [sb(f"u{i}", [128, 514], scope=sc) for i in range(2)]
                t_u = [kb.tok("u") for _ in range(2)]
                cv = [sb(f"cv{i}", [128, 512], scope=sc) for i in range(2)]
                t_cv = [kb.tok("cv") for _ in range(2)]
                cwlo = sb("cwlo", [3, D], BF16, scope=sc)
                cwt = sb("cwt", [3, D], scope=sc)
                dma("sp", lambda q: q.dma_start(out=cwr[:], in_=conv_w[j]), writes=[t_cw], key=t_cw)
                op("dve", lambda v: v.tensor_copy(out=cwb[:], in_=cwr[:]), reads=[t_cw], writes=[t_cw])
                op("dve", lambda v: v.tensor_copy(out=cwt[:], in_=cwb[:]), reads=[t_cw], writes=[t_cw])
                op("dve", lambda v: v.tensor_tensor(out=cwt[:], in0=cwr[:], in1=cwt[:], op=ALU.subtract), writes=[t_cw])
                op("dve", lambda v: v.tensor_copy(out=cwlo[:], in_=cwt[:]), writes=[t_cw])
                for part, src in ((0, cwb), (1, cwlo)):
                    def tr(pe):
                        for c in range(8):
                            r = pe.transpose(out=pcw[:, c, 0:3], in_=src[:, c * 128:(c + 1) * 128], identity=identb[0:3, 0:3])
                        return r
                    op("pe", tr, reads=[t_cw, t_const], writes=[t_pcw])
                    if part == 0:
                        op("dve", lambda v: v.tensor_copy(out=cw[:], in_=pcw[:, :, 0:3]), reads=[t_pcw], writes=[t_cw])
                    else:
                        op("dve", lambda v: v.tensor_tensor(out=cw[:], in0=cw[:], in1=pcw[:, :, 0:3], op=ALU.add),
                           reads=[t_pcw], writes=[t_cw])
                wsrc = conv_w_in[j].rearrange("(kc p) n -> p kc n", p=128)

                def load_w(c):
                    b = c % 2
                    for g in range(3):
                        dma("pool", lambda q: q.dma_start(out=wc[b][:, :, g * 128:(g + 1) * 128],
                                                          in_=wsrc[:, :, g * 1024 + c * 128:g * 1024 + (c + 1) * 128]),
                            writes=[t_wc[b]], key=t_wc[b], join=(g > 0))
                load_w(0)
                it = 0
                for c in range(8):
                    if c + 1 < 8:
                        load_w(c + 1)
                    wb_ = wc[c % 2]
                    for tq in range(NQ):
                        b = it % 2
                        it += 1
                        for (pp, tp, g) in ((pB[b], t_pB[b], 0), (pC[b], t_pC[b], 1), (pH[b], t_pH[b], 2)):
                            def mm(pe):
                                for kc in range(8):
                                    r = pe.matmul(pp[:], lhsT=wb_[:, kc, g * 128:(g + 1) * 128],
                                                  rhs=xT[:, kc, tq * 512:(tq + 1) * 512], start=(kc == 0), stop=(kc == 7))
                                return r
                            op("pe", mm, reads=[t_wc[c % 2], t_xT], writes=[tp])
                        op("act", lambda a: a.copy(out=ctmp[b][:], in_=pC[b][:]), reads=[t_pC[b]], writes=[t_ct[b]])
                        if tq == 0:
                            op("pool", lambda g_: g_.memset(u[b][:, 0:2], 0.0), writes=[t_u[b]])
                        else:
                            op("pool", lambda g_: g_.tensor_copy(out=u[b][:, 0:2], in_=u[1 - b][:, 512:514]),
                               reads=[t_u[1 - b]], writes=[t_u[b]])
                        op("dve", lambda v: v.tensor_tensor(out=u[b][:, 2:514], in0=ctmp[b][:], in1=pH[b][:], op=ALU.mult),
                           reads=[t_ct[b], t_pH[b]], writes=[t_u[b]])
                        op("dve", lambda v: v.tensor_scalar(out=cv[b][:], in0=u[b][:, 2:514], scalar1=cw[:, c, 2:3],
                                                            scalar2=None, op0=ALU.mult),
                           reads=[t_u[b], t_cw], writes=[t_cv[b]])
                        op("dve", lambda v: v.scalar_tensor_tensor(out=cv[b][:], in0=u[b][:, 1:513], scalar=cw[:, c, 1:2],
                                                                   in1=cv[b][:], op0=ALU.mult, op1=ALU.add),
                           reads=[t_u[b], t_cw], writes=[t_cv[b]])
                        op("dve", lambda v: v.scalar_tensor_tensor(out=cv[b][:], in0=u[b][:, 0:512], scalar=cw[:, c, 0:1],
                                                                   in1=cv[b][:], op0=ALU.mult, op1=ALU.add),
                           reads=[t_u[b], t_cw], writes=[t_cv[b]])
                        op("dve", lambda v: v.tensor_tensor(out=mT[:, c, tq * 512:(tq + 1) * 512], in0=cv[b][:],
                                                            in1=pB[b][:], op=ALU.mult),
                           reads=[t_cv[b], t_pB[b]], writes=[t_mT])
                kb.barrier()

        def phase_qkv(kind, w_in_d, xT, t_xT, ncum, refB, t_cum):
            with contextlib.ExitStack() as sc:
                wsrc = w_in_d.rearrange("(kc p) n -> p kc n", p=128)
                wq = [sb(f"wq{i}", [128, 8, 512], BF16, scope=sc) for i in range(2)]; t_wq = [kb.tok("wq") for _ in range(2)]
                wv = sb("wv", [128, 8, D], BF16, scope=sc); t_wv = kb.tok("wv")
                stg = [sb(f"stg{i}", [128, T], BF16, scope=sc) for i in range(2)]; t_stg = [kb.tok("stg") for _ in range(2)]
                vst = [sb(f"vst{i}", [128, D], BF16, scope=sc) for i in range(2)]; t_vst = [kb.tok("vst") for _ in range(2)]
                pq = [ps(f"pq{i}", [128, 512], scope=sc) for i in range(2)]; t_pq = [kb.tok("pq") for _ in range(2)]
                pv = [ps(f"pv{i}", [128, 512], scope=sc) for i in range(2)]; t_pv = [kb.tok("pv") for _ in range(2)]
                dma("pool", lambda q: q.dma_start(out=wv[:], in_=wsrc[:, :, 2048:3072]), writes=[t_wv], key=t_wv)

                def load_g(g):
                    dma("pool", lambda q: q.dma_start(out=wq[g % 2][:], in_=wsrc[:, :, g * 512:(g + 1) * 512]),
                        writes=[t_wq[g % 2]], key=t_wq[g % 2])
                load_g(0)
                it = 0
                for g in range(4):
                    if g + 1 < 4:
                        load_g(g + 1)
                    for cc in range(4):
                        c16 = g * 4 + cc
                        sbuf_ = stg[c16 % 2]; ts_ = t_stg[c16 % 2]
                        for tq in range(NQ):
                            b = it % 2
                            it += 1

                            def mm(pe):
                                for kc in range(8):
                                    r = pe.matmul(pq[b][:], lhsT=wq[g % 2][:, kc, cc * 128:(cc + 1) * 128],
                                                  rhs=xT[:, kc, tq * 512:(tq + 1) * 512], start=(kc == 0), stop=(kc == 7))
                                return r
                            op("pe", mm, reads=[t_wq[g % 2], t_xT], writes=[t_pq[b]])
                            scl = 0.125 if c16 < 8 else 1.0
                            if it % 2 == 0:
                                op("act", lambda a: a.mul(out=sbuf_[:, tq * 512:(tq + 1) * 512], in_=pq[b][:], mul=scl),
                                   reads=[t_pq[b]], writes=[ts_])
                            else:
                                op("dve", lambda v: v.tensor_scalar(out=sbuf_[:, tq * 512:(tq + 1) * 512], in0=pq[b][:], scalar1=scl,
                                                                    scalar2=None, op0=ALU.mult), reads=[t_pq[b]], writes=[ts_])
                        dst = qT_d[c16] if c16 < 8 else kT_d[c16 - 8]
                        dma("sp", lambda q: q.dma_start(out=dst, in_=sbuf_[:]), reads=[ts_], writes=[t_qkv], key=ts_, join=True)
                for i in range(NT):
                    b = i % 2
                    for hh in range(2):
                        def mmv(pe):
                            for kc in range(8):
                                r = pe.matmul(pv[hh][:], lhsT=xT[:, kc, i * 128:(i + 1) * 128],
                                              rhs=wv[:, kc, hh * 512:(hh + 1) * 512], start=(kc == 0), stop=(kc == 7))
                            return r
                        op("pe", mmv, reads=[t_xT, t_wv], writes=[t_pv[hh]])
                        if hh == 0:
                            op("act", lambda a: a.copy(out=vst[b][:, 0:512], in_=pv[hh][:]), reads=[t_pv[hh]], writes=[t_vst[b]])
                        else:
                            op("dve", lambda v: v.tensor_copy(out=vst[b][:, 512:1024], in_=pv[hh][:]), reads=[t_pv[hh]], writes=[t_vst[b]])
                    dma("sp", lambda q: q.dma_start(out=v_d[i * 128:(i + 1) * 128, :], in_=vst[b][:]), reads=[t_vst[b]],
                        writes=[t_qkv], key=t_vst[b], join=True)
                if kind == 2:
                    wf = sb("wf", [128, 8, 16], BF16, scope=sc); t_wf = kb.tok("wf")
                    bf_bc = sb("bf_bc", [128, 16], scope=sc)
                    basef = sb("basef", [128, 16], scope=sc); t_bf = kb.tok("basef")
                    fl = [sb(f"fl{i}", [128, 64], scope=sc) for i in range(2)]; t_fl = [kb.tok("fl") for _ in range(2)]
                    flb = [sb(f"flb{i}", [128, 32], BF16, scope=sc) for i in range(2)]; t_flb = [kb.tok("flb") for _ in range(2)]
                    pf = ps("pf", [128, 16], scope=sc); t_pf = kb.tok("pf")
                    pc = ps("pc", [128, 2, 16], scope=sc); t_pc = kb.tok("pc")
                    ptr = ps("ptr", [128, 3, 128], BF16, scope=sc); t_ptr = kb.tok("ptr")
                    nsp = [sb(f"nsp{i}", [128, 3, 16], BF16, scope=sc) for i in range(2)]; t_nsp = [kb.tok("nsp") for _ in range(2)]
                    nr = [sb(f"nr{i}", [128, 48], scope=sc) for i in range(2)]
                    dma("pool", lambda q: q.dma_start(out=wf[:], in_=wsrc[:, :, 3072:3088]), writes=[t_wf], key=t_wf)
                    dma("sp", lambda q: q.dma_start(out=bf_bc[:], in_=fox_b_f[0].partition_broadcast(128)), writes=[t_wf], key=t_wf, join=True)
                    op("pool", lambda g_: g_.memset(basef[:], 0.0), writes=[t_bf])
                    for i in range(NT):
                        b = i % 2
                        F = fl[b]

                        def mmf(pe):
                            for kc in range(8):
                                r = pe.matmul(pf[:], lhsT=xT[:, kc, i * 128:(i + 1) * 128], rhs=wf[:, kc, :], start=(kc == 0), stop=(kc == 7))
                            return r
                        op("pe", mmf, reads=[t_xT, t_wf], writes=[t_pf])
                        op("dve", lambda v: v.tensor_tensor(out=F[:, 0:16], in0=pf[:], in1=bf_bc[:], op=ALU.add), reads=[t_pf, t_wf], writes=[t_fl[b]])
                        op("act", lambda a: a.activation(out=F[:, 16:32], in_=F[:, 0:16], func=AF.Exp, scale=-1.0), writes=[t_fl[b]])
                        op("act", lambda a: a.activation(out=F[:, 32:48], in_=F[:, 16:32], func=AF.Ln, bias=c_one[:], scale=1.0),
                           reads=[t_const], writes=[t_fl[b]])
                        op("dve", lambda v: v.tensor_copy(out=flb[b][:, 0:16], in_=F[:, 32:48]), reads=[t_fl[b]], writes=[t_flb[b]])
                        op("dve", lambda v: v.tensor_copy(out=F[:, 48:64], in_=flb[b][:, 0:16]), writes=[t_fl[b]])
                        op("dve", lambda v: v.tensor_tensor(out=F[:, 48:64], in0=F[:, 32:48], in1=F[:, 48:64], op=ALU.subtract), writes=[t_fl[b]])
                        op("dve", lambda v: v.tensor_copy(out=flb[b][:, 16:32], in_=F[:, 48:64]), reads=[t_fl[b]], writes=[t_flb[b]])

                        def mmc(pe):
                            pe.matmul(pc[:, 0, :], lhsT=mask_le[:], rhs=flb[b][:, 0:16], start=True, stop=False)
                            pe.matmul(pc[:, 0, :], lhsT=mask_le[:], rhs=flb[b][:, 16:32], start=False, stop=True)
                            pe.matmul(pc[:, 1, :], lhsT=ones_b[:], rhs=flb[b][:, 0:16], start=True, stop=False)
                            return pe.matmul(pc[:, 1, :], lhsT=ones_b[:], rhs=flb[b][:, 16:32], start=False, stop=True)
                        op("pe", mmc, reads=[t_flb[b], t_const], writes=[t_pc])
                        op("dve", lambda v: v.tensor_tensor(out=ncum[:, i, :], in0=pc[:, 0, :], in1=basef[:], op=ALU.add),
                           reads=[t_pc, t_bf], writes=[t_cum])
                        op("dve", lambda v: v.tensor_tensor(out=basef[:], in0=basef[:], in1=pc[:, 1, :], op=ALU.add),
                           reads=[t_pc], writes=[t_bf])
                        N3 = nsp[b]
                        R_ = nr[b]
                        op("dve", lambda v: v.tensor_copy(out=N3[:, 0, :], in_=ncum[:, i, :]), reads=[t_cum], writes=[t_nsp[b]])
                        op("dve", lambda v: v.tensor_copy(out=R_[:, 0:16], in_=N3[:, 0, :]), writes=[t_nsp[b]])
                        op("dve", lambda v: v.tensor_tensor(out=R_[:, 16:32], in0=ncum[:, i, :], in1=R_[:, 0:16], op=ALU.subtract), writes=[t_nsp[b]])
                        op("dve", lambda v: v.tensor_copy(out=N3[:, 1, :], in_=R_[:, 16:32]), writes=[t_nsp[b]])
                        op("dve", lambda v: v.tensor_copy(out=R_[:, 0:16], in_=N3[:, 1, :]), writes=[t_nsp[b]])
                        op("dve", lambda v: v.tensor_tensor(out=R_[:, 32:48], in0=R_[:, 16:32], in1=R_[:, 0:16], op=ALU.subtract), writes=[t_nsp[b]])
                        op("dve", lambda v: v.tensor_copy(out=N3[:, 2, :], in_=R_[:, 32:48]), writes=[t_nsp[b]])

                        def trn(pe):
                            for g_ in range(3):
                                r = pe.transpose(out=ptr[0:16, g_, :], in_=N3[:, g_, :], identity=identb[:])
                            return r
                        op("pe", trn, reads=[t_nsp[b], t_const], writes=[t_ptr])
                        op("dve", lambda v: v.tensor_scalar(out=refB[0:16, :, i * 128:(i + 1) * 128], in0=ptr[0:16, :, :], scalar1=-1.0,
                                                            scalar2=None, op0=ALU.mult), reads=[t_ptr], writes=[t_cum])
                    dma("sp", lambda q: q.dma_start(out=nrow_d, in_=refB[0:16, :, :]), reads=[t_cum], writes=[t_qkv], key=t_cum, join=True)
                kb.barrier()

        def phase_attn(kind, mT, t_mT, ncum, refB, t_cum):
            with contextlib.ExitStack() as sc:
                KQ = 67 if kind == 2 else 64
                qs = [sb(f"qs{i}", [128, T], BF16, scope=sc) for i in range(2)]
                ks = [sb(f"ks{i}", [128, T], BF16, scope=sc) for i in range(2)]
                vs = [sb(f"vs{i}", [128, NT, 128], BF16, scope=sc) for i in range(2)]
                t_q = [kb.tok("q") for _ in range(2)]; t_k = [kb.tok("k") for _ in range(2)]; t_v = [kb.tok("v") for _ in range(2)]
                NB = 3
                ex = [sb(f"ex{i}", [128, 512], scope=sc) for i in range(NB)]; t_ex = [kb.tok("ex") for _ in range(NB)]
                spb = [sb(f"spb{i}", [128, 512], BF16, scope=sc) for i in range(NB)]; t_sp = [kb.tok("sp") for _ in range(NB)]
                wt = [sb(f"wt{i}", [128, 512], BF16, scope=sc) for i in range(NB)]; t_wt = [kb.tok("wt") for _ in range(NB)]
                acc = sb("acc", [128, 512], BF16, scope=sc); t_acc = kb.tok("acc")
                rden = sb("rden", [128, 512], scope=sc); t_rden = kb.tok("rden")
                pst = [ps(f"pst{i}", [128, 512], scope=sc) for i in range(2)]; t_pst = [kb.tok("pst") for _ in range(2)]
                parg = [ps(f"parg{i}", [128, 512], scope=sc) for i in range(2)]; t_parg = [kb.tok("parg") for _ in range(2)]
                po = [ps(f"po{i}", [128, 512], scope=sc) for i in range(2)]; t_po = [kb.tok("po") for _ in range(2)]
                pden = ps("pden", [128, 512], scope=sc); t_pden = kb.tok("pden")
                pfill = pden; t_fill = kb.tok("fill")
                vsrc = v_d.rearrange("(j p) d -> p j d", p=128)
                if kind == 2:
                    for b in range(2):
                        op("pool", lambda g: g.memset(ks[b][64:67, :], 1.0), writes=[t_k[b]])

                def load_h(hd):
                    b = hd % 2
                    c, hh = hd // 2, hd % 2
                    dma("sp", lambda q: q.dma_start(out=qs[b][0:64, :], in_=qT_d[c, hh * 64:(hh + 1) * 64, :]), reads=[t_qkv],
                        writes=[t_q[b]], key=t_q[b])
                    if kind == 2:
                        dma("sp", lambda q: q.dma_start(out=qs[b][64:67, :], in_=nrow_d[hd]), reads=[t_qkv],
                            writes=[t_q[b]], key=t_q[b], join=True)
                    dma("sp", lambda q: q.dma_start(out=ks[b][0:64, :], in_=kT_d[c, hh * 64:(hh + 1) * 64, :]), reads=[t_qkv],
                        writes=[t_k[b]], key=t_k[b], join=True)
                    if hh == 0:
                        dma("sp", lambda q: q.dma_start(out=vs[c % 2][:], in_=vsrc[:, :, c * 128:(c + 1) * 128]), reads=[t_qkv],
                            writes=[t_v[c % 2]], key=t_v[c % 2])
                load_h(0)
                it = 0
                grp = 0
                for hd in range(16):
                    c, hh = hd // 2, hd % 2
                    hb = hd % 2
                    cb = c % 2
                    P0 = hh * 64
                    if hd + 1 < 16:
                        load_h(hd + 1)
                    for tq in range(NQ):
                        jmax = 4 * tq + 3
                        pob = po[grp % 2]; t_pob = t_po[grp % 2]
                        grp += 1
                        if kind != 2:
                            op("pool", lambda g: g.memset(acc[:], 0.0), writes=[t_acc])
                        op("pe", lambda pe: pe.matmul(pob[:], lhsT=zeros_b[:], rhs=zsrc[:], start=True, stop=False),
                           reads=[t_const], writes=[t_pob])
                        if kind == 2:
                            op("pe", lambda pe: pe.matmul(pden[:], lhsT=zeros_b[:], rhs=zsrc[:], start=True, stop=False),
                               reads=[t_const], writes=[t_pden])
                        order = list(range(jmax + 1)) if kind == 2 else list(range(jmax, -1, -1))
                        ntile = len(order)
                        info = {}

                        def stage1(nj):
                            j = order[nj]
                            b2 = (it + nj) % 2
                            b3 = (it + nj) % NB
                            col0 = max(0, j - 4 * tq) * 128
                            diag = j >= 4 * tq
                            q0 = tq * 512 + col0
                            info[nj] = (j, b2, b3, col0, diag, q0)

                            def mms(pe):
                                r = pe.matmul(pst[b2][:, col0:512], lhsT=ks[hb][0:KQ, j * 128:(j + 1) * 128],
                                              rhs=qs[hb][0:KQ, q0:(tq + 1) * 512], start=True, stop=not (kind == 2 and diag))
                                if kind == 2 and diag:
                                    r = pe.matmul(pst[b2][:, col0:col0 + 128], lhsT=identb[:], rhs=negmask_gt[:], start=False, stop=True)
                                return r
                            op("pe", mms, reads=[t_q[hb], t_k[hb], t_const], writes=[t_pst[b2]])
                            if kind == 2:
                                op("act", lambda a: a.activation(out=wt[b3][:, col0:512], in_=pst[b2][:, col0:512], func=AF.Exp,
                                                                 bias=ncum[:, j, hd:hd + 1], scale=1.0),
                                   reads=[t_pst[b2], t_cum], writes=[t_wt[b3]])
                            else:
                                op("act", lambda a: a.activation(out=ex[b3][:, col0:512], in_=pst[b2][:, col0:512], func=AF.Exp),
                                   reads=[t_pst[b2]], writes=[t_ex[b3]])
                                op("act", lambda a: a.activation(out=spb[b3][:, col0:512], in_=ex[b3][:, col0:512], func=AF.Ln,
                                                                 bias=c_one[:], scale=1.0), reads=[t_ex[b3], t_const], writes=[t_sp[b3]])
                                if diag:
                                    op("pool", lambda g: g.tensor_tensor(out=spb[b3][:, col0:col0 + 128], in0=spb[b3][:, col0:col0 + 128],
                                                                         in1=mask_lt[:], op=ALU.mult), reads=[t_const], writes=[t_sp[b3]])

                        def stage2(nj):
                            (j, b2, b3, col0, diag, q0) = info[nj]

                            def mma(pe):
                                pe.matmul(parg[b2][:, col0:512], lhsT=ks[hb][0:64, j * 128:(j + 1) * 128],
                                          rhs=qs[hb][0:64, q0:(tq + 1) * 512], start=True, stop=False)
                                pe.matmul(parg[b2][:, col0:512], lhsT=ntri_ge[:], rhs=spb[b3][:, col0:512], start=False, stop=False)
                                return pe.matmul(parg[b2][:, col0:512], lhsT=nones_b[:], rhs=acc[:, col0:512], start=False, stop=True)
                            op("pe", mma, reads=[t_q[hb], t_k[hb], t_sp[b3], t_acc, t_const], writes=[t_parg[b2]])

                            def fill(pe):
                                pe.matmul(pfill[:], lhsT=zeros_b[:], rhs=zsrc[:], start=True, stop=True)
                                return pe.matmul(pfill[:], lhsT=zeros_b[:], rhs=zsrc[:], start=True, stop=True)
                            op("pe", fill, reads=[t_const], writes=[t_fill])
                            op("pool", lambda g: g.tensor_tensor(out=acc[:, col0:512], in0=acc[:, col0:512], in1=spb[b3][:, col0:512],
                                                                 op=ALU.add), reads=[t_sp[b3]], writes=[t_acc])
                            op("act", lambda a: a.activation(out=wt[b3][:, col0:512], in_=parg[b2][:, col0:512], func=AF.Exp),
                               reads=[t_parg[b2]], writes=[t_wt[b3]])
                            if diag:
                                op("pool", lambda g: g.tensor_tensor(out=wt[b3][:, col0:col0 + 128], in0=wt[b3][:, col0:col0 + 128],
                                                                     in1=mask_lt[:], op=ALU.mult), reads=[t_const], writes=[t_wt[b3]])

                        def stage3(nj):
                            (j, b2, b3, col0, diag, q0) = info[nj]
                            last = (nj == ntile - 1)

                            def mmo(pe):
                                r = pe.matmul(pob[:, col0:512], lhsT=vs[cb][:, j, :], rhs=wt[b3][:, col0:512], start=False, stop=last)
                                if kind == 2:
                                    r = pe.matmul(pden[:, col0:512], lhsT=ones_b[:], rhs=wt[b3][:, col0:512], start=False, stop=last)
                                return r
                            op("pe", mmo, reads=[t_wt[b3], t_v[cb], t_const], writes=[t_pob] + ([t_pden] if kind == 2 else []))
                            if kind == 2:
                                op("pe", lambda pe: pe.matmul(parg[0][:], lhsT=zeros_b[:], rhs=zsrc[:], start=True, stop=True),
                                   reads=[t_const], writes=[t_fill])

                        if kind == 2:
                            for n in range(ntile + 1):
                                if n < ntile:
                                    stage1(n)
                                if n >= 1:
                                    stage3(n - 1)
                        else:
                            for n in range(ntile + 2):
                                if n < ntile:
                                    stage1(n)
                                if 1 <= n <= ntile:
                                    stage2(n - 1)
                                if n >= 2:
                                    stage3(n - 2)
                        it += ntile
                        osl = mT[P0:P0 + 64, c, tq * 512:(tq + 1) * 512]
                        if kind == 2:
                            op("dve", lambda v: v.reciprocal(out=rden[P0:P0 + 64, :], in_=pden[P0:P0 + 64, :]), reads=[t_pden], writes=[t_rden])
                            op("dve", lambda v: v.tensor_tensor(out=osl, in0=pob[P0:P0 + 64, :], in1=rden[P0:P0 + 64, :], op=ALU.mult),
                               reads=[t_pob, t_rden], writes=[t_mT])
                        else:
                            op("dve", lambda v: v.tensor_copy(out=osl, in_=pob[P0:P0 + 64, :]), reads=[t_pob], writes=[t_mT])
                kb.barrier()

        def phase_outproj_ln1_route(li, w_out_d, xin, t_xin, mT, t_mT, gates, dests, t_route):
            with contextlib.ExitStack() as sc:
                wo = sb("wo", [128, 8, D], BF16, scope=sc); t_wo = kb.tok("wo")
                g_bc = sb("g_bc", [128, D], scope=sc); b_bc = sb("b_bc", [128, D], scope=sc); t_gb = kb.tok("gb")
                wr32 = sb("wr32", [128, 8, NE], scope=sc)
                wrh = sb("wrh", [128, 8, NE], BF16, scope=sc)
                wrl = sb("wrl", [128, 8, NE], BF16, scope=sc)
                wrt = sb("wrt", [128, 8, NE], scope=sc)
                rb_bc = sb("rb_bc", [128, NE], scope=sc)
                t_wr = kb.tok("wr")
                base = sb("base", [128, NE], scope=sc); t_base = kb.tok("base")
                xs = [sb(f"xs{i}", [128, D], scope=sc) for i in range(2)]; t_xs = [kb.tok("xs") for _ in range(2)]
                h = [sb(f"h{i}", [128, D], scope=sc) for i in range(2)]; t_h = [kb.tok("h") for _ in range(2)]
                x1 = [sb(f"x1_{i}", [128, D], scope=sc) for i in range(2)]; t_x1 = [kb.tok("x1") for _ in range(2)]
                x1h = [sb(f"x1h{i}", [128, D], BF16, scope=sc) for i in range(2)]; t_x1h = [kb.tok("x1h") for _ in range(2)]
                x1l = [sb(f"x1l{i}", [128, D], BF16, scope=sc) for i in range(2)]; t_x1l = [kb.tok("x1l") for _ in range(2)]
                x1t = [sb(f"x1t{i}", [128, D], scope=sc) for i in range(2)]
                xTh = [sb(f"xTh{i}", [128, 8, 128], BF16, scope=sc) for i in range(2)]; t_xTh = [kb.tok("xTh") for _ in range(2)]
                xTl = [sb(f"xTl{i}", [128, 8, 128], BF16, scope=sc) for i in range(2)]; t_xTl = [kb.tok("xTl") for _ in range(2)]
                stat = [sb(f"stat{i}", [128, 16], scope=sc) for i in range(2)]; t_stat = [kb.tok("stat") for _ in range(2)]
                rt = [sb(f"rt{i}", [128, 8 * NE], scope=sc) for i in range(2)]; t_rt = [kb.tok("rt") for _ in range(2)]
                mkb = [sb(f"mkb{i}", [128, NE], BF16, scope=sc) for i in range(2)]; t_mkb = [kb.tok("mkb") for _ in range(2)]
                sm = [sb(f"sm{i}", [128, 32], scope=sc) for i in range(2)]
                desti = sb("desti", [128, NT, 4], I32, scope=sc)
                py = [ps(f"py{i}", [128, D], scope=sc) for i in range(2)]; t_py = [kb.tok("py") for _ in range(2)]
                pTh = ps("pTh", [128, 8, 128], BF16, scope=sc); t_pTh = kb.tok("pTh")
                pTl = ps("pTl", [128, 8, 128], BF16, scope=sc); t_pTl = kb.tok("pTl")
                plog = ps("plog", [128, NE], scope=sc); t_plog = kb.tok("plog")
                pcnt = ps("pcnt", [128, 2, NE], scope=sc); t_pcnt = kb.tok("pcnt")

                dma("pool", lambda q: q.dma_start(out=wo[:], in_=w_out_d.rearrange("(kc p) n -> p kc n", p=128)),
                    writes=[t_wo], key=t_wo)
                dma("sp", lambda q: q.dma_start(out=g_bc[:], in_=ln1_g[li].partition_broadcast(128)), writes=[t_gb], key=t_gb)
                dma("sp", lambda q: q.dma_start(out=b_bc[:], in_=ln1_b[li].partition_broadcast(128)), writes=[t_gb], key=t_gb, join=True)
                dma("sp", lambda q: q.dma_start(out=wr32[:], in_=router_w[li].rearrange("(kc p) n -> p kc n", p=128)),
                    writes=[t_wr], key=t_wr)
                dma("sp", lambda q: q.dma_start(out=rb_bc[:], in_=router_b[li].partition_broadcast(128)), writes=[t_wr], key=t_wr, join=True)
                op("dve", lambda v: v.tensor_copy(out=wrh[:], in_=wr32[:]), reads=[t_wr], writes=[t_wr])
                op("dve", lambda v: v.tensor_copy(out=wrt[:], in_=wrh[:]), writes=[t_wr])
                op("dve", lambda v: v.tensor_tensor(out=wrt[:], in0=wr32[:], in1=wrt[:], op=ALU.subtract), writes=[t_wr])
                op("dve", lambda v: v.tensor_copy(out=wrl[:], in_=wrt[:]), writes=[t_wr])
                op("pool", lambda g: g.memset(base[:], 0.0), writes=[t_base])
                dma("sp", lambda q: q.dma_start(out=toklist.rearrange("(p r) o -> p (r o)", p=128), in_=zi[:]),
                    reads=[t_const], writes=[t_tl], key=t_tl)

                for i in range(NT):
                    b = i % 2
                    dma("sp", lambda q: q.dma_start(out=xs[b][:], in_=xin[i * 128:(i + 1) * 128, :]),
                        reads=[t_xin], writes=[t_xs[b]], key=t_xs[b])

                    def mm(pe):
                        for hh in range(2):
                            for c in range(8):
                                r = pe.matmul(py[b][:, hh * 512:(hh + 1) * 512], lhsT=mT[:, c, i * 128:(i + 1) * 128],
                                              rhs=wo[:, c, hh * 512:(hh + 1) * 512], start=(c == 0), stop=(c == 7))
                        return r
                    op("pe", mm, reads=[t_mT, t_wo], writes=[t_py[b]])
                    op("dve", lambda v: v.scalar_tensor_tensor(out=h[b][:], in0=xs[b][:], scalar=ALPHA, in1=py[b][:],
                                                               op0=ALU.mult, op1=ALU.add),
                       reads=[t_xs[b], t_py[b]], writes=[t_h[b]])
                    layer_norm_tile(h[b], g_bc, b_bc, x1[b], stat[b], t_h[b], t_x1[b], t_stat[b], t_gb)
                    dma("sp", lambda q: q.dma_start(out=x1res[i * 128:(i + 1) * 128, :], in_=x1[b][:]),
                        reads=[t_x1[b]], writes=[t_x1res], key=t_x1[b], join=True)
                    op("act", lambda a: a.copy(out=x1h[b][:], in_=x1[b][:]), reads=[t_x1[b]], writes=[t_x1h[b]])
                    dma("sp", lambda q: q.dma_start(out=x1bf[i * 128:(i + 1) * 128, :], in_=x1h[b][:]),
                        reads=[t_x1h[b]], writes=[t_x1bf], key=t_x1h[b], join=True)
                    op("pool", lambda g: g.tensor_copy(out=x1t[b][:], in_=x1h[b][:]), reads=[t_x1h[b]], writes=[t_x1l[b]])
                    op("pool", lambda g: g.tensor_tensor(out=x1t[b][:], in0=x1[b][:], in1=x1t[b][:], op=ALU.subtract),
                       reads=[t_x1[b]], writes=[t_x1l[b]])
                    op("pool", lambda g: g.tensor_copy(out=x1l[b][:], in_=x1t[b][:]), writes=[t_x1l[b]])
                    for (src, tsrc, pp, tpp, dst, tdst) in ((x1h[b], t_x1h[b], pTh, t_pTh, xTh[b], t_xTh[b]),
                                                            (x1l[b], t_x1l[b], pTl, t_pTl, xTl[b], t_xTl[b])):
                        def tr(pe):
                            for c in range(8):
                                r = pe.transpose(out=pp[:, c, :], in_=src[:, c * 128:(c + 1) * 128], identity=identb[:])
                            return r
                        op("pe", tr, reads=[tsrc, t_const], writes=[tpp])
                        op("act", lambda a: a.copy(out=dst[:], in_=pp[:]), reads=[tpp], writes=[tdst])

                    def mmr(pe):
                        n = 0
                        for (xa, wa) in ((xTh[b], wrh), (xTh[b], wrl), (xTl[b], wrh)):
                            for c in range(8):
                                r = pe.matmul(plog[:], lhsT=xa[:, c, :], rhs=wa[:, c, :], start=(n == 0), stop=(n == 23))
                                n += 1
                        return r
                    op("pe", mmr, reads=[t_xTh[b], t_xTl[b], t_wr], writes=[t_plog])
                    R = rt[b]
                    L = R[:, 0:32]; MK = R[:, 32:64]; EX = R[:, 64:96]; G = R[:, 96:128]
                    POS = R[:, 128:160]; TMP = R[:, 160:192]; OH = R[:, 192:224]; TMP2 = R[:, 224:256]
                    S = sm[b]
                    top8 = S[:, 0:8]; nmax = S[:, 8:9]; den = S[:, 9:10]; rden = S[:, 10:11]
                    dstf = S[:, 12:16]
                    tr_ = t_rt[b]
                    op("dve", lambda v: v.tensor_tensor(out=L, in0=plog[:], in1=rb_bc[:], op=ALU.add),
                       reads=[t_plog, t_wr], writes=[tr_])
                    op("dve", lambda v: v.max(out=top8, in_=L), writes=[tr_])
                    op("dve", lambda v: v.tensor_scalar(out=MK, in0=L, scalar1=S[:, 3:4], scalar2=None, op0=ALU.is_ge), writes=[tr_])
                    op("dve", lambda v: v.tensor_scalar(out=nmax, in0=S[:, 0:1], scalar1=-1.0, scalar2=None, op0=ALU.mult), writes=[tr_])
                    op("act", lambda a: a.activation(out=EX, in_=L, func=AF.Exp, bias=nmax, scale=1.0), writes=[tr_])
                    op("dve", lambda v: v.tensor_tensor(out=EX, in0=EX, in1=MK, op=ALU.mult), writes=[tr_])
                    op("dve", lambda v: v.reduce_sum(out=den, in_=EX, axis=AX.X), writes=[tr_])
                    op("dve", lambda v: v.reciprocal(out=rden, in_=den), writes=[tr_])
                    op("dve", lambda v: v.tensor_scalar(out=G, in0=EX, scalar1=rden, scalar2=None, op0=ALU.mult), writes=[tr_])
                    op("dve", lambda v: v.tensor_copy(out=mkb[b][:], in_=MK), reads=[tr_], writes=[t_mkb[b]])

                    def mmc(pe):
                        pe.matmul(pcnt[:, 0, :], lhsT=tri_lt[:], rhs=mkb[b][:], start=True, stop=True)
                        return pe.matmul(pcnt[:, 1, :], lhsT=ones_b[:], rhs=mkb[b][:], start=True, stop=True)
                    op("pe", mmc, reads=[t_mkb[b], t_const], writes=[t_pcnt])
                    op("dve", lambda v: v.tensor_tensor(out=POS, in0=pcnt[:, 0, :], in1=base[:], op=ALU.add),
                       reads=[t_pcnt, t_base], writes=[tr_])
                    op("dve", lambda v: v.tensor_tensor(out=base[:], in0=base[:], in1=pcnt[:, 1, :], op=ALU.add),
                       reads=[t_pcnt], writes=[t_base])
                    op("dve", lambda v: v.tensor_scalar(out=TMP, in0=POS, scalar1=float(C), scalar2=None, op0=ALU.is_lt), writes=[tr_])
                    op("dve", lambda v: v.tensor_tensor(out=POS, in0=POS, in1=ecol[:], op=ALU.add), reads=[t_const], writes=[tr_])
                    op("dve", lambda v: v.tensor_scalar(out=POS, in0=POS, scalar1=float(-NSLOT), scalar2=None, op0=ALU.add), writes=[tr_])
                    op("dve", lambda v: v.tensor_tensor(out=POS, in0=POS, in1=TMP, op=ALU.mult), writes=[tr_])
                    op("dve", lambda v: v.tensor_scalar(out=POS, in0=POS, scalar1=float(NSLOT), scalar2=None, op0=ALU.add), writes=[tr_])
                    for k in range(4):
                        op("dve", lambda v: v.tensor_scalar(out=OH, in0=L, scalar1=S[:, k:k + 1], scalar2=None, op0=ALU.is_equal), writes=[tr_])
                        op("dve", lambda v: v.tensor_tensor(out=TMP2, in0=OH, in1=POS, op=ALU.mult), writes=[tr_])
                        op("dve", lambda v: v.reduce_sum(out=dstf[:, k:k + 1], in_=TMP2, axis=AX.X), writes=[tr_])
                        op("dve", lambda v: v.tensor_tensor(out=TMP2, in0=OH, in1=G, op=ALU.mult), writes=[tr_])
                        op("dve", lambda v: v.reduce_sum(out=gates[:, i, k:k + 1], in_=TMP2, axis=AX.X), writes=[tr_, t_route])
                    op("dve", lambda v: v.tensor_copy(out=dests[:, i, :], in_=dstf), reads=[tr_], writes=[t_route])
                    for k in range(4):
                        dma("pool", lambda q: q.indirect_dma_start(
                            out=toklist[:, :], out_offset=bass.IndirectOffsetOnAxis(ap=dests[:, i, k:k + 1], axis=0),
                            in_=tokid[:, i:i + 1], in_offset=None), reads=[t_route, t_const], writes=[t_tl], key=t_tl, join=True)
                kb.barrier()

        def phase_experts(li):
            with contextlib.ExitStack() as sc:
                wg = [sb(f"wg{i}", [128, 8, D], BF16, scope=sc) for i in range(2)]
                wu = [sb(f"wu{i}", [128, 8, D], BF16, scope=sc) for i in range(2)]
                wd = [sb(f"wd{i}", [128, 8, D], BF16, scope=sc) for i in range(2)]
                t_wg = [kb.tok("wg") for _ in range(2)]; t_wu = [kb.tok("wu") for _ in range(2)]; t_wd = [kb.tok("wd") for _ in range(2)]
                bd = [sb(f"bd{i}", [128, D], scope=sc) for i in range(2)]; t_bd = [kb.tok("bd") for _ in range(2)]
                braw = sb("braw", [NE, 2, D], scope=sc)
                brb = sb("brb", [NE, 2, D], BF16, scope=sc)
                bT = sb("bT", [128, 2, 8, NE], scope=sc)
                t_b = kb.tok("bias")
                idx = [sb(f"idx{i}", [128, CR], I32, scope=sc) for i in range(2)]; t_idx = [kb.tok("idx") for _ in range(2)]
                xg = [sb(f"xg{i}", [128, CR, D], BF16, scope=sc) for i in range(2)]; t_xg = [kb.tok("xg") for _ in range(2)]
                xeT = sb("xeT", [128, 8, C], BF16, scope=sc); t_xeT = kb.tok("xeT")
                aT = sb("aT", [128, 8, C], BF16, scope=sc); t_aT = kb.tok("aT")
                gt = [sb(f"gt{i}", [128, 512], scope=sc) for i in range(2)]; t_gt = [kb.tok("gt") for _ in range(2)]
                sg = [sb(f"sg{i}", [128, 512], scope=sc) for i in range(2)]; t_sg = [kb.tok("sg") for _ in range(2)]
                ut = [sb(f"ut{i}", [128, 512], scope=sc) for i in range(2)]; t_ut = [kb.tok("ut") for _ in range(2)]
                yo = [sb(f"yo{i}", [128, D], scope=sc) for i in range(2)]; t_yo = [kb.tok("yo") for _ in range(2)]
                pT = ps("pT", [128, 8, 128], BF16, scope=sc); t_pT = kb.tok("pT")
                pTs = [pT, ps("pTb", [128, 8, 128], BF16, scope=sc)]; t_pTs = [t_pT, kb.tok("pTb")]
                pg = [ps(f"pg{i}", [128, 512], scope=sc) for i in range(2)]; t_pg = [kb.tok("pg") for _ in range(2)]
                pu = [ps(f"pu{i}", [128, 512], scope=sc) for i in range(2)]; t_pu = [kb.tok("pu") for _ in range(2)]
                py = ps("py", [128, 1024], scope=sc); t_py = kb.tok("py")
                t_pyh = [kb.tok("pyh0"), kb.tok("pyh1")]
                t_yoh = [kb.tok("yoh") for _ in range(2)]
                ytmp = [sb(f"ytmp{i}", [128, 512], scope=sc) for i in range(2)]; t_ytmp = [kb.tok("ytmp") for _ in range(2)]
                pbt = pT[:, :, 0:NE]; t_pbt = t_pT

                dma("sp", lambda q: q.dma_start(out=braw[:, 0, :], in_=exp_b_gate[li]), writes=[t_b], key=t_b)
                dma("sp", lambda q: q.dma_start(out=braw[:, 1, :], in_=exp_b_up[li]), writes=[t_b], key=t_b, join=True)
                op("dve", lambda v: v.tensor_copy(out=brb[:], in_=braw[:]), reads=[t_b], writes=[t_b])
                for g in range(2):
                    def tr(pe):
                        for c in range(8):
                            r = pe.transpose(out=pT[:, c, 0:NE], in_=brb[:, g, c * 128:(c + 1) * 128], identity=identb[0:NE, 0:NE])
                        return r
                    op("pe", tr, reads=[t_b, t_const], writes=[t_pbt])
                    op("dve", lambda v: v.tensor_copy(out=bT[:, g, :, :], in_=pT[:, :, 0:NE]), reads=[t_pbt], writes=[t_b])

                def load_w(e):
                    b = e % 2
                    for (dst, td, src) in ((wg[b], t_wg[b], exp_w_gate), (wu[b], t_wu[b], exp_w_up), (wd[b], t_wd[b], exp_w_down)):
                        dma("pool", lambda q: q.dma_start(out=dst[:], in_=src[li, e].rearrange("(kc p) n -> p kc n", p=128)),
                            writes=[td], key=td)
                    dma("sp", lambda q: q.dma_start(out=bd[b][:], in_=exp_b_down[li, e].partition_broadcast(128)),
                        writes=[t_bd[b]], key=t_bd[b])

                def load_x(e):
                    b = e % 2
                    dma("sp", lambda q: q.dma_start(out=idx[b][:], in_=toklist[e * C:(e + 1) * C, :].rearrange("(p r) o -> p (r o)", p=128)),
                        reads=[t_tl], writes=[t_idx[b]], key=t_idx[b])
                    for r in range(CR):
                        dma("pool", lambda q: q.indirect_dma_start(
                            out=xg[b][:, r, :], out_offset=None, in_=x1bf[:, :],
                            in_offset=bass.IndirectOffsetOnAxis(ap=idx[b][:, r:r + 1], axis=0)),
                            reads=[t_idx[b], t_x1bf], writes=[t_xg[b]], key=t_xg[b], join=(r > 0))

                load_x(0)
                load_w(0)
                it = 0
                for e in range(NE):
                    b = e % 2
                    if e + 1 < NE:
                        load_x(e + 1)
                        load_w(e + 1)
                    for r in range(CR):
                        pTr, t_pTr = pTs[r % 2], t_pTs[r % 2]

                        def tr(pe):
                            for c in range(8):
                                rr = pe.transpose(out=pTr[:, c, :], in_=xg[b][:, r, c * 128:(c + 1) * 128], identity=identb[:])
                            return rr
                        op("pe", tr, reads=[t_xg[b], t_const], writes=[t_pTr])
                        if r % 2 == 0:
                            op("act", lambda a: a.copy(out=xeT[:, :, r * 128:(r + 1) * 128], in_=pTr[:]), reads=[t_pTr], writes=[t_xeT])
                        else:
                            op("dve", lambda v: v.tensor_copy(out=xeT[:, :, r * 128:(r + 1) * 128], in_=pTr[:]), reads=[t_pTr], writes=[t_xeT])
                    segs = [(s0, min(512, C - s0)) for s0 in range(0, C, 512)]
                    for fc in range(8):
                        for (s0, sn) in segs:
                            bb = it % 2
                            it += 1
                            for (pp, tp, w_, tw) in ((pg[bb], t_pg[bb], wg[b], t_wg[b]), (pu[bb], t_pu[bb], wu[b], t_wu[b])):
                                def mm(pe):
                                    for kc in range(8):
                                        rr = pe.matmul(pp[:, 0:sn], lhsT=w_[:, kc, fc * 128:(fc + 1) * 128],
                                                       rhs=xeT[:, kc, s0:s0 + sn], start=(kc == 0), stop=(kc == 7))
                                    return rr
                                op("pe", mm, reads=[tw, t_xeT], writes=[tp])
                            G_, S_, U_ = gt[bb][:, 0:sn], sg[bb][:, 0:sn], ut[bb][:, 0:sn]
                            op("dve", lambda v: v.tensor_scalar(out=G_, in0=pg[bb][:, 0:sn], scalar1=bT[:, 0, fc, e:e + 1], scalar2=7.0,
                                                                op0=ALU.add, op1=ALU.min), reads=[t_pg[bb], t_b], writes=[t_gt[bb]])
                            op("act", lambda a: a.activation(out=S_, in_=G_, func=AF.Silu, scale=1.702),
                               reads=[t_gt[bb]], writes=[t_sg[bb]])
                            op("dve", lambda v: v.tensor_scalar(out=U_, in0=pu[bb][:, 0:sn], scalar1=bT[:, 1, fc, e:e + 1], scalar2=7.0,
                                                                op0=ALU.add, op1=ALU.min), reads=[t_pu[bb], t_b], writes=[t_ut[bb]])
                            op("dve", lambda v: v.tensor_scalar(out=U_, in0=U_, scalar1=-7.0, scalar2=1.0 / 1.702,
                                                                op0=ALU.max, op1=ALU.mult), writes=[t_ut[bb]])
                            op("dve", lambda v: v.scalar_tensor_tensor(out=aT[:, fc, s0:s0 + sn], in0=U_, scalar=1.0 / 1.702, in1=S_,
                                                                       op0=ALU.add, op1=ALU.mult),
                               reads=[t_sg[bb], t_ut[bb]], writes=[t_aT])
                    for r in range(CR):
                        yb = (e * CR + r) % 2

                        for hh in range(2):
                            def mmd(pe):
                                for fc in range(8):
                                    rr = pe.matmul(py[:, hh * 512:(hh + 1) * 512], lhsT=aT[:, fc, r * 128:(r + 1) * 128],
                                                   rhs=wd[b][:, fc, hh * 512:(hh + 1) * 512], start=(fc == 0), stop=(fc == 7))
                                return rr
                            op("pe", mmd, reads=[t_aT, t_wd[b]], writes=[t_pyh[hh]])
                            if hh == 0:
                                op("dve", lambda v: v.tensor_tensor(out=yo[yb][:, 0:512], in0=py[:, 0:512], in1=bd[b][:, 0:512], op=ALU.add),
                                   reads=[t_pyh[hh], t_bd[b]], writes=[t_yo[yb]])
                            else:
                                op("pool", lambda g: g.tensor_copy(out=yo[yb][:, 512:1024], in_=bd[b][:, 512:1024]), reads=[t_bd[b]], writes=[t_yoh[yb]])
                                op("act", lambda a: a.copy(out=ytmp[yb][:], in_=py[:, 512:1024]),
                                   reads=[t_pyh[hh]], writes=[t_ytmp[yb]])
                                op("pool", lambda g: g.tensor_tensor(out=yo[yb][:, 512:1024], in0=yo[yb][:, 512:1024], in1=ytmp[yb][:], op=ALU.add),
                                   reads=[t_ytmp[yb], t_yoh[yb]], writes=[t_yoh[yb]])
                        ydst = yslots[e * C:(e + 1) * C, :].rearrange("(p r) d -> p r d", p=128)
                        dma("sp", lambda q: q.dma_start(out=ydst[:, r, :], in_=yo[yb][:]),
                            reads=[t_yo[yb], t_yoh[yb]], writes=[t_ys], key=t_yo[yb], join=True)
                kb.barrier()

        def phase_combine_ln2_ple(li, gates, dests, t_route, xout, t_xout):
            with contextlib.ExitStack() as sc:
                wpg = sb("wpg", [128, 8, D], BF16, scope=sc); wpp = sb("wpp", [128, 2, D], BF16, scope=sc); t_w = kb.tok("wple")
                g_bc = sb("g_bc", [128, D], scope=sc); b_bc = sb("b_bc", [128, D], scope=sc)
                bpg = sb("bpg", [128, D], scope=sc); t_gb = kb.tok("gb")
                yk = [[sb(f"yk{i}_{k}", [128, D], scope=sc) for k in range(4)] for i in range(3)]
                t_yk = [[kb.tok("yk") for k in range(4)] for i in range(3)]
                xs = [sb(f"xs{i}", [128, D], scope=sc) for i in range(3)]; t_xs = [kb.tok("xs") for _ in range(3)]
                h = [sb(f"h{i}", [128, D], scope=sc) for i in range(2)]; t_h = [kb.tok("h") for _ in range(2)]
                x2 = [sb(f"x2_{i}", [128, D], scope=sc) for i in range(2)]; t_x2 = [kb.tok("x2") for _ in range(2)]
                x2b = [sb(f"x2b{i}", [128, D], BF16, scope=sc) for i in range(2)]; t_x2b = [kb.tok("x2b") for _ in range(2)]
                x2T = [sb(f"x2T{i}", [128, 8, 128], BF16, scope=sc) for i in range(2)]; t_x2T = [kb.tok("x2T") for _ in range(2)]
                pp32 = [sb(f"pp32_{i}", [128, PLE], scope=sc) for i in range(3)]; t_pp = [kb.tok("pp") for _ in range(3)]
                ppb = [sb(f"ppb{i}", [128, PLE], BF16, scope=sc) for i in range(2)]; t_ppb = [kb.tok("ppb") for _ in range(2)]
                ppT = [sb(f"ppT{i}", [128, 2, 128], BF16, scope=sc) for i in range(2)]; t_ppT = [kb.tok("ppT") for _ in range(2)]
                stat = [sb(f"stat{i}", [128, 16], scope=sc) for i in range(2)]; t_stat = [kb.tok("stat") for _ in range(2)]
                gs = [sb(f"gs{i}", [128, D], scope=sc) for i in range(2)]; t_gs = [kb.tok("gs") for _ in range(2)]
                xo = [sb(f"xo{i}", [128, D], scope=sc) for i in range(2)]; t_xo = [kb.tok("xo") for _ in range(2)]
                pT = ps("pT", [128, 8, 128], BF16, scope=sc); t_pT = kb.tok("pT")
                pT2 = ps("pT2", [128, 2, 128], BF16, scope=sc); t_pT2 = kb.tok("pT2")
                pgt = ps("pgt", [128, D], scope=sc); t_pgt = kb.tok("pgt")
                ppj = ps("ppj", [128, D], scope=sc); t_ppj = kb.tok("ppj")
                dma("pool", lambda q: q.dma_start(out=wpg[:], in_=ple_w_gate[li].rearrange("(kc p) n -> p kc n", p=128)), writes=[t_w], key=t_w)
                dma("pool", lambda q: q.dma_start(out=wpp[:], in_=ple_w_proj[li].rearrange("(kc p) n -> p kc n", p=128)), writes=[t_w], key=t_w, join=True)
                dma("sp", lambda q: q.dma_start(out=g_bc[:], in_=ln2_g[li].partition_broadcast(128)), writes=[t_gb], key=t_gb)
                dma("sp", lambda q: q.dma_start(out=b_bc[:], in_=ln2_b[li].partition_broadcast(128)), writes=[t_gb], key=t_gb, join=True)
                dma("sp", lambda q: q.dma_start(out=bpg[:], in_=ple_b_gate[li].partition_broadcast(128)), writes=[t_gb], key=t_gb, join=True)
                def loads(i):
                    b3 = i % 3
                    dma("sp", lambda q: q.dma_start(out=xs[b3][:], in_=x1res[i * 128:(i + 1) * 128, :]),
                        reads=[t_x1res], writes=[t_xs[b3]], key=t_xs[b3])
                    dma("sp", lambda q: q.dma_start(out=pp32[b3][:], in_=p_in[li, i * 128:(i + 1) * 128, :]),
                        writes=[t_pp[b3]], key=t_pp[b3])
                    for k in range(4):
                        dma("pool", lambda q: q.indirect_dma_start(
                            out=yk[b3][k][:], out_offset=None, in_=yslots[:, :],
                            in_offset=bass.IndirectOffsetOnAxis(ap=dests[:, i, k:k + 1], axis=0)),
                            reads=[t_route, t_ys], writes=[t_yk[b3][k]], key=t_yk[b3][k])
                loads(0)
                if NT > 1:
                    loads(1)
                for i in range(NT):
                    b = i % 2
                    b3 = i % 3
                    if i + 2 < NT:
                        loads(i + 2)
                    op("act", lambda a: a.mul(out=h[b][:], in_=xs[b3][:], mul=ALPHA), reads=[t_xs[b3]], writes=[t_h[b]])
                    for k in range(4):
                        op("dve", lambda v: v.scalar_tensor_tensor(out=h[b][:], in0=yk[b3][k][:], scalar=gates[:, i, k:k + 1],
                                                                   in1=h[b][:], op0=ALU.mult, op1=ALU.add),
                           reads=[t_yk[b3][k], t_route], writes=[t_h[b]])
                    layer_norm_tile(h[b], g_bc, b_bc, x2[b], stat[b], t_h[b], t_x2[b], t_stat[b], t_gb)
                    op("act", lambda a: a.copy(out=x2b[b][:], in_=x2[b][:]), reads=[t_x2[b]], writes=[t_x2b[b]])
                    op("act", lambda a: a.copy(out=ppb[b][:], in_=pp32[b3][:]), reads=[t_pp[b3]], writes=[t_ppb[b]])

                    def tr(pe):
                        for c in range(8):
                            r = pe.transpose(out=pT[:, c, :], in_=x2b[b][:, c * 128:(c + 1) * 128], identity=identb[:])
                        return r
                    op("pe", tr, reads=[t_x2b[b], t_const], writes=[t_pT])
                    op("act", lambda a: a.copy(out=x2T[b][:], in_=pT[:]), reads=[t_pT], writes=[t_x2T[b]])

                    def tr2(pe):
                        for c in range(2):
                            r = pe.transpose(out=pT2[:, c, :], in_=ppb[b][:, c * 128:(c + 1) * 128], identity=identb[:])
                        return r
                    op("pe", tr2, reads=[t_ppb[b], t_const], writes=[t_pT2])
                    op("act", lambda a: a.copy(out=ppT[b][:], in_=pT2[:]), reads=[t_pT2], writes=[t_ppT[b]])

                    def mmg(pe):
                        for hh in range(2):
                            for c in range(8):
                                r = pe.matmul(pgt[:, hh * 512:(hh + 1) * 512], lhsT=x2T[b][:, c, :], rhs=wpg[:, c, hh * 512:(hh + 1) * 512],
                                              start=(c == 0), stop=(c == 7))
                        return r
                    op("pe", mmg, reads=[t_x2T[b], t_w], writes=[t_pgt])

                    def mmp(pe):
                        for hh in range(2):
                            for c in range(2):
                                r = pe.matmul(ppj[:, hh * 512:(hh + 1) * 512], lhsT=ppT[b][:, c, :], rhs=wpp[:, c, hh * 512:(hh + 1) * 512],
                                              start=(c == 0), stop=(c == 1))
                        return r
                    op("pe", mmp, reads=[t_ppT[b], t_w], writes=[t_ppj])
                    op("dve", lambda v: v.tensor_tensor(out=gs[b][:], in0=pgt[:], in1=bpg[:], op=ALU.add),
                       reads=[t_pgt, t_gb], writes=[t_gs[b]])
                    op("act", lambda a: a.activation(out=gs[b][:], in_=gs[b][:], func=AF.Sigmoid), writes=[t_gs[b]])
                    op("dve", lambda v: v.tensor_tensor(out=xo[b][:], in0=ppj[:], in1=gs[b][:], op=ALU.mult),
                       reads=[t_ppj, t_gs[b]], writes=[t_xo[b]])
                    op("pool", lambda g: g.tensor_tensor(out=xo[b][:], in0=xo[b][:], in1=x2[b][:], op=ALU.add),
                       reads=[t_x2[b]], writes=[t_xo[b]])
                    dma("sp", lambda q: q.dma_start(out=xout[i * 128:(i + 1) * 128, :], in_=xo[b][:]),
                        reads=[t_xo[b]], writes=[t_xout], key=t_xo[b], join=True)
                kb.barrier()

        gates = sb("gates", [128, NT, 4])
        dests = sb("dests", [128, NT, 4], I32)
        t_route = kb.tok("route")
        xin, t_xin = x_in, kb.tok("xin")
        nl = len(layers)
        for n, li in enumerate(layers):
            kind, j = KINDS[li], JIDX[li]
            last = (n == nl - 1)
            xout, t_xout = (out_d, t_out) if last else (xres[n % 2], t_xres[n % 2])
            with contextlib.ExitStack() as lsc:
                t_mT = kb.tok("mT")
                if kind == 0:
                    mT = sb("mT", [128, 8, T], BF16, scope=lsc)
                    with contextlib.ExitStack() as asc:
                        xT = sb("xT", [128, 8, T], BF16, scope=asc); t_xT = kb.tok("xT")
                        phase_xT(xin, t_xin, xT, t_xT)
                        phase_conv(j, xT, t_xT, mT, t_mT)
                    w_out_d = conv_w_out[j]
                else:
                    ncum = sb("ncum", [128, NT, 16], scope=lsc)
                    refB = sb("refB", [16, 3, T], BF16, scope=lsc)
                    t_cum = kb.tok("cum")
                    w_in_d = sb_w_in[0] if kind == 1 else fox_w_in[0]
                    with contextlib.ExitStack() as asc:
                        xT = sb("xT", [128, 8, T], BF16, scope=asc); t_xT = kb.tok("xT")
                        phase_xT(xin, t_xin, xT, t_xT)
                        phase_qkv(kind, w_in_d, xT, t_xT, ncum, refB, t_cum)
                    mT = sb("mT", [128, 8, T], BF16, scope=lsc)
                    phase_attn(kind, mT, t_mT, ncum, refB, t_cum)
                    w_out_d = sb_w_out[0] if kind == 1 else fox_w_out[0]
                phase_outproj_ln1_route(li, w_out_d, xin, t_xin, mT, t_mT, gates, dests, t_route)
            phase_experts(li)
            phase_combine_ln2_ple(li, gates, dests, t_route, xout, t_xout)
            xin, t_xin = xout, t_xout
        kb.barrier()
    return nc


_NAMES = ["x", "p", "conv_w_in", "conv_w", "conv_w_out", "sb_w_in", "sb_w_out", "fox_w_in", "fox_b_f", "fox_w_out",
          "ln1_g", "ln1_b", "ln2_g", "ln2_b", "router_w", "router_b", "exp_w_gate", "exp_b_gate", "exp_w_up",
          "exp_b_up", "exp_w_down", "exp_b_down", "ple_w_proj", "ple_w_gate", "ple_b_gate"]


def kernel(**inputs):
    B = inputs["x"].shape[0]
    T = inputs["x"].shape[1]
    nc = build_program(T=T)
    in_maps = []
    shared = {k: np.ascontiguousarray(inputs[k], dtype=np.float32) for k in _NAMES if k not in ("x", "p")}
    for b in range(B):
        m = dict(shared)
        m["x"] = np.ascontiguousarray(inputs["x"][b], dtype=np.float32)
        m["p"] = np.ascontiguousarray(inputs["p"][:, b], dtype=np.float32)
        in_maps.append(m)
    res = run_bass_kernel_spmd(nc, in_maps, core_ids=list(range(B)))
    return np.stack([np.asarray(r["out"]) for r in res.results], axis=0).astype(np.float32)
```

```python
import contextlib
import numpy as np
import concourse.bass as bass
import concourse.mybir as mybir
from concourse.bass_utils import run_bass_kernel_spmd

F32 = mybir.dt.float32
BF16 = mybir.dt.bfloat16
I32 = mybir.dt.int32
AF = mybir.ActivationFunctionType
ALU = mybir.AluOpType
AX = mybir.AxisListType

D = 1024
NE = 32
PLE = 256
ALPHA = 8 ** 0.25
LN_EPS = 1e-5
KINDS = (0, 1, 2, 0)
JIDX = (0, 0, 0, 1)


class Tok:
    __slots__ = ("name", "w", "r", "dkey")

    def __init__(self, name):
        self.name = name
        self.w = {}
        self.r = {}
        self.dkey = None


class KB:
    def __init__(self, nc, es):
        self.nc = nc
        self.es = es
        self.eng = dict(pe=nc.tensor, dve=nc.vector, act=nc.scalar, pool=nc.gpsimd, sp=nc.sync)
        self.sems, self.cnt, self.isdma = {}, {}, {}
        self.waited = {k: {} for k in self.eng}
        for k in self.eng:
            self._newsem(k, False)
        self.ntok = 0
        self.free_slots = []
        for i in range(64):
            self._newsem(f"d{i}", True)
            self.free_slots.append(f"d{i}")
        self.live = []
        self.mark = {}

    def _newsem(self, key, isdma):
        self.sems[key] = self.es.enter_context(self.nc.semaphore("s_" + key))
        self.cnt[key] = 0
        self.isdma[key] = isdma

    def tok(self, name="t"):
        self.ntok += 1
        return Tok(f"{name}{self.ntok}")

    def _wait(self, e, evs):
        need = {}
        for ev in evs:
            for k, v in ev.items():
                if self.isdma[k]:
                    if v <= self.mark.get(k, 0):
                        continue
                    v = self.cnt[k]
                if v > need.get(k, 0):
                    need[k] = v
        for k, v in need.items():
            if self.waited[e].get(k, 0) >= v:
                continue
            self.eng[e].wait_ge(self.sems[k], v)
            self.waited[e][k] = v

    def op(self, e, emit, reads=(), writes=()):
        evs = [t.w for t in reads] + [t.w for t in writes] + [t.r for t in writes]
        self._wait(e, evs)
        inst = emit(self.eng[e])
        self.cnt[e] += 1
        inst.then_inc(self.sems[e], 1)
        v = self.cnt[e]
        for t in reads:
            t.r[e] = v
        for t in writes:
            t.w = {e: v}
            t.r = {}
        return inst

    def dma(self, q, emit, reads=(), writes=(), key=None, join=False):
        evs = [t.w for t in reads] + [t.r for t in writes]
        if not join:
            evs += [t.w for t in writes]
        self._wait(q, evs)
        if key.dkey is None:
            key.dkey = self.free_slots.pop(0)
            self.live.append(key)
        k = key.dkey
        inst = emit(self.eng[q])
        self.cnt[k] += 16
        inst.then_inc(self.sems[k], 16)
        v = self.cnt[k]
        for t in reads:
            t.r[k] = v
        for t in writes:
            if join:
                t.w[k] = v
            else:
                t.w = {k: v}
                t.r = {}
        return inst

    def barrier(self):
        allev = [dict(self.cnt)]
        for e in self.eng:
            self._wait(e, allev)
        self.mark = {k: v for k, v in self.cnt.items() if self.isdma[k]}
        for t in self.live:
            self.free_slots.append(t.dkey)
            t.dkey = None
        self.live = []


def _cfg_caps(T):
    return {4096: 640, 512: 128, 1024: 256}[T]


def build_program(T=4096, layers=(0, 1, 2, 3), dbg=False):
    C = _cfg_caps(T)
    NT = T // 128
    NQ = T // 512
    CR = C // 128
    NSLOT = NE * C
    nc = bass.Bass("TRN2", target_bir_lowering=False)

    def din(name, shape, dt=F32):
        return nc.dram_tensor(name, list(shape), dt, kind="ExternalInput").ap()

    def dscr(name, shape, dt=F32):
        return nc.dram_tensor(name, list(shape), dt).ap()

    x_in = din("x", [T, D])
    p_in = din("p", [4, T, PLE])
    conv_w_in = din("conv_w_in", [2, D, 3 * D])
    conv_w = din("conv_w", [2, 3, D])
    conv_w_out = din("conv_w_out", [2, D, D])
    sb_w_in = din("sb_w_in", [1, D, 3 * D])
    sb_w_out = din("sb_w_out", [1, D, D])
    fox_w_in = din("fox_w_in", [1, D, 3 * D + 16])
    fox_b_f = din("fox_b_f", [1, 16])
    fox_w_out = din("fox_w_out", [1, D, D])
    ln1_g = din("ln1_g", [4, D]); ln1_b = din("ln1_b", [4, D])
    ln2_g = din("ln2_g", [4, D]); ln2_b = din("ln2_b", [4, D])
    router_w = din("router_w", [4, D, NE]); router_b = din("router_b", [4, NE])
    exp_w_gate = din("exp_w_gate", [4, NE, D, D]); exp_b_gate = din("exp_b_gate", [4, NE, D])
    exp_w_up = din("exp_w_up", [4, NE, D, D]); exp_b_up = din("exp_b_up", [4, NE, D])
    exp_w_down = din("exp_w_down", [4, NE, D, D]); exp_b_down = din("exp_b_down", [4, NE, D])
    ple_w_proj = din("ple_w_proj", [4, PLE, D]); ple_w_gate = din("ple_w_gate", [4, D, D])
    ple_b_gate = din("ple_b_gate", [4, D])
    out_d = nc.dram_tensor("out", [T, D], F32, kind="ExternalOutput").ap()

    xres = [dscr("xres0", [T, D]), dscr("xres1", [T, D])]
    x1res = dscr("x1res", [T, D])
    x1bf = dscr("x1bf", [T, D], BF16)
    yslots = dscr("yslots", [NSLOT + 128, D])
    toklist = dscr("toklist", [NSLOT + 128, 1], I32)
    qT_d = dscr("qT_d", [8, 128, T], BF16)
    kT_d = dscr("kT_d", [8, 128, T], BF16)
    v_d = dscr("v_d", [T, D], BF16)
    nrow_d = dscr("nrow_d", [16, 3, T], BF16)
    dbg_d = None
    if dbg:
        dbg_d = nc.dram_tensor("dbg", [T, D], F32, kind="ExternalOutput").ap()

    es = contextlib.ExitStack()
    with es:
        kb = KB(nc, es)
        op, dma = kb.op, kb.dma

        uid = [0]

        def sb(name, shape, dt=F32, scope=es):
            uid[0] += 1
            return scope.enter_context(nc.sbuf_tensor(f"{name}_{uid[0]}", list(shape), dt))

        def ps(name, shape, dt=F32, scope=es):
            uid[0] += 1
            return scope.enter_context(nc.psum_tensor(f"{name}_{uid[0]}", list(shape), dt))

        t_const = kb.tok("const")
        ident32 = sb("ident32", [128, 128])
        identb = sb("identb", [128, 128], BF16)
        ones_b = sb("ones_b", [128, 128], BF16)
        tri_lt = sb("tri_lt", [128, 128], BF16)
        ntri_ge = sb("ntri_ge", [128, 128], BF16)
        nones_b = sb("nones_b", [128, 128], BF16)
        mask_lt = sb("mask_lt", [128, 128], BF16)
        mask_le = sb("mask_le", [128, 128], BF16)
        c_one = sb("c_one", [128, 1])
        c_eps = sb("c_eps", [128, 1])
        c_nhalf = sb("c_nhalf", [128, 1])
        tokid = sb("tokid", [128, NT], I32)
        ecol = sb("ecol", [128, NE])
        zrow = sb("zrow", [128, D])
        zi = sb("zi", [128, (NSLOT + 128) // 128], I32)
        tmp32 = sb("tmp32", [128, 128])

        def mk_tri(dst, cmp, mult_p, mult_f, fillv, inv):
            def e1(g):
                return g.memset(tmp32[:], inv)
            op("pool", e1, writes=[t_const])

            def e2(g):
                return g.affine_select(out=tmp32[:], in_=tmp32[:], pattern=[[mult_f, 128]], compare_op=cmp,
                                       fill=fillv, base=0, channel_multiplier=mult_p)
            op("pool", e2, writes=[t_const])
            op("dve", lambda v: v.tensor_copy(out=dst[:], in_=tmp32[:]), reads=[t_const], writes=[t_const])

        mk_tri(ident32, ALU.not_equal, 1, -1, 1.0, 0.0)
        mk_tri(identb, ALU.not_equal, 1, -1, 1.0, 0.0)
        mk_tri(tri_lt, ALU.is_gt, -1, 1, 0.0, 1.0)
        mk_tri(mask_lt, ALU.is_gt, -1, 1, 0.0, 1.0)
        mk_tri(mask_le, ALU.is_ge, -1, 1, 0.0, 1.0)
        mk_tri(ntri_ge, ALU.is_ge, 1, -1, 0.0, -1.0)
        zeros_b = sb("zeros_b", [128, 128], BF16)
        sel0 = sb("sel0", [128, 128], BF16)
        mk_tri(sel0, ALU.is_equal, 1, 0, 0.0, 1.0)
        negmask_gt = sb("negmask_gt", [128, 128], BF16)
        mk_tri(negmask_gt, ALU.is_gt, 1, -1, 0.0, -30000.0)
        selh = sb("selh", [16, 16, 128], BF16)
        selt = sb("selt", [16, 16, 128])
        op("pool", lambda g: g.memset(selt[:], 1.0), writes=[t_const])
        op("pool", lambda g: g.affine_select(out=selt[:], in_=selt[:], pattern=[[-1, 16], [0, 128]], compare_op=ALU.is_equal,
                                             fill=0.0, base=0, channel_multiplier=1), writes=[t_const])
        op("dve", lambda v: v.tensor_copy(out=selh[:], in_=selt[:]), reads=[t_const], writes=[t_const])
        zsrc = sb("zsrc", [128, 512], BF16)
        op("pool", lambda g: g.memset(zsrc[:], 0.0), writes=[t_const])
        op("pool", lambda g: g.memset(zeros_b[:], 0.0), writes=[t_const])
        op("pool", lambda g: g.memset(ones_b[:], 1.0), writes=[t_const])
        op("pool", lambda g: g.memset(nones_b[:], -1.0), writes=[t_const])
        op("pool", lambda g: g.memset(c_one[:], 1.0), writes=[t_const])
        op("pool", lambda g: g.memset(c_eps[:], LN_EPS), writes=[t_const])
        op("pool", lambda g: g.memset(c_nhalf[:], -0.5), writes=[t_const])
        op("pool", lambda g: g.memset(zrow[:], 0.0), writes=[t_const])
        op("pool", lambda g: g.memset(zi[:], 0), writes=[t_const])
        op("pool", lambda g: g.iota(tokid[:], pattern=[[128, NT]], base=0, channel_multiplier=1), writes=[t_const])
        op("pool", lambda g: g.iota(ecol[:], pattern=[[C, NE]], base=0, channel_multiplier=0,
                                    allow_small_or_imprecise_dtypes=True), writes=[t_const])
        t_ys = kb.tok("yslots")
        t_tl = kb.tok("toklist")
        dma("sp", lambda q: q.dma_start(out=yslots[NSLOT:NSLOT + 128, :], in_=zrow[:]), reads=[t_const],
            writes=[t_ys], key=t_ys, join=True)

        t_xres = [kb.tok("xres0"), kb.tok("xres1")]
        t_x1res = kb.tok("x1res")
        t_x1bf = kb.tok("x1bf")
        t_qkv = kb.tok("qkv")
        t_out = kb.tok("out")

        def layer_norm_tile(h, g_bc, b_bc, xo, stat, t_h, t_xo, t_stat, t_gb, eng2="pool"):
            def s1(v):
                v.bn_stats(out=stat[:, 0:6], in_=h[:, 0:512])
                return v.bn_stats(out=stat[:, 6:12], in_=h[:, 512:1024])
            op("dve", s1, reads=[t_h], writes=[t_stat])
            op("dve", lambda v: v.bn_aggr(out=stat[:, 12:14], in_=stat[:, 0:12]), reads=[t_stat], writes=[t_stat])
            op("pool", lambda g: g.tensor_tensor(out=stat[:, 14:15], in0=stat[:, 13:14], in1=c_eps[:], op=ALU.add),
               reads=[t_stat, t_const], writes=[t_stat])
            op("pool", lambda g: g.tensor_tensor(out=stat[:, 15:16], in0=stat[:, 14:15], in1=c_nhalf[:], op=ALU.pow),
               reads=[t_stat, t_const], writes=[t_stat])
            op("dve", lambda v: v.tensor_scalar(out=xo[:], in0=h[:], scalar1=stat[:, 12:13], scalar2=stat[:, 15:16],
                                                op0=ALU.subtract, op1=ALU.mult),
               reads=[t_h, t_stat], writes=[t_xo])
            op(eng2, lambda g: g.tensor_tensor(out=xo[:], in0=xo[:], in1=g_bc[:], op=ALU.mult),
               reads=[t_gb], writes=[t_xo])
            op(eng2, lambda g: g.tensor_tensor(out=xo[:], in0=xo[:], in1=b_bc[:], op=ALU.add),
               reads=[t_gb], writes=[t_xo])

        def bcast_load(dst, src_row, t_dst, q="sp"):
            n = dst.shape[-1] if hasattr(dst, "shape") else None
            dma(q, lambda e: e.dma_start(out=dst[:], in_=src_row.partition_broadcast(128)), writes=[t_dst], key=t_dst)

        def phase_xT(xin, t_xin, xT, t_xT):
            with contextlib.ExitStack() as sc:
                xs = [sb(f"xs{i}", [128, D], scope=sc) for i in range(2)]
                xb = [sb(f"xb{i}", [128, D], BF16, scope=sc) for i in range(2)]
                pT = [ps(f"pT{i}", [128, 8, 128], BF16, scope=sc) for i in range(2)]
                t_xs = [kb.tok("xs") for _ in range(2)]
                t_xb = [kb.tok("xb") for _ in range(2)]
                t_pT = [kb.tok("pT") for _ in range(2)]
                for i in range(NT):
                    b = i % 2
                    dma("sp", lambda q: q.dma_start(out=xs[b][:], in_=xin[i * 128:(i + 1) * 128, :]),
                        reads=[t_xin], writes=[t_xs[b]], key=t_xs[b])
                    op("act", lambda a: a.copy(out=xb[b][:], in_=xs[b][:]), reads=[t_xs[b]], writes=[t_xb[b]])

                    def tr(pe):
                        for c in range(8):
                            r = pe.transpose(out=pT[b][:, c, :], in_=xb[b][:, c * 128:(c + 1) * 128], identity=identb[:])
                        return r
                    op("pe", tr, reads=[t_xb[b], t_const], writes=[t_pT[b]])
                    op("dve", lambda v: v.tensor_copy(out=xT[:, :, i * 128:(i + 1) * 128], in_=pT[b][:]),
                       reads=[t_pT[b]], writes=[t_xT])
                kb.barrier()

        def phase_conv(j, xT, t_xT, mT, t_mT):
            with contextlib.ExitStack() as sc:
                wc = [sb(f"wc{i}", [128, 8, 384], BF16, scope=sc) for i in range(2)]
                t_wc = [kb.tok("wc") for _ in range(2)]
                cw = sb("cw", [128, 8, 3], scope=sc)
                cwr = sb("cwr", [3, D], scope=sc)
                cwb = sb("cwb", [3, D], BF16, scope=sc)
                t_cw = kb.tok("cw")
                pcw = ps("pcw", [128, 8, 4], BF16, scope=sc)
                t_pcw = kb.tok("pcw")
                pB = [ps(f"pB{i}", [128, 512], scope=sc) for i in range(2)]
                pC = [ps(f"pC{i}", [128, 512], scope=sc) for i in range(2)]
                pH = [ps(f"pH{i}", [128, 512], scope=sc) for i in range(2)]
                t_pB = [kb.tok("pB") for _ in range(2)]
                t_pC = [kb.tok("pC") for _ in range(2)]
                t_pH = [kb.tok("pH") for _ in range(2)]
                ctmp = [sb(f"ctmp{i}", [128, 512], scope=sc) for i in range(2)]
                t_ct = [kb.tok("ct") for _ in range(2)]
                u = [sb(f"u{i}", [128, 514], scope=sc) for i in range(2)]
                t_u = [kb.tok("u") for _ in range(2)]
                cv = [sb(f"cv{i}", [128, 512], scope=sc) for i in range(2)]
                t_cv = [kb.tok("cv") for _ in range(2)]
                cwlo = sb("cwlo", [3, D], BF16, scope=sc)
                cwt = sb("cwt", [3, D], scope=sc)
                dma("sp", lambda q: q.dma_start(out=cwr[:], in_=conv_w[j]), writes=[t_cw], key=t_cw)
                op("dve", lambda v: v.tensor_copy(out=cwb[:], in_=cwr[:]), reads=[t_cw], writes=[t_cw])
                op("dve", lambda v: v.tensor_copy(out=cwt[:], in_=cwb[:]), reads=[t_cw], writes=[t_cw])
                op("dve", lambda v: v.tensor_tensor(out=cwt[:], in0=cwr[:], in1=cwt[:], op=ALU.subtract), writes=[t_cw])
                op("dve", lambda v: v.tensor_copy(out=cwlo[:], in_=cwt[:]), writes=[t_cw])
                for part, src in ((0, cwb), (1, cwlo)):
                    def tr(pe):
                        for c in range(8):
                            r = pe.transpose(out=pcw[:, c, 0:3], in_=src[:, c * 128:(c + 1) * 128], identity=identb[0:3, 0:3])
                        return r
                    op("pe", tr, reads=[t_cw, t_const], writes=[t_pcw])
                    if part == 0:
                        op("dve", lambda v: v.tensor_copy(out=cw[:], in_=pcw[:, :, 0:3]), reads=[t_pcw], writes=[t_cw])
                    else:
                        op("dve", lambda v: v.tensor_tensor(out=cw[:], in0=cw[:], in1=pcw[:, :, 0:3], op=ALU.add),
                           reads=[t_pcw], writes=[t_cw])
                wsrc = conv_w_in[j].rearrange("(kc p) n -> p kc n", p=128)

                def load_w(c):
                    b = c % 2
                    for g in range(3):
                        dma("pool", lambda q: q.dma_start(out=wc[b][:, :, g * 128:(g + 1) * 128],
                                                          in_=wsrc[:, :, g * 1024 + c * 128:g * 1024 + (c + 1) * 128]),
                            writes=[t_wc[b]], key=t_wc[b], join=(g > 0))
                load_w(0)
                it = 0
                for c in range(8):
                    if c + 1 < 8:
                        load_w(c + 1)
                    wb_ = wc[c % 2]
                    for tq in range(NQ):
                        b = it % 2
                        it += 1
                        for (pp, tp, g) in ((pB[b], t_pB[b], 0), (pC[b], t_pC[b], 1), (pH[b], t_pH[b], 2)):
                            def mm(pe):
                                for kc in range(8):
                                    r = pe.matmul(pp[:], lhsT=wb_[:, kc, g * 128:(g + 1) * 128],
                                                  rhs=xT[:, kc, tq * 512:(tq + 1) * 512], start=(kc == 0), stop=(kc == 7))
                                return r
                            op("pe", mm, reads=[t_wc[c % 2], t_xT], writes=[tp])
                        op("act", lambda a: a.copy(out=ctmp[b][:], in_=pC[b][:]), reads=[t_pC[b]], writes=[t_ct[b]])
                        if tq == 0:
                            op("pool", lambda g_: g_.memset(u[b][:, 0:2], 0.0), writes=[t_u[b]])
                        else:
                            op("pool", lambda g_: g_.tensor_copy(out=u[b][:, 0:2], in_=u[1 - b][:, 512:514]),
                               reads=[t_u[1 - b]], writes=[t_u[b]])
                        op("dve", lambda v: v.tensor_tensor(out=u[b][:, 2:514], in0=ctmp[b][:], in1=pH[b][:], op=ALU.mult),
                           reads=[t_ct[b], t_pH[b]], writes=[t_u[b]])
                        op("dve", lambda v: v.tensor_scalar(out=cv[b][:], in0=u[b][:, 2:514], scalar1=cw[:, c, 2:3],
                                                            scalar2=None, op0=ALU.mult),
                           reads=[t_u[b], t_cw], writes=[t_cv[b]])
                        op("dve", lambda v: v.scalar_tensor_tensor(out=cv[b][:], in0=u[b][:, 1:513], scalar=cw[:, c, 1:2],
                                                                   in1=cv[b][:], op0=ALU.mult, op1=ALU.add),
                           reads=[t_u[b], t_cw], writes=[t_cv[b]])
                        op("dve", lambda v: v.scalar_tensor_tensor(out=cv[b][:], in0=u[b][:, 0:512], scalar=cw[:, c, 0:1],
                                                                   in1=cv[b][:], op0=ALU.mult, op1=ALU.add),
                           reads=[t_u[b], t_cw], writes=[t_cv[b]])
                        op("dve", lambda v: v.tensor_tensor(out=mT[:, c, tq * 512:(tq + 1) * 512], in0=cv[b][:],
                                                            in1=pB[b][:], op=ALU.mult),
                           reads=[t_cv[b], t_pB[b]], writes=[t_mT])
                kb.barrier()

        def phase_qkv(kind, w_in_d, xT, t_xT, ncum, refB, t_cum):
            with contextlib.ExitStack() as sc:
                wsrc = w_in_d.rearrange("(kc p) n -> p kc n", p=128)
                wq = [sb(f"wq{i}", [128, 8, 512], BF16, scope=sc) for i in range(2)]; t_wq = [kb.tok("wq") for _ in range(2)]
                wv = sb("wv", [128, 8, D], BF16, scope=sc); t_wv = kb.tok("wv")
                stg = [sb(f"stg{i}", [128, T], BF16, scope=sc) for i in range(2)]; t_stg = [kb.tok("stg") for _ in range(2)]
                vst = [sb(f"vst{i}", [128, D], BF16, scope=sc) for i in range(2)]; t_vst = [kb.tok("vst") for _ in range(2)]
                pq = [ps(f"pq{i}", [128, 512], scope=sc) for i in range(2)]; t_pq = [kb.tok("pq") for _ in range(2)]
                pv = [ps(f"pv{i}", [128, 512], scope=sc) for i in range(2)]; t_pv = [kb.tok("pv") for _ in range(2)]
                dma("pool", lambda q: q.dma_start(out=wv[:], in_=wsrc[:, :, 2048:3072]), writes=[t_wv], key=t_wv)

                def load_g(g):
                    dma("pool", lambda q: q.dma_start(out=wq[g % 2][:], in_=wsrc[:, :, g * 512:(g + 1) * 512]),
                        writes=[t_wq[g % 2]], key=t_wq[g % 2])
                load_g(0)
                it = 0
                for g in range(4):
                    if g + 1 < 4:
                        load_g(g + 1)
                    for cc in range(4):
                        c16 = g * 4 + cc
                        sbuf_ = stg[c16 % 2]; ts_ = t_stg[c16 % 2]
                        for tq in range(NQ):
                            b = it % 2
                            it += 1

                            def mm(pe):
                                for kc in range(8):
                                    r = pe.matmul(pq[b][:], lhsT=wq[g % 2][:, kc, cc * 128:(cc + 1) * 128],
                                                  rhs=xT[:, kc, tq * 512:(tq + 1) * 512], start=(kc == 0), stop=(kc == 7))
                                return r
                            op("pe", mm, reads=[t_wq[g % 2], t_xT], writes=[t_pq[b]])
                            scl = 0.125 if c16 < 8 else 1.0
                            if it % 2 == 0:
                                op("act", lambda a: a.mul(out=sbuf_[:, tq * 512:(tq + 1) * 512], in_=pq[b][:], mul=scl),
                                   reads=[t_pq[b]], writes=[ts_])
                            else:
                                op("dve", lambda v: v.tensor_scalar(out=sbuf_[:, tq * 512:(tq + 1) * 512], in0=pq[b][:], scalar1=scl,
                                                                    scalar2=None, op0=ALU.mult), reads=[t_pq[b]], writes=[ts_])
                        dst = qT_d[c16] if c16 < 8 else kT_d[c16 - 8]
                        dma("sp", lambda q: q.dma_start(out=dst, in_=sbuf_[:]), reads=[ts_], writes=[t_qkv], key=ts_, join=True)
                for i in range(NT):
                    b = i % 2
                    for hh in range(2):
                        def mmv(pe):
                            for kc in range(8):
                                r = pe.matmul(pv[hh][:], lhsT=xT[:, kc, i * 128:(i + 1) * 128],
                                              rhs=wv[:, kc, hh * 512:(hh + 1) * 512], start=(kc == 0), stop=(kc == 7))
                            return r
                        op("pe", mmv, reads=[t_xT, t_wv], writes=[t_pv[hh]])
                        if hh == 0:
                            op("act", lambda a: a.copy(out=vst[b][:, 0:512], in_=pv[hh][:]), reads=[t_pv[hh]], writes=[t_vst[b]])
                        else:
                            op("dve", lambda v: v.tensor_copy(out=vst[b][:, 512:1024], in_=pv[hh][:]), reads=[t_pv[hh]], writes=[t_vst[b]])
                    dma("sp", lambda q: q.dma_start(out=v_d[i * 128:(i + 1) * 128, :], in_=vst[b][:]), reads=[t_vst[b]],
                        writes=[t_qkv], key=t_vst[b], join=True)
                if kind == 2:
                    wf = sb("wf", [128, 8, 16], BF16, scope=sc); t_wf = kb.tok("wf")
                    bf_bc = sb("bf_bc", [128, 16], scope=sc)
                    basef = sb("basef", [128, 16], scope=sc); t_bf = kb.tok("basef")
                    fl = [sb(f"fl{i}", [128, 64], scope=sc) for i in range(2)]; t_fl = [kb.tok("fl") for _ in range(2)]
                    flb = [sb(f"flb{i}", [128, 32], BF16, scope=sc) for i in range(2)]; t_flb = [kb.tok("flb") for _ in range(2)]
                    pf = ps("pf", [128, 16], scope=sc); t_pf = kb.tok("pf")
                    pc = ps("pc", [128, 2, 16], scope=sc); t_pc = kb.tok("pc")
                    ptr = ps("ptr", [128, 3, 128], BF16, scope=sc); t_ptr = kb.tok("ptr")
                    nsp = [sb(f"nsp{i}", [128, 3, 16], BF16, scope=sc) for i in range(2)]; t_nsp = [kb.tok("nsp") for _ in range(2)]
                    nr = [sb(f"nr{i}", [128, 48], scope=sc) for i in range(2)]
                    dma("pool", lambda q: q.dma_start(out=wf[:], in_=wsrc[:, :, 3072:3088]), writes=[t_wf], key=t_wf)
                    dma("sp", lambda q: q.dma_start(out=bf_bc[:], in_=fox_b_f[0].partition_broadcast(128)), writes=[t_wf], key=t_wf, join=True)
                    op("pool", lambda g_: g_.memset(basef[:], 0.0), writes=[t_bf])
                    for i in range(NT):
                        b = i % 2
                        F = fl[b]

                        def mmf(pe):
                            for kc in range(8):
                                r = pe.matmul(pf[:], lhsT=xT[:, kc, i * 128:(i + 1) * 128], rhs=wf[:, kc, :], start=(kc == 0), stop=(kc == 7))
                            return r
                        op("pe", mmf, reads=[t_xT, t_wf], writes=[t_pf])
                        op("dve", lambda v: v.tensor_tensor(out=F[:, 0:16], in0=pf[:], in1=bf_bc[:], op=ALU.add), reads=[t_pf, t_wf], writes=[t_fl[b]])
                        op("act", lambda a: a.activation(out=F[:, 16:32], in_=F[:, 0:16], func=AF.Exp, scale=-1.0), writes=[t_fl[b]])
                        op("act", lambda a: a.activation(out=F[:, 32:48], in_=F[:, 16:32], func=AF.Ln, bias=c_one[:], scale=1.0),
                           reads=[t_const], writes=[t_fl[b]])
                        op("dve", lambda v: v.tensor_copy(out=flb[b][:, 0:16], in_=F[:, 32:48]), reads=[t_fl[b]], writes=[t_flb[b]])
                        op("dve", lambda v: v.tensor_copy(out=F[:, 48:64], in_=flb[b][:, 0:16]), writes=[t_fl[b]])
                        op("dve", lambda v: v.tensor_tensor(out=F[:, 48:64], in0=F[:, 32:48], in1=F[:, 48:64], op=ALU.subtract), writes=[t_fl[b]])
                        op("dve", lambda v: v.tensor_copy(out=flb[b][:, 16:32], in_=F[:, 48:64]), reads=[t_fl[b]], writes=[t_flb[b]])

                        def mmc(pe):
                            pe.matmul(pc[:, 0, :], lhsT=mask_le[:], rhs=flb[b][:, 0:16], start=True, stop=False)
                            pe.matmul(pc[:, 0, :], lhsT=mask_le[:], rhs=flb[b][:, 16:32], start=False, stop=True)
                            pe.matmul(pc[:, 1, :], lhsT=ones_b[:], rhs=flb[b][:, 0:16], start=True, stop=False)
                            return pe.matmul(pc[:, 1, :], lhsT=ones_b[:], rhs=flb[b][:, 16:32], start=False, stop=True)
                        op("pe", mmc, reads=[t_flb[b], t_const], writes=[t_pc])
                        op("dve", lambda v: v.tensor_tensor(out=ncum[:, i, :], in0=pc[:, 0, :], in1=basef[:], op=ALU.add),
                           reads=[t_pc, t_bf], writes=[t_cum])
                        op("dve", lambda v: v.tensor_tensor(out=basef[:], in0=basef[:], in1=pc[:, 1, :], op=ALU.add),
                           reads=[t_pc], writes=[t_bf])
                        N3 = nsp[b]
                        R_ = nr[b]
                        op("dve", lambda v: v.tensor_copy(out=N3[:, 0, :], in_=ncum[:, i, :]), reads=[t_cum], writes=[t_nsp[b]])
                        op("dve", lambda v: v.tensor_copy(out=R_[:, 0:16], in_=N3[:, 0, :]), writes=[t_nsp[b]])
                        op("dve", lambda v: v.tensor_tensor(out=R_[:, 16:32], in0=ncum[:, i, :], in1=R_[:, 0:16], op=ALU.subtract), writes=[t_nsp[b]])
                        op("dve", lambda v: v.tensor_copy(out=N3[:, 1, :], in_=R_[:, 16:32]), writes=[t_nsp[b]])
                        op("dve", lambda v: v.tensor_copy(out=R_[:, 0:16], in_=N3[:, 1, :]), writes=[t_nsp[b]])
                        op("dve", lambda v: v.tensor_tensor(out=R_[:, 32:48], in0=R_[:, 16:32], in1=R_[:, 0:16], op=ALU.subtract), writes=[t_nsp[b]])
                        op("dve", lambda v: v.tensor_copy(out=N3[:, 2, :], in_=R_[:, 32:48]), writes=[t_nsp[b]])

                        def trn(pe):
                            for g_ in range(3):
                                r = pe.transpose(out=ptr[0:16, g_, :], in_=N3[:, g_, :], identity=identb[:])
                            return r
                        op("pe", trn, reads=[t_nsp[b], t_const], writes=[t_ptr])
                        op("dve", lambda v: v.tensor_scalar(out=refB[0:16, :, i * 128:(i + 1) * 128], in0=ptr[0:16, :, :], scalar1=-1.0,
                                                            scalar2=None, op0=ALU.mult), reads=[t_ptr], writes=[t_cum])
                    dma("sp", lambda q: q.dma_start(out=nrow_d, in_=refB[0:16, :, :]), reads=[t_cum], writes=[t_qkv], key=t_cum, join=True)
                kb.barrier()

        def phase_attn(kind, mT, t_mT, ncum, refB, t_cum):
            with contextlib.ExitStack() as sc:
                KQ = 67 if kind == 2 else 64
                qs = [sb(f"qs{i}", [128, T], BF16, scope=sc) for i in range(2)]
                ks = [sb(f"ks{i}", [128, T], BF16, scope=sc) for i in range(2)]
                vs = [sb(f"vs{i}", [128, NT, 128], BF16, scope=sc) for i in range(2)]
                t_q = [kb.tok("q") for _ in range(2)]; t_k = [kb.tok("k") for _ in range(2)]; t_v = [kb.tok("v") for _ in range(2)]
                NB = 3
                ex = [sb(f"ex{i}", [128, 512], scope=sc) for i in range(NB)]; t_ex = [kb.tok("ex") for _ in range(NB)]
                spb = [sb(f"spb{i}", [128, 512], BF16, scope=sc) for i in range(NB)]; t_sp = [kb.tok("sp") for _ in range(NB)]
                wt = [sb(f"wt{i}", [128, 512], BF16, scope=sc) for i in range(NB)]; t_wt = [kb.tok("wt") for _ in range(NB)]
                acc = sb("acc", [128, 512], BF16, scope=sc); t_acc = kb.tok("acc")
                rden = sb("rden", [128, 512], scope=sc); t_rden = kb.tok("rden")
                pst = [ps(f"pst{i}", [128, 512], scope=sc) for i in range(2)]; t_pst = [kb.tok("pst") for _ in range(2)]
                parg = [ps(f"parg{i}", [128, 512], scope=sc) for i in range(2)]; t_parg = [kb.tok("parg") for _ in range(2)]
                po = [ps(f"po{i}", [128, 512], scope=sc) for i in range(2)]; t_po = [kb.tok("po") for _ in range(2)]
                pden = ps("pden", [128, 512], scope=sc); t_pden = kb.tok("pden")
                pfill = pden; t_fill = kb.tok("fill")
                vsrc = v_d.rearrange("(j p) d -> p j d", p=128)
                if kind == 2:
                    for b in range(2):
                        op("pool", lambda g: g.memset(ks[b][64:67, :], 1.0), writes=[t_k[b]])

                def load_h(hd):
                    b = hd % 2
                    c, hh = hd // 2, hd % 2
                    dma("sp", lambda q: q.dma_start(out=qs[b][0:64, :], in_=qT_d[c, hh * 64:(hh + 1) * 64, :]), reads=[t_qkv],
                        writes=[t_q[b]], key=t_q[b])
                    if kind == 2:
                        dma("sp", lambda q: q.dma_start(out=qs[b][64:67, :], in_=nrow_d[hd]), reads=[t_qkv],
                            writes=[t_q[b]], key=t_q[b], join=True)
                    dma("sp", lambda q: q.dma_start(out=ks[b][0:64, :], in_=kT_d[c, hh * 64:(hh + 1) * 64, :]), reads=[t_qkv],
                        writes=[t_k[b]], key=t_k[b], join=True)
                    if hh == 0:
                        dma("sp", lambda q: q.dma_start(out=vs[c % 2][:], in_=vsrc[:, :, c * 128:(c + 1) * 128]), reads=[t_qkv],
                            writes=[t_v[c % 2]], key=t_v[c % 2])
                load_h(0)
                it = 0
                grp = 0
                for hd in range(16):
                    c, hh = hd // 2, hd % 2
                    hb = hd % 2
                    cb = c % 2
                    P0 = hh * 64
                    if hd + 1 < 16:
                        load_h(hd + 1)
                    for tq in range(NQ):
                        jmax = 4 * tq + 3
                        pob = po[grp % 2]; t_pob = t_po[grp % 2]
                        grp += 1
                        if kind != 2:
                            op("pool", lambda g: g.memset(acc[:], 0.0), writes=[t_acc])
                        op("pe", lambda pe: pe.matmul(pob[:], lhsT=zeros_b[:], rhs=zsrc[:], start=True, stop=False),
                           reads=[t_const], writes=[t_pob])
                        if kind == 2:
                            op("pe", lambda pe: pe.matmul(pden[:], lhsT=zeros_b[:], rhs=zsrc[:], start=True, stop=False),
                               reads=[t_const], writes=[t_pden])
                        order = list(range(jmax + 1)) if kind == 2 else list(range(jmax, -1, -1))
                        ntile = len(order)
                        info = {}

                        def stage1(nj):
                            j = order[nj]
                            b2 = (it + nj) % 2
                            b3 = (it + nj) % NB
                            col0 = max(0, j - 4 * tq) * 128
                            diag = j >= 4 * tq
                            q0 = tq * 512 + col0
                            info[nj] = (j, b2, b3, col0, diag, q0)

                            def mms(pe):
                                r = pe.matmul(pst[b2][:, col0:512], lhsT=ks[hb][0:KQ, j * 128:(j + 1) * 128],
                                              rhs=qs[hb][0:KQ, q0:(tq + 1) * 512], start=True, stop=not (kind == 2 and diag))
                                if kind == 2 and diag:
                                    r = pe.matmul(pst[b2][:, col0:col0 + 128], lhsT=identb[:], rhs=negmask_gt[:], start=False, stop=True)
                                return r
                            op("pe", mms, reads=[t_q[hb], t_k[hb], t_const], writes=[t_pst[b2]])
                            if kind == 2:
                                op("act", lambda a: a.activation(out=wt[b3][:, col0:512], in_=pst[b2][:, col0:512], func=AF.Exp,
                                                                 bias=ncum[:, j, hd:hd + 1], scale=1.0),
                                   reads=[t_pst[b2], t_cum], writes=[t_wt[b3]])
                            else:
                                op("act", lambda a: a.activation(out=ex[b3][:, col0:512], in_=pst[b2][:, col0:512], func=AF.Exp),
                                   reads=[t_pst[b2]], writes=[t_ex[b3]])
                                op("act", lambda a: a.activation(out=spb[b3][:, col0:512], in_=ex[b3][:, col0:512], func=AF.Ln,
                                                                 bias=c_one[:], scale=1.0), reads=[t_ex[b3], t_const], writes=[t_sp[b3]])
                                if diag:
                                    op("pool", lambda g: g.tensor_tensor(out=spb[b3][:, col0:col0 + 128], in0=spb[b3][:, col0:col0 + 128],
                                                                         in1=mask_lt[:], op=ALU.mult), reads=[t_const], writes=[t_sp[b3]])

                        def stage2(nj):
                            (j, b2, b3, col0, diag, q0) = info[nj]

                            def mma(pe):
                                pe.matmul(parg[b2][:, col0:512], lhsT=ks[hb][0:64, j * 128:(j + 1) * 128],
                                          rhs=qs[hb][0:64, q0:(tq + 1) * 512], start=True, stop=False)
                                pe.matmul(parg[b2][:, col0:512], lhsT=ntri_ge[:], rhs=spb[b3][:, col0:512], start=False, stop=False)
                                return pe.matmul(parg[b2][:, col0:512], lhsT=nones_b[:], rhs=acc[:, col0:512], start=False, stop=True)
                            op("pe", mma, reads=[t_q[hb], t_k[hb], t_sp[b3], t_acc, t_const], writes=[t_parg[b2]])

                            def fill(pe):
                                pe.matmul(pfill[:], lhsT=zeros_b[:], rhs=zsrc[:], start=True, stop=True)
                                return pe.matmul(pfill[:], lhsT=zeros_b[:], rhs=zsrc[:], start=True, stop=True)
                            op("pe", fill, reads=[t_const], writes=[t_fill])
                            op("pool", lambda g: g.tensor_tensor(out=acc[:, col0:512], in0=acc[:, col0:512], in1=spb[b3][:, col0:512],
                                                                 op=ALU.add), reads=[t_sp[b3]], writes=[t_acc])
                            op("act", lambda a: a.activation(out=wt[b3][:, col0:512], in_=parg[b2][:, col0:512], func=AF.Exp),
                               reads=[t_parg[b2]], writes=[t_wt[b3]])
                            if diag:
                                op("pool", lambda g: g.tensor_tensor(out=wt[b3][:, col0:col0 + 128], in0=wt[b3][:, col0:col0 + 128],
                                                                     in1=mask_lt[:], op=ALU.mult), reads=[t_const], writes=[t_wt[b3]])

                        def stage3(nj):
                            (j, b2, b3, col0, diag, q0) = info[nj]
                            last = (nj == ntile - 1)

                            def mmo(pe):
                                r = pe.matmul(pob[:, col0:512], lhsT=vs[cb][:, j, :], rhs=wt[b3][:, col0:512], start=False, stop=last)
                                if kind == 2:
                                    r = pe.matmul(pden[:, col0:512], lhsT=ones_b[:], rhs=wt[b3][:, col0:512], start=False, stop=last)
                                return r
                            op("pe", mmo, reads=[t_wt[b3], t_v[cb], t_const], writes=[t_pob] + ([t_pden] if kind == 2 else []))

                        if kind == 2:
                            for n in range(ntile + 1):
                                if n < ntile:
                                    stage1(n)
                                if n >= 1:
                                    stage3(n - 1)
                        else:
                            for n in range(ntile + 2):
                                if n < ntile:
                                    stage1(n)
                                if 1 <= n <= ntile:
                                    stage2(n - 1)
                                if n >= 2:
                                    stage3(n - 2)
                        it += ntile
                        osl = mT[P0:P0 + 64, c, tq * 512:(tq + 1) * 512]
                        if kind == 2:
                            op("dve", lambda v: v.reciprocal(out=rden[P0:P0 + 64, :], in_=pden[P0:P0 + 64, :]), reads=[t_pden], writes=[t_rden])
                            op("dve", lambda v: v.tensor_tensor(out=osl, in0=pob[P0:P0 + 64, :], in1=rden[P0:P0 + 64, :], op=ALU.mult),
                               reads=[t_pob, t_rden], writes=[t_mT])
                        else:
                            op("dve", lambda v: v.tensor_copy(out=osl, in_=pob[P0:P0 + 64, :]), reads=[t_pob], writes=[t_mT])
                kb.barrier()

        def phase_outproj_ln1_route(li, w_out_d, xin, t_xin, mT, t_mT, gates, dests, t_route):
            with contextlib.ExitStack() as sc:
                wo = sb("wo", [128, 8, D], BF16, scope=sc); t_wo = kb.tok("wo")
                g_bc = sb("g_bc", [128, D], scope=sc); b_bc = sb("b_bc", [128, D], scope=sc); t_gb = kb.tok("gb")
                wr32 = sb("wr32", [128, 8, NE], scope=sc)
                wrh = sb("wrh", [128, 8, NE], BF16, scope=sc)
                wrl = sb("wrl", [128, 8, NE], BF16, scope=sc)
                wrt = sb("wrt", [128, 8, NE], scope=sc)
                rb_bc = sb("rb_bc", [128, NE], scope=sc)
                t_wr = kb.tok("wr")
                base = sb("base", [128, NE], scope=sc); t_base = kb.tok("base")
                xs = [sb(f"xs{i}", [128, D], scope=sc) for i in range(2)]; t_xs = [kb.tok("xs") for _ in range(2)]
                h = [sb(f"h{i}", [128, D], scope=sc) for i in range(2)]; t_h = [kb.tok("h") for _ in range(2)]
                x1 = [sb(f"x1_{i}", [128, D], scope=sc) for i in range(2)]; t_x1 = [kb.tok("x1") for _ in range(2)]
                x1h = [sb(f"x1h{i}", [128, D], BF16, scope=sc) for i in range(2)]; t_x1h = [kb.tok("x1h") for _ in range(2)]
                x1l = [sb(f"x1l{i}", [128, D], BF16, scope=sc) for i in range(2)]; t_x1l = [kb.tok("x1l") for _ in range(2)]
                x1t = [sb(f"x1t{i}", [128, D], scope=sc) for i in range(2)]
                xTh = [sb(f"xTh{i}", [128, 8, 128], BF16, scope=sc) for i in range(2)]; t_xTh = [kb.tok("xTh") for _ in range(2)]
                xTl = [sb(f"xTl{i}", [128, 8, 128], BF16, scope=sc) for i in range(2)]; t_xTl = [kb.tok("xTl") for _ in range(2)]
                stat = [sb(f"stat{i}", [128, 16], scope=sc) for i in range(2)]; t_stat = [kb.tok("stat") for _ in range(2)]
                rt = [sb(f"rt{i}", [128, 8 * NE], scope=sc) for i in range(2)]; t_rt = [kb.tok("rt") for _ in range(2)]
                mkb = [sb(f"mkb{i}", [128, NE], BF16, scope=sc) for i in range(2)]; t_mkb = [kb.tok("mkb") for _ in range(2)]
                sm = [sb(f"sm{i}", [128, 32], scope=sc) for i in range(2)]
                desti = sb("desti", [128, NT, 4], I32, scope=sc)
                py = [ps(f"py{i}", [128, D], scope=sc) for i in range(2)]; t_py = [kb.tok("py") for _ in range(2)]
                pTh = ps("pTh", [128, 8, 128], BF16, scope=sc); t_pTh = kb.tok("pTh")
                pTl = ps("pTl", [128, 8, 128], BF16, scope=sc); t_pTl = kb.tok("pTl")
                plog = ps("plog", [128, NE], scope=sc); t_plog = kb.tok("plog")
                pcnt = ps("pcnt", [128, 2, NE], scope=sc); t_pcnt = kb.tok("pcnt")

                dma("pool", lambda q: q.dma_start(out=wo[:], in_=w_out_d.rearrange("(kc p) n -> p kc n", p=128)),
                    writes=[t_wo], key=t_wo)
                dma("sp", lambda q: q.dma_start(out=g_bc[:], in_=ln1_g[li].partition_broadcast(128)), writes=[t_gb], key=t_gb)
                dma("sp", lambda q: q.dma_start(out=b_bc[:], in_=ln1_b[li].partition_broadcast(128)), writes=[t_gb], key=t_gb, join=True)
                dma("sp", lambda q: q.dma_start(out=wr32[:], in_=router_w[li].rearrange("(kc p) n -> p kc n", p=128)),
                    writes=[t_wr], key=t_wr)
                dma("sp", lambda q: q.dma_start(out=rb_bc[:], in_=router_b[li].partition_broadcast(128)), writes=[t_wr], key=t_wr, join=True)
                op("dve", lambda v: v.tensor_copy(out=wrh[:], in_=wr32[:]), reads=[t_wr], writes=[t_wr])
                op("dve", lambda v: v.tensor_copy(out=wrt[:], in_=wrh[:]), writes=[t_wr])
                op("dve", lambda v: v.tensor_tensor(out=wrt[:], in0=wr32[:], in1=wrt[:], op=ALU.subtract), writes=[t_wr])
                op("dve", lambda v: v.tensor_copy(out=wrl[:], in_=wrt[:]), writes=[t_wr])
                op("pool", lambda g: g.memset(base[:], 0.0), writes=[t_base])
                dma("sp", lambda q: q.dma_start(out=toklist.rearrange("(p r) o -> p (r o)", p=128), in_=zi[:]),
                    reads=[t_const], writes=[t_tl], key=t_tl)

                for i in range(NT):
                    b = i % 2
                    dma("sp", lambda q: q.dma_start(out=xs[b][:], in_=xin[i * 128:(i + 1) * 128, :]),
                        reads=[t_xin], writes=[t_xs[b]], key=t_xs[b])

                    def mm(pe):
                        for hh in range(2):
                            for c in range(8):
                                r = pe.matmul(py[b][:, hh * 512:(hh + 1) * 512], lhsT=mT[:, c, i * 128:(i + 1) * 128],
                                              rhs=wo[:, c, hh * 512:(hh + 1) * 512], start=(c == 0), stop=(c == 7))
                        return r
                    op("pe", mm, reads=[t_mT, t_wo], writes=[t_py[b]])
                    op("dve", lambda v: v.scalar_tensor_tensor(out=h[b][:], in0=xs[b][:], scalar=ALPHA, in1=py[b][:],
                                                               op0=ALU.mult, op1=ALU.add),
                       reads=[t_xs[b], t_py[b]], writes=[t_h[b]])
                    layer_norm_tile(h[b], g_bc, b_bc, x1[b], stat[b], t_h[b], t_x1[b], t_stat[b], t_gb)
                    dma("sp", lambda q: q.dma_start(out=x1res[i * 128:(i + 1) * 128, :], in_=x1[b][:]),
                        reads=[t_x1[b]], writes=[t_x1res], key=t_x1[b], join=True)
                    op("act", lambda a: a.copy(out=x1h[b][:], in_=x1[b][:]), reads=[t_x1[b]], writes=[t_x1h[b]])
                    dma("sp", lambda q: q.dma_start(out=x1bf[i * 128:(i + 1) * 128, :], in_=x1h[b][:]),
                        reads=[t_x1h[b]], writes=[t_x1bf], key=t_x1h[b], join=True)
                    op("pool", lambda g: g.tensor_copy(out=x1t[b][:], in_=x1h[b][:]), reads=[t_x1h[b]], writes=[t_x1l[b]])
                    op("pool", lambda g: g.tensor_tensor(out=x1t[b][:], in0=x1[b][:], in1=x1t[b][:], op=ALU.subtract),
                       reads=[t_x1[b]], writes=[t_x1l[b]])
                    op("pool", lambda g: g.tensor_copy(out=x1l[b][:], in_=x1t[b][:]), writes=[t_x1l[b]])
                    for (src, tsrc, pp, tpp, dst, tdst) in ((x1h[b], t_x1h[b], pTh, t_pTh, xTh[b], t_xTh[b]),
                                                            (x1l[b], t_x1l[b], pTl, t_pTl, xTl[b], t_xTl[b])):
                        def tr(pe):
                            for c in range(8):
                                r = pe.transpose(out=pp[:, c, :], in_=src[:, c * 128:(c + 1) * 128], identity=identb[:])
                            return r
                        op("pe", tr, reads=[tsrc, t_const], writes=[tpp])
                        op("act", lambda a: a.copy(out=dst[:], in_=pp[:]), reads=[tpp], writes=[tdst])

                    def mmr(pe):
                        n = 0
                        for (xa, wa) in ((xTh[b], wrh), (xTh[b], wrl), (xTl[b], wrh)):
                            for c in range(8):
                                r = pe.matmul(plog[:], lhsT=xa[:, c, :], rhs=wa[:, c, :], start=(n == 0), stop=(n == 23))
                                n += 1
                        return r
                    op("pe", mmr, reads=[t_xTh[b], t_xTl[b], t_wr], writes=[t_plog])
                    R = rt[b]
                    L = R[:, 0:32]; MK = R[:, 32:64]; EX = R[:, 64:96]; G = R[:, 96:128]
                    POS = R[:, 128:160]; TMP = R[:, 160:192]; OH = R[:, 192:224]; TMP2 = R[:, 224:256]
                    S = sm[b]
                    top8 = S[:, 0:8]; nmax = S[:, 8:9]; den = S[:, 9:10]; rden = S[:, 10:11]
                    dstf = S[:, 12:16]
                    tr_ = t_rt[b]
                    op("dve", lambda v: v.tensor_tensor(out=L, in0=plog[:], in1=rb_bc[:], op=ALU.add),
                       reads=[t_plog, t_wr], writes=[tr_])
                    op("dve", lambda v: v.max(out=top8, in_=L), writes=[tr_])
                    op("dve", lambda v: v.tensor_scalar(out=MK, in0=L, scalar1=S[:, 3:4], scalar2=None, op0=ALU.is_ge), writes=[tr_])
                    op("dve", lambda v: v.tensor_scalar(out=nmax, in0=S[:, 0:1], scalar1=-1.0, scalar2=None, op0=ALU.mult), writes=[tr_])
                    op("act", lambda a: a.activation(out=EX, in_=L, func=AF.Exp, bias=nmax, scale=1.0), writes=[tr_])
                    op("dve", lambda v: v.tensor_tensor(out=EX, in0=EX, in1=MK, op=ALU.mult), writes=[tr_])
                    op("dve", lambda v: v.reduce_sum(out=den, in_=EX, axis=AX.X), writes=[tr_])
                    op("dve", lambda v: v.reciprocal(out=rden, in_=den), writes=[tr_])
                    op("dve", lambda v: v.tensor_scalar(out=G, in0=EX, scalar1=rden, scalar2=None, op0=ALU.mult), writes=[tr_])
                    op("dve", lambda v: v.tensor_copy(out=mkb[b][:], in_=MK), reads=[tr_], writes=[t_mkb[b]])

                    def mmc(pe):
                        pe.matmul(pcnt[:, 0, :], lhsT=tri_lt[:], rhs=mkb[b][:], start=True, stop=True)
                        return pe.matmul(pcnt[:, 1, :], lhsT=ones_b[:], rhs=mkb[b][:], start=True, stop=True)
                    op("pe", mmc, reads=[t_mkb[b], t_const], writes=[t_pcnt])
                    op("dve", lambda v: v.tensor_tensor(out=POS, in0=pcnt[:, 0, :], in1=base[:], op=ALU.add),
                       reads=[t_pcnt, t_base], writes=[tr_])
                    op("dve", lambda v: v.tensor_tensor(out=base[:], in0=base[:], in1=pcnt[:, 1, :], op=ALU.add),
                       reads=[t_pcnt], writes=[t_base])
                    op("dve", lambda v: v.tensor_scalar(out=TMP, in0=POS, scalar1=float(C), scalar2=None, op0=ALU.is_lt), writes=[tr_])
                    op("dve", lambda v: v.tensor_tensor(out=POS, in0=POS, in1=ecol[:], op=ALU.add), reads=[t_const], writes=[tr_])
                    op("dve", lambda v: v.tensor_scalar(out=POS, in0=POS, scalar1=float(-NSLOT), scalar2=None, op0=ALU.add), writes=[tr_])
                    op("dve", lambda v: v.tensor_tensor(out=POS, in0=POS, in1=TMP, op=ALU.mult), writes=[tr_])
                    op("dve", lambda v: v.tensor_scalar(out=POS, in0=POS, scalar1=float(NSLOT), scalar2=None, op0=ALU.add), writes=[tr_])
                    for k in range(4):
                        op("dve", lambda v: v.tensor_scalar(out=OH, in0=L, scalar1=S[:, k:k + 1], scalar2=None, op0=ALU.is_equal), writes=[tr_])
                        op("dve", lambda v: v.tensor_tensor(out=TMP2, in0=OH, in1=POS, op=ALU.mult), writes=[tr_])
                        op("dve", lambda v: v.reduce_sum(out=dstf[:, k:k + 1], in_=TMP2, axis=AX.X), writes=[tr_])
                        op("dve", lambda v: v.tensor_tensor(out=TMP2, in0=OH, in1=G, op=ALU.mult), writes=[tr_])
                        op("dve", lambda v: v.reduce_sum(out=gates[:, i, k:k + 1], in_=TMP2, axis=AX.X), writes=[tr_, t_route])
                    op("dve", lambda v: v.tensor_copy(out=dests[:, i, :], in_=dstf), reads=[tr_], writes=[t_route])
                    for k in range(4):
                        dma("pool", lambda q: q.indirect_dma_start(
                            out=toklist[:, :], out_offset=bass.IndirectOffsetOnAxis(ap=dests[:, i, k:k + 1], axis=0),
                            in_=tokid[:, i:i + 1], in_offset=None), reads=[t_route, t_const], writes=[t_tl], key=t_tl, join=True)
                kb.barrier()

        def phase_experts(li):
            with contextlib.ExitStack() as sc:
                wg = [sb(f"wg{i}", [128, 8, D], BF16, scope=sc) for i in range(2)]
                wu = [sb(f"wu{i}", [128, 8, D], BF16, scope=sc) for i in range(2)]
                wd = [sb(f"wd{i}", [128, 8, D], BF16, scope=sc) for i in range(2)]
                t_wg = [kb.tok("wg") for _ in range(2)]; t_wu = [kb.tok("wu") for _ in range(2)]; t_wd = [kb.tok("wd") for _ in range(2)]
                bd = [sb(f"bd{i}", [128, D], scope=sc) for i in range(2)]; t_bd = [kb.tok("bd") for _ in range(2)]
                braw = sb("braw", [NE, 2, D], scope=sc)
                brb = sb("brb", [NE, 2, D], BF16, scope=sc)
                bT = sb("bT", [128, 2, 8, NE], scope=sc)
                t_b = kb.tok("bias")
                idx = [sb(f"idx{i}", [128, CR], I32, scope=sc) for i in range(2)]; t_idx = [kb.tok("idx") for _ in range(2)]
                xg = [sb(f"xg{i}", [128, CR, D], BF16, scope=sc) for i in range(2)]; t_xg = [kb.tok("xg") for _ in range(2)]
                xeT = sb("xeT", [128, 8, C], BF16, scope=sc); t_xeT = kb.tok("xeT")
                aT = sb("aT", [128, 8, C], BF16, scope=sc); t_aT = kb.tok("aT")
                gt = [sb(f"gt{i}", [128, 512], scope=sc) for i in range(2)]; t_gt = [kb.tok("gt") for _ in range(2)]
                sg = [sb(f"sg{i}", [128, 512], scope=sc) for i in range(2)]; t_sg = [kb.tok("sg") for _ in range(2)]
                ut = [sb(f"ut{i}", [128, 512], scope=sc) for i in range(2)]; t_ut = [kb.tok("ut") for _ in range(2)]
                yo = [sb(f"yo{i}", [128, D], scope=sc) for i in range(2)]; t_yo = [kb.tok("yo") for _ in range(2)]
                pT = ps("pT", [128, 8, 128], BF16, scope=sc); t_pT = kb.tok("pT")
                pTs = [pT, ps("pTb", [128, 8, 128], BF16, scope=sc)]; t_pTs = [t_pT, kb.tok("pTb")]
                pg = [ps(f"pg{i}", [128, 512], scope=sc) for i in range(2)]; t_pg = [kb.tok("pg") for _ in range(2)]
                pu = [ps(f"pu{i}", [128, 512], scope=sc) for i in range(2)]; t_pu = [kb.tok("pu") for _ in range(2)]
                py = ps("py", [128, 1024], scope=sc); t_py = kb.tok("py")
                t_pyh = [kb.tok("pyh0"), kb.tok("pyh1")]
                t_yoh = [kb.tok("yoh") for _ in range(2)]
                ytmp = [sb(f"ytmp{i}", [128, 512], scope=sc) for i in range(2)]; t_ytmp = [kb.tok("ytmp") for _ in range(2)]
                pbt = pT[:, :, 0:NE]; t_pbt = t_pT

                dma("sp", lambda q: q.dma_start(out=braw[:, 0, :], in_=exp_b_gate[li]), writes=[t_b], key=t_b)
                dma("sp", lambda q: q.dma_start(out=braw[:, 1, :], in_=exp_b_up[li]), writes=[t_b], key=t_b, join=True)
                op("dve", lambda v: v.tensor_copy(out=brb[:], in_=braw[:]), reads=[t_b], writes=[t_b])
                for g in range(2):
                    def tr(pe):
                        for c in range(8):
                            r = pe.transpose(out=pT[:, c, 0:NE], in_=brb[:, g, c * 128:(c + 1) * 128], identity=identb[0:NE, 0:NE])
                        return r
                    op("pe", tr, reads=[t_b, t_const], writes=[t_pbt])
                    op("dve", lambda v: v.tensor_copy(out=bT[:, g, :, :], in_=pT[:, :, 0:NE]), reads=[t_pbt], writes=[t_b])

                def load_w(e):
                    b = e % 2
                    for (dst, td, src) in ((wg[b], t_wg[b], exp_w_gate), (wu[b], t_wu[b], exp_w_up), (wd[b], t_wd[b], exp_w_down)):
                        dma("pool", lambda q: q.dma_start(out=dst[:], in_=src[li, e].rearrange("(kc p) n -> p kc n", p=128)),
                            writes=[td], key=td)
                    dma("sp", lambda q: q.dma_start(out=bd[b][:], in_=exp_b_down[li, e].partition_broadcast(128)),
                        writes=[t_bd[b]], key=t_bd[b])

                def load_x(e):
                    b = e % 2
                    dma("sp", lambda q: q.dma_start(out=idx[b][:], in_=toklist[e * C:(e + 1) * C, :].rearrange("(p r) o -> p (r o)", p=128)),
                        reads=[t_tl], writes=[t_idx[b]], key=t_idx[b])
                    for r in range(CR):
                        dma("pool", lambda q: q.indirect_dma_start(
                            out=xg[b][:, r, :], out_offset=None, in_=x1bf[:, :],
                            in_offset=bass.IndirectOffsetOnAxis(ap=idx[b][:, r:r + 1], axis=0)),
                            reads=[t_idx[b], t_x1bf], writes=[t_xg[b]], key=t_xg[b], join=(r > 0))

                load_x(0)
                load_w(0)
                it = 0
                for e in range(NE):
                    b = e % 2
                    if e + 1 < NE:
                        load_x(e + 1)
                        load_w(e + 1)
                    for r in range(CR):
                        pTr, t_pTr = pTs[r % 2], t_pTs[r % 2]

                        def tr(pe):
                            for c in range(8):
                                rr = pe.transpose(out=pTr[:, c, :], in_=xg[b][:, r, c * 128:(c + 1) * 128], identity=identb[:])
                            return rr
                        op("pe", tr, reads=[t_xg[b], t_const], writes=[t_pTr])
                        if r % 2 == 0:
                            op("act", lambda a: a.copy(out=xeT[:, :, r * 128:(r + 1) * 128], in_=pTr[:]), reads=[t_pTr], writes=[t_xeT])
                        else:
                            op("dve", lambda v: v.tensor_copy(out=xeT[:, :, r * 128:(r + 1) * 128], in_=pTr[:]), reads=[t_pTr], writes=[t_xeT])
                    segs = [(s0, min(512, C - s0)) for s0 in range(0, C, 512)]
                    for fc in range(8):
                        for (s0, sn) in segs:
                            bb = it % 2
                            it += 1
                            for (pp, tp, w_, tw) in ((pg[bb], t_pg[bb], wg[b], t_wg[b]), (pu[bb], t_pu[bb], wu[b], t_wu[b])):
                                def mm(pe):
                                    for kc in range(8):
                                        rr = pe.matmul(pp[:, 0:sn], lhsT=w_[:, kc, fc * 128:(fc + 1) * 128],
                                                       rhs=xeT[:, kc, s0:s0 + sn], start=(kc == 0), stop=(kc == 7))
                                    return rr
                                op("pe", mm, reads=[tw, t_xeT], writes=[tp])
                            G_, S_, U_ = gt[bb][:, 0:sn], sg[bb][:, 0:sn], ut[bb][:, 0:sn]
                            op("dve", lambda v: v.tensor_scalar(out=G_, in0=pg[bb][:, 0:sn], scalar1=bT[:, 0, fc, e:e + 1], scalar2=7.0,
                                                                op0=ALU.add, op1=ALU.min), reads=[t_pg[bb], t_b], writes=[t_gt[bb]])
                            op("act", lambda a: a.activation(out=S_, in_=G_, func=AF.Silu, scale=1.702),
                               reads=[t_gt[bb]], writes=[t_sg[bb]])
                            op("dve", lambda v: v.tensor_scalar(out=U_, in0=pu[bb][:, 0:sn], scalar1=bT[:, 1, fc, e:e + 1], scalar2=7.0,
                                                                op0=ALU.add, op1=ALU.min), reads=[t_pu[bb], t_b], writes=[t_ut[bb]])
                            op("dve", lambda v: v.tensor_scalar(out=U_, in0=U_, scalar1=-7.0, scalar2=1.0 / 1.702,
                                                                op0=ALU.max, op1=ALU.mult), writes=[t_ut[bb]])
                            op("dve", lambda v: v.scalar_tensor_tensor(out=aT[:, fc, s0:s0 + sn], in0=U_, scalar=1.0 / 1.702, in1=S_,
                                                                       op0=ALU.add, op1=ALU.mult),
                               reads=[t_sg[bb], t_ut[bb]], writes=[t_aT])
                    for r in range(CR):
                        yb = (e * CR + r) % 2

                        for hh in range(2):
                            def mmd(pe):
                                for fc in range(8):
                                    rr = pe.matmul(py[:, hh * 512:(hh + 1) * 512], lhsT=aT[:, fc, r * 128:(r + 1) * 128],
                                                   rhs=wd[b][:, fc, hh * 512:(hh + 1) * 512], start=(fc == 0), stop=(fc == 7))
                                return rr
                            op("pe", mmd, reads=[t_aT, t_wd[b]], writes=[t_pyh[hh]])
                            if hh == 0:
                                op("dve", lambda v: v.tensor_tensor(out=yo[yb][:, 0:512], in0=py[:, 0:512], in1=bd[b][:, 0:512], op=ALU.add),
                                   reads=[t_pyh[hh], t_bd[b]], writes=[t_yo[yb]])
                            else:
                                op("pool", lambda g: g.tensor_copy(out=yo[yb][:, 512:1024], in_=bd[b][:, 512:1024]), reads=[t_bd[b]], writes=[t_yoh[yb]])
                                op("act", lambda a: a.copy(out=ytmp[yb][:], in_=py[:, 512:1024]),
                                   reads=[t_pyh[hh]], writes=[t_ytmp[yb]])
                                op("pool", lambda g: g.tensor_tensor(out=yo[yb][:, 512:1024], in0=yo[yb][:, 512:1024], in1=ytmp[yb][:], op=ALU.add),
                                   reads=[t_ytmp[yb], t_yoh[yb]], writes=[t_yoh[yb]])
                        ydst = yslots[e * C:(e + 1) * C, :].rearrange("(p r) d -> p r d", p=128)
                        dma("sp", lambda q: q.dma_start(out=ydst[:, r, :], in_=yo[yb][:]),
                            reads=[t_yo[yb], t_yoh[yb]], writes=[t_ys], key=t_yo[yb], join=True)
                kb.barrier()

        def phase_combine_ln2_ple(li, gates, dests, t_route, xout, t_xout):
            with contextlib.ExitStack() as sc:
                wpg = sb("wpg", [128, 8, D], BF16, scope=sc); wpp = sb("wpp", [128, 2, D], BF16, scope=sc); t_w = kb.tok("wple")
                g_bc = sb("g_bc", [128, D], scope=sc); b_bc = sb("b_bc", [128, D], scope=sc)
                bpg = sb("bpg", [128, D], scope=sc); t_gb = kb.tok("gb")
                yk = [[sb(f"yk{i}_{k}", [128, D], scope=sc) for k in range(4)] for i in range(3)]
                t_yk = [[kb.tok("yk") for k in range(4)] for i in range(3)]
                xs = [sb(f"xs{i}", [128, D], scope=sc) for i in range(3)]; t_xs = [kb.tok("xs") for _ in range(3)]
                h = [sb(f"h{i}", [128, D], scope=sc) for i in range(2)]; t_h = [kb.tok("h") for _ in range(2)]
                x2 = [sb(f"x2_{i}", [128, D], scope=sc) for i in range(2)]; t_x2 = [kb.tok("x2") for _ in range(2)]
                x2b = [sb(f"x2b{i}", [128, D], BF16, scope=sc) for i in range(2)]; t_x2b = [kb.tok("x2b") for _ in range(2)]
                x2T = [sb(f"x2T{i}", [128, 8, 128], BF16, scope=sc) for i in range(2)]; t_x2T = [kb.tok("x2T") for _ in range(2)]
                pp32 = [sb(f"pp32_{i}", [128, PLE], scope=sc) for i in range(3)]; t_pp = [kb.tok("pp") for _ in range(3)]
                ppb = [sb(f"ppb{i}", [128, PLE], BF16, scope=sc) for i in range(2)]; t_ppb = [kb.tok("ppb") for _ in range(2)]
                ppT = [sb(f"ppT{i}", [128, 2, 128], BF16, scope=sc) for i in range(2)]; t_ppT = [kb.tok("ppT") for _ in range(2)]
                stat = [sb(f"stat{i}", [128, 16], scope=sc) for i in range(2)]; t_stat = [kb.tok("stat") for _ in range(2)]
                gs = [sb(f"gs{i}", [128, D], scope=sc) for i in range(2)]; t_gs = [kb.tok("gs") for _ in range(2)]
                xo = [sb(f"xo{i}", [128, D], scope=sc) for i in range(2)]; t_xo = [kb.tok("xo") for _ in range(2)]
                pT = ps("pT", [128, 8, 128], BF16, scope=sc); t_pT = kb.tok("pT")
                pT2 = ps("pT2", [128, 2, 128], BF16, scope=sc); t_pT2 = kb.tok("pT2")
                pgt = ps("pgt", [128, D], scope=sc); t_pgt = kb.tok("pgt")
                ppj = ps("ppj", [128, D], scope=sc); t_ppj = kb.tok("ppj")
                dma("pool", lambda q: q.dma_start(out=wpg[:], in_=ple_w_gate[li].rearrange("(kc p) n -> p kc n", p=128)), writes=[t_w], key=t_w)
                dma("pool", lambda q: q.dma_start(out=wpp[:], in_=ple_w_proj[li].rearrange("(kc p) n -> p kc n", p=128)), writes=[t_w], key=t_w, join=True)
                dma("sp", lambda q: q.dma_start(out=g_bc[:], in_=ln2_g[li].partition_broadcast(128)), writes=[t_gb], key=t_gb)
                dma("sp", lambda q: q.dma_start(out=b_bc[:], in_=ln2_b[li].partition_broadcast(128)), writes=[t_gb], key=t_gb, join=True)
                dma("sp", lambda q: q.dma_start(out=bpg[:], in_=ple_b_gate[li].partition_broadcast(128)), writes=[t_gb], key=t_gb, join=True)
                def loads(i):
                    b3 = i % 3
                    dma("sp", lambda q: q.dma_start(out=xs[b3][:], in_=x1res[i * 128:(i + 1) * 128, :]),
                        reads=[t_x1res], writes=[t_xs[b3]], key=t_xs[b3])
                    dma("sp", lambda q: q.dma_start(out=pp32[b3][:], in_=p_in[li, i * 128:(i + 1) * 128, :]),
                        writes=[t_pp[b3]], key=t_pp[b3])
                    for k in range(4):
                        dma("pool", lambda q: q.indirect_dma_start(
                            out=yk[b3][k][:], out_offset=None, in_=yslots[:, :],
                            in_offset=bass.IndirectOffsetOnAxis(ap=dests[:, i, k:k + 1], axis=0)),
                            reads=[t_route, t_ys], writes=[t_yk[b3][k]], key=t_yk[b3][k])
                loads(0)
                for i in range(NT):
                    b = i % 2
                    b3 = i % 3
                    if i + 1 < NT:
                        loads(i + 1)
                    op("act", lambda a: a.mul(out=h[b][:], in_=xs[b3][:], mul=ALPHA), reads=[t_xs[b3]], writes=[t_h[b]])
                    for k in range(4):
                        op("dve", lambda v: v.scalar_tensor_tensor(out=h[b][:], in0=yk[b3][k][:], scalar=gates[:, i, k:k + 1],
                                                                   in1=h[b][:], op0=ALU.mult, op1=ALU.add),
                           reads=[t_yk[b3][k], t_route], writes=[t_h[b]])
                    layer_norm_tile(h[b], g_bc, b_bc, x2[b], stat[b], t_h[b], t_x2[b], t_stat[b], t_gb)
                    op("act", lambda a: a.copy(out=x2b[b][:], in_=x2[b][:]), reads=[t_x2[b]], writes=[t_x2b[b]])
                    op("act", lambda a: a.copy(out=ppb[b][:], in_=pp32[b3][:]), reads=[t_pp[b3]], writes=[t_ppb[b]])

                    def tr(pe):
                        for c in range(8):
                            r = pe.transpose(out=pT[:, c, :], in_=x2b[b][:, c * 128:(c + 1) * 128], identity=identb[:])
                        return r
                    op("pe", tr, reads=[t_x2b[b], t_const], writes=[t_pT])
                    op("act", lambda a: a.copy(out=x2T[b][:], in_=pT[:]), reads=[t_pT], writes=[t_x2T[b]])

                    def tr2(pe):
                        for c in range(2):
                            r = pe.transpose(out=pT2[:, c, :], in_=ppb[b][:, c * 128:(c + 1) * 128], identity=identb[:])
                        return r
                    op("pe", tr2, reads=[t_ppb[b], t_const], writes=[t_pT2])
                    op("act", lambda a: a.copy(out=ppT[b][:], in_=pT2[:]), reads=[t_pT2], writes=[t_ppT[b]])

                    def mmg(pe):
                        for hh in range(2):
                            for c in range(8):
                                r = pe.matmul(pgt[:, hh * 512:(hh + 1) * 512], lhsT=x2T[b][:, c, :], rhs=wpg[:, c, hh * 512:(hh + 1) * 512],
                                              start=(c == 0), stop=(c == 7))
                        return r
                    op("pe", mmg, reads=[t_x2T[b], t_w], writes=[t_pgt])

                    def mmp(pe):
                        for hh in range(2):
                            for c in range(2):
                                r = pe.matmul(ppj[:, hh * 512:(hh + 1) * 512], lhsT=ppT[b][:, c, :], rhs=wpp[:, c, hh * 512:(hh + 1) * 512],
                                              start=(c == 0), stop=(c == 1))
                        return r
                    op("pe", mmp, reads=[t_ppT[b], t_w], writes=[t_ppj])
                    op("dve", lambda v: v.tensor_tensor(out=gs[b][:], in0=pgt[:], in1=bpg[:], op=ALU.add),
                       reads=[t_pgt, t_gb], writes=[t_gs[b]])
                    op("act", lambda a: a.activation(out=gs[b][:], in_=gs[b][:], func=AF.Sigmoid), writes=[t_gs[b]])
                    op("dve", lambda v: v.tensor_tensor(out=xo[b][:], in0=ppj[:], in1=gs[b][:], op=ALU.mult),
                       reads=[t_ppj, t_gs[b]], writes=[t_xo[b]])
                    op("pool", lambda g: g.tensor_tensor(out=xo[b][:], in0=xo[b][:], in1=x2[b][:], op=ALU.add),
                       reads=[t_x2[b]], writes=[t_xo[b]])
                    dma("sp", lambda q: q.dma_start(out=xout[i * 128:(i + 1) * 128, :], in_=xo[b][:]),
                        reads=[t_xo[b]], writes=[t_xout], key=t_xo[b], join=True)
                kb.barrier()

        gates = sb("gates", [128, NT, 4])
        dests = sb("dests", [128, NT, 4], I32)
        t_route = kb.tok("route")
        xin, t_xin = x_in, kb.tok("xin")
        nl = len(layers)
        for n, li in enumerate(layers):
            kind, j = KINDS[li], JIDX[li]
            last = (n == nl - 1)
            xout, t_xout = (out_d, t_out) if last else (xres[n % 2], t_xres[n % 2])
            with contextlib.ExitStack() as lsc:
                t_mT = kb.tok("mT")
                if kind == 0:
                    mT = sb("mT", [128, 8, T], BF16, scope=lsc)
                    with contextlib.ExitStack() as asc:
                        xT = sb("xT", [128, 8, T], BF16, scope=asc); t_xT = kb.tok("xT")
                        phase_xT(xin, t_xin, xT, t_xT)
                        phase_conv(j, xT, t_xT, mT, t_mT)
                    w_out_d = conv_w_out[j]
                else:
                    ncum = sb("ncum", [128, NT, 16], scope=lsc)
                    refB = sb("refB", [16, 3, T], BF16, scope=lsc)
                    t_cum = kb.tok("cum")
                    w_in_d = sb_w_in[0] if kind == 1 else fox_w_in[0]
                    with contextlib.ExitStack() as asc:
                        xT = sb("xT", [128, 8, T], BF16, scope=asc); t_xT = kb.tok("xT")
                        phase_xT(xin, t_xin, xT, t_xT)
                        phase_qkv(kind, w_in_d, xT, t_xT, ncum, refB, t_cum)
                    mT = sb("mT", [128, 8, T], BF16, scope=lsc)
                    phase_attn(kind, mT, t_mT, ncum, refB, t_cum)
                    w_out_d = sb_w_out[0] if kind == 1 else fox_w_out[0]
                phase_outproj_ln1_route(li, w_out_d, xin, t_xin, mT, t_mT, gates, dests, t_route)
            phase_experts(li)
            phase_combine_ln2_ple(li, gates, dests, t_route, xout, t_xout)
            xin, t_xin = xout, t_xout
        kb.barrier()
    return nc


_NAMES = ["x", "p", "conv_w_in", "conv_w", "conv_w_out", "sb_w_in", "sb_w_out", "fox_w_in", "fox_b_f", "fox_w_out",
          "ln1_g", "ln1_b", "ln2_g", "ln2_b", "router_w", "router_b", "exp_w_gate", "exp_b_gate", "exp_w_up",
          "exp_b_up", "exp_w_down", "exp_b_down", "ple_w_proj", "ple_w_gate", "ple_b_gate"]


def kernel(**inputs):
    B = inputs["x"].shape[0]
    T = inputs["x"].shape[1]
    nc = build_program(T=T)
    in_maps = []
    shared = {k: np.ascontiguousarray(inputs[k], dtype=np.float32) for k in _NAMES if k not in ("x", "p")}
    for b in range(B):
        m = dict(shared)
        m["x"] = np.ascontiguousarray(inputs["x"][b], dtype=np.float32)
        m["p"] = np.ascontiguousarray(inputs["p"][:, b], dtype=np.float32)
        in_maps.append(m)
    res = run_bass_kernel_spmd(nc, in_maps, core_ids=list(range(B)))
    return np.stack([np.asarray(r["out"]) for r in res.results], axis=0).astype(np.float32)
```
